# Optimizing a Trainium2 kernel written in Bass

```python
import math
import jax
import jax.numpy as jnp
from jax import lax
import numpy as np

D_MODEL = 1024
BATCH = 8
SEQ = 2048
DEPTH = 2

GRID_W = 64
CTX_LEN = 256
ROT_DIM = 64
ROPE_BASE = 10000.0
Q_BLOCK = 128
CHUNK = 32

DA_HEADS = 4
DA_DH = ROT_DIM
DA_DV = 2 * DA_DH
GLA_HEADS = 4
GLA_DK = 64
GLA_DV = 128
GLA_LR = 16
GLA_NORMALIZER = 16.0
MLA_HEADS = 4
MLA_Q_RANK = 256
MLA_KV_RANK = 128
MLA_NOPE = 128
MLA_ROPE = ROT_DIM
MLA_DV = 128
MLA_SCALE = (MLA_NOPE + MLA_ROPE) ** -0.5
HG_HEADS = 4
HG_DK = 128
HG_DV = 128
D_FF = 3584
N_EXPERTS = 8
TOP_K = 2

LN_EPS = 1e-5
RMS_EPS = 1e-6
DN_ALPHA = (2 * DEPTH) ** 0.25
DN_BETA = (8 * DEPTH) ** -0.25
N_EVEN = (DEPTH + 1) // 2
N_ODD = DEPTH // 2

A_QK = DA_HEADS * 2 * DA_DH
A_V = DA_HEADS * DA_DV
B_QK = GLA_HEADS * GLA_DK
B_V = GLA_HEADS * GLA_DV
EV_SIZES = (A_QK, A_QK, A_V, B_QK, B_QK, B_V, B_V, GLA_LR, GLA_LR)
EV_IN = sum(EV_SIZES)
EV_MIX = A_V + B_V
HG_K = HG_HEADS * HG_DK
HG_W = HG_HEADS * HG_DV
OD_SIZES = (MLA_Q_RANK, MLA_KV_RANK, MLA_ROPE, HG_K, HG_K, HG_K, HG_W, HG_W)
OD_IN = sum(OD_SIZES)
OD_MIX = MLA_HEADS * MLA_DV + HG_W

kernel_name = "hybrid_diffusion_backbone"


def _layernorm(x, g, b):
    xf = x.astype(jnp.float32)
    mu = jnp.mean(xf, axis=-1, keepdims=True)
    var = jnp.mean(jnp.square(xf - mu), axis=-1, keepdims=True)
    return ((xf - mu) * lax.rsqrt(var + LN_EPS) * g + b).astype(x.dtype)


def _rmsnorm(x, g):
    xf = x.astype(jnp.float32)
    return (xf * lax.rsqrt(jnp.mean(xf * xf, axis=-1, keepdims=True) + RMS_EPS) * g).astype(x.dtype)


def _split_cols(t, sizes):
    return jnp.split(t, np.cumsum(sizes)[:-1].tolist(), axis=-1)


def _heads(t, n):
    b, s, _ = t.shape
    return t.reshape(b, s, n, -1).transpose(0, 2, 1, 3)


def _unheads(t):
    b, n, s, d = t.shape
    return t.transpose(0, 2, 1, 3).reshape(b, s, n * d)


def _axial_rope_table(rows, rot_dim):
    n_freq = rot_dim // 4
    inv = ROPE_BASE ** (-jnp.arange(n_freq, dtype=jnp.float32) / n_freq)
    row = jnp.repeat(jnp.arange(rows, dtype=jnp.float32), GRID_W)
    col = jnp.tile(jnp.arange(GRID_W, dtype=jnp.float32), rows)
    ang = jnp.concatenate([row[:, None] * inv, col[:, None] * inv], axis=-1)
    return jnp.cos(ang), jnp.sin(ang)


def _rope(t, cos, sin):
    tf = t.astype(jnp.float32).reshape(t.shape[:-1] + (-1, 2))
    t0, t1 = tf[..., 0], tf[..., 1]
    out = jnp.stack([t0 * cos - t1 * sin, t0 * sin + t1 * cos], axis=-1)
    return out.reshape(t.shape).astype(t.dtype)


def _split_q_blocks(t):
    b, h, s, d = t.shape
    return t.reshape(b, h, s // Q_BLOCK, Q_BLOCK, d).transpose(2, 0, 1, 3, 4)


def _merge_q_blocks(t):
    nb, b, h, q, d = t.shape
    return t.transpose(1, 2, 0, 3, 4).reshape(b, h, nb * q, d)


def _softmax32(s):
    return jax.nn.softmax(s.astype(jnp.float32), axis=-1)


def _diff_attend(q1, q2, k1, k2, v, lam):
    scale = DA_DH ** -0.5
    p1 = _softmax32(jnp.einsum('bhqd,bhkd->bhqk', q1, k1) * scale)
    p2 = _softmax32(jnp.einsum('bhqd,bhkd->bhqk', q2, k2) * scale)
    return jnp.einsum('bhqk,bhkd->bhqd', (p1 - lam * p2).astype(v.dtype), v)


def _mla_attend(q, k, v):
    p = _softmax32(jnp.einsum('bhqd,bhkd->bhqk', q, k) * MLA_SCALE).astype(v.dtype)
    return jnp.einsum('bhqk,bhkd->bhqd', p, v)


def _chunk_scan(q, k, v, g, s0, need_out):
    dtype = v.dtype
    b_, h_, s_, dk = q.shape
    dv = v.shape[-1]
    n = s_ // CHUNK
    q, k, v, g = (t.astype(jnp.float32).reshape(b_, h_, n, CHUNK, t.shape[-1]) for t in (q, k, v, g))
    bcum = jnp.cumsum(g, axis=3)
    b_last = bcum[:, :, :, -1:, :]
    d_state = jnp.einsum('bhnck,bhncv->bhnkv', k * jnp.exp(b_last - bcum), v)
    chunk_decay = jnp.exp(b_last[:, :, :, 0, :])

    def step(state, inp):
        ds_n, dec_n = inp
        return state * dec_n[..., None] + ds_n, state

    s_fin, s_start = lax.scan(step, s0, (jnp.moveaxis(d_state, 2, 0), jnp.moveaxis(chunk_decay, 2, 0)))
    if not need_out:
        return None, s_fin
    s_start = jnp.moveaxis(s_start, 0, 2)
    b_ref = bcum[:, :, :, CHUNK // 2 - 1:CHUNK // 2, :]
    att = jnp.einsum('bhnik,bhnjk->bhnij', q * jnp.exp(bcum - b_ref), k * jnp.exp(b_ref - bcum))
    att = jnp.where(jnp.tril(jnp.ones((CHUNK, CHUNK), dtype=bool)), att, 0.0)
    o = jnp.einsum('bhnij,bhnjv->bhniv', att, v) + jnp.einsum('bhnik,bhnkv->bhniv', q * jnp.exp(bcum), s_start)
    return o.reshape(b_, h_, s_, dv).astype(dtype), s_fin


def _bidir_scan(q_c, k_c, v_c, g_c, q_l, k_l, v_l, g_l, need_ctx):
    b_, h_, _, dk = q_l.shape
    dv = v_l.shape[-1]
    o_c_sum, o_l_sum = None, None
    for d in range(2):
        fl = (lambda t: jnp.flip(t, axis=2)) if d == 1 else (lambda t: t)
        s0 = jnp.zeros((b_, h_, dk, dv), jnp.float32)
        o_c, s_c = _chunk_scan(fl(q_c), fl(k_c[d]), fl(v_c), fl(g_c[d]), s0, need_ctx)
        o_l, _ = _chunk_scan(fl(q_l), fl(k_l[d]), fl(v_l), fl(g_l[d]), s_c, True)
        o_l_sum = fl(o_l) if d == 0 else o_l_sum + fl(o_l)
        if need_ctx:
            o_c_sum = fl(o_c) if d == 0 else o_c_sum + fl(o_c)
    return o_c_sum, o_l_sum


def _even_mixer(h_c, h_l, cos, sin, w_in, lam_vecs, lam_init, subln_g, gk_w2, gk_b, gla_g, w_out, need_ctx):
    def project(h, rotate):
        qa, ka, va, qb, kb, vb, gb, lr_f, lr_b = _split_cols(h @ w_in, EV_SIZES)
        qa, ka = _heads(qa, DA_HEADS), _heads(ka, DA_HEADS)
        q1, q2, k1, k2 = qa[..., :DA_DH], qa[..., DA_DH:], ka[..., :DA_DH], ka[..., DA_DH:]
        if rotate:
            q1, q2, k1, k2 = (_rope(t, cos, sin) for t in (q1, q2, k1, k2))
        gk = tuple(_heads(jax.nn.log_sigmoid((lr @ gk_w2[d] + gk_b[d]).astype(jnp.float32)) / GLA_NORMALIZER, GLA_HEADS)
                   for d, lr in enumerate((lr_f, lr_b)))
        gla_k = _heads(kb, GLA_HEADS)
        return (q1, q2, k1, k2, _heads(va, DA_HEADS), _heads(qb, GLA_HEADS) * GLA_DK ** -0.5,
                (gla_k, gla_k), _heads(vb, GLA_HEADS), gk, _heads(gb, GLA_HEADS))

    q1c, q2c, k1c, k2c, vac, qbc, kbc, vbc, gkc, gbc = project(h_c, False)
    q1l, q2l, k1l, k2l, val, qbl, kbl, vbl, gkl, gbl = project(h_l, True)
    lv = lam_vecs.astype(jnp.float32)
    lam = jnp.exp(jnp.sum(lv[0] * lv[1])) - jnp.exp(jnp.sum(lv[2] * lv[3])) + lam_init
    k1_all = jnp.concatenate([k1c, k1l], axis=2)
    k2_all = jnp.concatenate([k2c, k2l], axis=2)
    v_all = jnp.concatenate([vac, val], axis=2)
    o_a_l = _merge_q_blocks(lax.map(lambda qs: _diff_attend(qs[0], qs[1], k1_all, k2_all, v_all, lam),
                                    (_split_q_blocks(q1l), _split_q_blocks(q2l))))
    o_b_c, o_b_l = _bidir_scan(qbc, kbc, vbc, gkc, qbl, kbl, vbl, gkl, need_ctx)

    def merge(o_a, o_b, gate):
        o_a = _rmsnorm(o_a, subln_g) * (1.0 - lam_init)
        o_b = _rmsnorm(o_b, gla_g) * jax.nn.silu(gate)
        return jnp.concatenate([_unheads(o_a), _unheads(o_b)], axis=-1) @ w_out

    y_l = merge(o_a_l, o_b_l, gbl)
    y_c = merge(_diff_attend(q1c, q2c, k1c, k2c, vac, lam), o_b_c, gbc) if need_ctx else None
    return y_c, y_l


def _odd_mixer(h_c, h_l, cos, sin, w_in, q_norm_g, kv_norm_g, w_uq, w_ukv, lb, hg_g, w_out, need_ctx):
    def project(h, rotate):
        cq, ckv, kr, hq, hf_f, hf_b, hi, hgate = _split_cols(h @ w_in, OD_SIZES)
        q = _heads(_rmsnorm(cq, q_norm_g) @ w_uq, MLA_HEADS)
        kv = _heads(_rmsnorm(ckv, kv_norm_g) @ w_ukv, MLA_HEADS)
        q_nope, q_rope = q[..., :MLA_NOPE], q[..., MLA_NOPE:]
        k_nope, v = kv[..., :MLA_NOPE], kv[..., MLA_NOPE:]
        k_rope = kr[:, None]
        if rotate:
            q_rope, k_rope = _rope(q_rope, cos, sin), _rope(k_rope, cos, sin)
        q = jnp.concatenate([q_nope, q_rope], axis=-1)
        k = jnp.concatenate([k_nope, jnp.broadcast_to(k_rope, k_nope.shape[:-1] + (MLA_ROPE,))], axis=-1)
        f = [lb + (1.0 - lb) * jax.nn.sigmoid(t.astype(jnp.float32)) for t in (hf_f, hf_b)]
        hk = tuple(_heads(1.0 - t, HG_HEADS) for t in f)
        hlog = tuple(_heads(jnp.log(t), HG_HEADS) for t in f)
        return q, k, v, _heads(hq, HG_HEADS), hk, _heads(hi, HG_HEADS), hlog, _heads(hgate, HG_HEADS)

    qc, kc, vc, hqc, hkc, hic, hgc, gtc = project(h_c, False)
    ql, kl, vl, hql, hkl, hil, hgl, gtl = project(h_l, True)
    k_all = jnp.concatenate([kc, kl], axis=2)
    v_all = jnp.concatenate([vc, vl], axis=2)
    o_m_l = _merge_q_blocks(lax.map(lambda qb: _mla_attend(qb, k_all, v_all), _split_q_blocks(ql)))
    o_h_c, o_h_l = _bidir_scan(hqc, hkc, hic, hgc, hql, hkl, hil, hgl, need_ctx)

    def merge(o_m, o_h, gate):
        o_h = _rmsnorm(o_h, hg_g) * jax.nn.silu(gate)
        return jnp.concatenate([_unheads(o_m), _unheads(o_h)], axis=-1) @ w_out

    y_l = merge(o_m_l, o_h_l, gtl)
    y_c = merge(_mla_attend(qc, kc, vc), o_h_c, gtc) if need_ctx else None
    return y_c, y_l


def _swiglu(h, w1, w2):
    hg, hu = jnp.split(h @ w1, 2, axis=-1)
    return (jax.nn.silu(hg) * hu) @ w2


def _moe(h, w_router, w1, w2):
    shape = h.shape
    hf = h.reshape(-1, shape[-1])
    logits = (hf @ w_router).astype(jnp.float32)
    top_v, top_i = lax.top_k(logits, TOP_K)
    gates = jax.nn.softmax(top_v, axis=-1)
    comb = jnp.sum(jax.nn.one_hot(top_i, N_EXPERTS, dtype=jnp.float32) * gates[..., None], axis=1)
    out = jnp.zeros_like(hf)
    for e in range(N_EXPERTS):
        out = out + comb[:, e:e + 1].astype(hf.dtype) * _swiglu(hf, w1[e], w2[e])
    return out.reshape(shape)


def _ada(cond, w, b):
    return jnp.split(jax.nn.silu(cond) @ w + b, 6, axis=-1)


def _diff_lambda_init(layer):
    return 0.8 - 0.6 * math.exp(-0.3 * layer)


def setup_inputs(seed: int = 0) -> dict:
    key = jax.random.key(seed)
    ks = iter(jax.random.split(key, 32))

    def nrm(shape, scale):
        return jax.random.normal(next(ks), shape, jnp.float32) * scale

    D = D_MODEL
    return {
        "x": nrm((BATCH, SEQ, D), 1.0),
        "c": nrm((BATCH, D), 1.0),
        "ctx": nrm((BATCH, CTX_LEN, D), 1.0),
        "c_ctx": nrm((D,), 1.0),
        "ada_w": nrm((DEPTH, D, 6 * D), 0.5 * D ** -0.5),
        "ada_b": nrm((DEPTH, 6 * D), 0.02),
        "post_ln_g": 1.0 + nrm((DEPTH, 2, D), 0.02),
        "post_ln_b": nrm((DEPTH, 2, D), 0.02),
        "lb_table": nrm((DEPTH, HG_K), 1.0),
        "ev_w_in": nrm((N_EVEN, D, EV_IN), D ** -0.5),
        "ev_lam": nrm((N_EVEN, 4, DA_DH), 0.1),
        "ev_subln_g": 1.0 + nrm((N_EVEN, DA_DV), 0.02),
        "ev_gk_w2": nrm((N_EVEN, 2, GLA_LR, B_QK), GLA_LR ** -0.5),
        "ev_gk_b": nrm((N_EVEN, 2, B_QK), 0.02),
        "ev_gla_norm_g": 1.0 + nrm((N_EVEN, GLA_DV), 0.02),
        "ev_w_out": nrm((N_EVEN, EV_MIX, D), DN_BETA * EV_MIX ** -0.5),
        "ev_ffn_w1": nrm((N_EVEN, D, 2 * D_FF), D ** -0.5),
        "ev_ffn_w2": nrm((N_EVEN, D_FF, D), DN_BETA * D_FF ** -0.5),
        "od_w_in": nrm((N_ODD, D, OD_IN), D ** -0.5),
        "od_q_norm_g": 1.0 + nrm((N_ODD, MLA_Q_RANK), 0.02),
        "od_kv_norm_g": 1.0 + nrm((N_ODD, MLA_KV_RANK), 0.02),
        "od_w_uq": nrm((N_ODD, MLA_Q_RANK, MLA_HEADS * (MLA_NOPE + MLA_ROPE)), MLA_Q_RANK ** -0.5),
        "od_w_ukv": nrm((N_ODD, MLA_KV_RANK, MLA_HEADS * (MLA_NOPE + MLA_DV)), MLA_KV_RANK ** -0.5),
        "od_hg_norm_g": 1.0 + nrm((N_ODD, HG_DV), 0.02),
        "od_w_out": nrm((N_ODD, OD_MIX, D), DN_BETA * OD_MIX ** -0.5),
        "od_router": nrm((N_ODD, D, N_EXPERTS), D ** -0.5),
        "od_exp_w1": nrm((N_ODD, N_EXPERTS, D, 2 * D_FF), D ** -0.5),
        "od_exp_w2": nrm((N_ODD, N_EXPERTS, D_FF, D), DN_BETA * D_FF ** -0.5),
    }


def reference(x, c, ctx, c_ctx, ada_w, ada_b, post_ln_g, post_ln_b, lb_table,
              ev_w_in, ev_lam, ev_subln_g, ev_gk_w2, ev_gk_b, ev_gla_norm_g, ev_w_out, ev_ffn_w1, ev_ffn_w2,
              od_w_in, od_q_norm_g, od_kv_norm_g, od_w_uq, od_w_ukv, od_hg_norm_g, od_w_out,
              od_router, od_exp_w1, od_exp_w2):
    rows = x.shape[1] // GRID_W
    cos, sin = _axial_rope_table(rows, ROT_DIM)
    lb_soft = jax.nn.softmax(lb_table.astype(jnp.float32), axis=0)
    lower_bounds = jnp.cumsum(lb_soft, axis=0) - lb_soft[0]
    for layer in range(DEPTH):
        last = layer == DEPTH - 1
        j = layer // 2
        m_l = [t[:, None, :] for t in _ada(c, ada_w[layer], ada_b[layer])]
        m_c = _ada(c_ctx, ada_w[layer], ada_b[layer])
        h_l = x * (1.0 + m_l[1]) + m_l[0]
        h_c = ctx * (1.0 + m_c[1]) + m_c[0]
        if layer % 2 == 0:
            y_c, y_l = _even_mixer(h_c, h_l, cos, sin, ev_w_in[j], ev_lam[j], _diff_lambda_init(layer),
                                   ev_subln_g[j], ev_gk_w2[j], ev_gk_b[j], ev_gla_norm_g[j], ev_w_out[j], not last)
        else:
            y_c, y_l = _odd_mixer(h_c, h_l, cos, sin, od_w_in[j], od_q_norm_g[j], od_kv_norm_g[j], od_w_uq[j],
                                  od_w_ukv[j], lower_bounds[layer], od_hg_norm_g[j], od_w_out[j], not last)
        x = _layernorm(DN_ALPHA * x + m_l[2] * y_l, post_ln_g[layer, 0], post_ln_b[layer, 0])
        h_l = x * (1.0 + m_l[4]) + m_l[3]
        if layer % 2 == 0:
            f_l = _swiglu(h_l, ev_ffn_w1[j], ev_ffn_w2[j])
        else:
            f_l = _moe(h_l, od_router[j], od_exp_w1[j], od_exp_w2[j])
        x = _layernorm(DN_ALPHA * x + m_l[5] * f_l, post_ln_g[layer, 1], post_ln_b[layer, 1])
        if not last:
            ctx = _layernorm(DN_ALPHA * ctx + m_c[2] * y_c, post_ln_g[layer, 0], post_ln_b[layer, 0])
            h_c = ctx * (1.0 + m_c[4]) + m_c[3]
            if layer % 2 == 0:
                f_c = _swiglu(h_c, ev_ffn_w1[j], ev_ffn_w2[j])
            else:
                f_c = _moe(h_c, od_router[j], od_exp_w1[j], od_exp_w2[j])
            ctx = _layernorm(DN_ALPHA * ctx + m_c[5] * f_c, post_ln_g[layer, 1], post_ln_b[layer, 1])
    return x
```

```python
import bisect
import math
from contextlib import ExitStack

import numpy as np
import concourse.bass as bass
import concourse.mybir as mybir
from concourse.bass_utils import run_bass_kernel_spmd

F32 = mybir.dt.float32
BF16 = mybir.dt.bfloat16
AF = mybir.ActivationFunctionType
ALU = mybir.AluOpType
AX = mybir.AxisListType

ENGS = ("pe", "act", "dve", "pool", "sp")
SAME_ENG_SYNC = {"pe": False, "act": True, "dve": True, "pool": True, "sp": False}

D = 1024
S = 2048
CTX = 256
T = S + CTX
NT = T // 128
NTC = CTX // 128
DFF = 3584
NFF = DFF // 128
NE = 8
ALPHA = 4 ** 0.25
LN_EPS = 1e-5
RMS_EPS = 1e-6


class Prog:
    def __init__(self, nc):
        self.nc = nc
        self.ops = []
        self.state = {}
        self.cls = {"w": [0, 1, 2, 3], "ld": [4, 5, 6, 7], "st": [8, 9, 10, 11], "m": [12, 13]}
        self.rr = {k: 0 for k in self.cls}
        self.n_dma_sems = 14
        self.last_op = {}
        self.last_dma = {}
        self.cur_barrier = None
        self.mute = False

    def _rec(self, eng, fn, reads, writes, dma_sem=None):
        if self.mute:
            return -1
        oid = len(self.ops)
        deps = set()
        for k in reads:
            st = self.state.get(k)
            if st and st[0] is not None:
                deps.add(st[0])
        for k in writes:
            st = self.state.get(k)
            if st:
                if st[0] is not None:
                    deps.add(st[0])
                deps.update(st[1])
        if self.cur_barrier is not None:
            deps.add(self.cur_barrier)
        self.ops.append(dict(eng=eng, fn=fn, deps=deps, dma=dma_sem))
        if dma_sem is None:
            self.last_op[eng] = oid
        else:
            self.last_dma[dma_sem] = oid
        for k in reads:
            self.state.setdefault(k, [None, []])[1].append(oid)
        for k in writes:
            self.state[k] = [oid, []]
        return oid

    def op(self, eng, fn, reads=(), writes=()):
        return self._rec(eng, fn, tuple(reads), tuple(writes))

    def barrier(self):
        if self.mute:
            return -1
        deps = set(self.last_op.values()) | set(self.last_dma.values())
        if self.cur_barrier is not None:
            deps.add(self.cur_barrier)
        oid = len(self.ops)
        self.ops.append(dict(eng="sp", fn=lambda e: e.nop(), deps=deps, dma=None))
        self.last_op["sp"] = oid
        self.cur_barrier = oid
        return oid

    def dma(self, eng, out, in_, reads=(), writes=(), cls="ld"):
        lst = self.cls[cls]
        sem = lst[self.rr[cls] % len(lst)]
        self.rr[cls] += 1
        return self._rec(eng, lambda e, o=out, i=in_: e.dma_start(out=o, in_=i),
                         tuple(reads), tuple(writes), dma_sem=sem)

    def emit(self, final_keys):
        nc = self.nc
        ops = self.ops
        self.mute = False
        self.barrier()
        self._rec("sp", None, tuple(final_keys), ())
        needed = set()
        for o in ops:
            for d in o["deps"]:
                src = ops[d]
                if src["dma"] is None and src["eng"] == o["eng"] and not SAME_ENG_SYNC[o["eng"]]:
                    continue
                needed.add(d)
        cnt = {e: 0 for e in ENGS}
        dcnt = [0] * self.n_dma_sems
        dma_hist = [[] for _ in range(self.n_dma_sems)]
        for i, o in enumerate(ops):
            if o["dma"] is not None:
                s = o["dma"]
                dcnt[s] += 16
                o["val"] = dcnt[s]
                dma_hist[s].append((i, dcnt[s]))
            elif i in needed:
                cnt[o["eng"]] += 1
                o["val"] = cnt[o["eng"]]
        self.stats = dict(cnt=dict(cnt), dcnt=list(dcnt), nops=len(ops))
        per = {e: [] for e in ENGS}
        seen = {e: {} for e in ENGS}
        for i, o in enumerate(ops):
            e = o["eng"]
            req = {}
            for d in o["deps"]:
                src = ops[d]
                if src["dma"] is not None:
                    s = src["dma"]
                    hist = dma_hist[s]
                    j = bisect.bisect_left(hist, (i, -1)) - 1
                    v = hist[j][1]
                    key = ("d", s)
                else:
                    if src["eng"] == e and not SAME_ENG_SYNC[e]:
                        continue
                    v = src["val"]
                    key = ("e", src["eng"])
                if v > req.get(key, 0):
                    req[key] = v
            waits = []
            for key, v in req.items():
                if seen[e].get(key, 0) >= v:
                    continue
                seen[e][key] = v
                waits.append((key, v))
            per[e].append((i, o, waits))

        with ExitStack() as es:
            esem = {e: es.enter_context(nc.semaphore("s_" + e)) for e in ENGS}
            dsem = [es.enter_context(nc.semaphore("d_%d" % i)) for i in range(self.n_dma_sems)]
            block = es.enter_context(nc.Block())

            def run(engname):
                def body(eng):
                    for i, o, waits in per[engname]:
                        for key, v in waits:
                            sem = dsem[key[1]] if key[0] == "d" else esem[key[1]]
                            eng.wait_ge(sem, v)
                        if o["fn"] is None:
                            continue
                        ins = o["fn"](eng)
                        if o["dma"] is not None:
                            ins.then_inc(dsem[o["dma"]], 16)
                        elif i in needed:
                            ins.then_inc(esem[engname], 1)
                return body

            block.tensor(run("pe"))
            block.scalar(run("act"))
            block.vector(run("dve"))
            block.gpsimd(run("pool"))
            block.sync(run("sp"))


class Ring:
    def __init__(self, tiles, name):
        self.tiles, self.name, self.i = tiles, name, 0

    def next(self):
        j = self.i % len(self.tiles)
        self.i += 1
        return self.tiles[j], (self.name, j)


def tgroups(t0, t1, w=512):
    out = []
    a = t0
    while a < t1:
        b = min(a + w, t1)
        out.append((a, b - a))
        a = b
    return out


def tkeys(name, a, w):
    return [(name, t) for t in range(a // 128, (a + w + 127) // 128)]


def build(debug=None):
    nc = bass.Bass("TRN2", target_bir_lowering=False)
    P = Prog(nc)

    def din(name, shape):
        return nc.dram_tensor(name, list(shape), F32, kind="ExternalInput").ap()

    x_in = din("x", [S, D])
    ctx_in = din("ctx", [CTX, D])
    c2 = din("c2", [2, D])
    ada_w = din("ada_w", [2, D, 6 * D])
    ada_b = din("ada_b", [2, 6 * D])
    ln_g = din("post_ln_g", [2, 2, D])
    ln_b = din("post_ln_b", [2, 2, D])
    lb_table = din("lb_table", [2, 512])
    ev_w_in = din("ev_w_in", [D, 3104])
    ev_w_in_sw = din("ev_w_in_sw", [D, 1024])
    ev_lam = din("ev_lam", [1, 256])
    ev_subln_g = din("ev_subln_g", [1, 128])
    ev_gk_w2 = din("ev_gk_w2", [2, 16, 256])
    ev_gk_b = din("ev_gk_b", [2, 256])
    ev_gla_g = din("ev_gla_norm_g", [1, 128])
    ev_w_out = din("ev_w_out", [D, D])
    ev_w1 = din("ev_ffn_w1", [D, 2 * DFF])
    ev_w2 = din("ev_ffn_w2", [DFF, D])
    od_w_in = din("od_w_in", [D, 3008])
    od_kr_sw = din("od_kr_sw", [D, 64])
    od_qg = din("od_q_norm_g", [256])
    od_kvg = din("od_kv_norm_g", [128])
    od_w_uq = din("od_w_uq", [256, 768])
    od_w_uq_sw = din("od_w_uq_sw", [256, 256])
    od_w_ukv = din("od_w_ukv", [128, 1024])
    od_hg_g = din("od_hg_norm_g", [1, 128])
    od_w_out = din("od_w_out", [D, D])
    od_router = din("od_router", [D, NE])
    od_w1 = din("od_exp_w1", [NE, D, 2 * DFF])
    od_w2 = din("od_exp_w2", [NE, DFF, D])
    ident_d = din("ident", [128, 128])
    cos_d = din("cosT", [128, T])
    sin_d = din("sinT", [128, T])
    mask_d = din("masks", [4, 128, 128])
    out_d = nc.dram_tensor("out", [S, D], F32, kind="ExternalOutput").ap()

    def dscr(name, shape, dt=F32):
        return nc.dram_tensor(name, list(shape), dt).ap()

    mrow_d = dscr("mrow_d", [2, 2, 6 * D])
    pfm = dscr("pfm", [22 * 128, T])
    ptm = dscr("ptm", [T, 1536])
    xa = dscr("xa", [T, D])
    xb = dscr("xb", [T, D])
    xc = dscr("xc", [T, D])
    dbg_keys = []

    def dump(name, src_ap, rk, eng="sp"):
        if debug is None or name not in debug:
            return
        d_ = nc.dram_tensor("dbg_" + name, list(src_ap.shape), F32, kind="ExternalOutput").ap()
        P.dma(eng, d_, src_ap, rk, [("dbg", name)], cls="st")
        dbg_keys.append(("dbg", name))

    def MM(out, lhsT, rhs, start, stop, r, w):
        P.op("pe", lambda e, a=(out, lhsT, rhs, start, stop): e.matmul(a[0], a[1], a[2], start=a[3], stop=a[4]), r, w)

    def TR(out, in_, ident, r, w):
        P.op("pe", lambda e, a=(out, in_, ident): e.transpose(a[0], a[1], a[2]), r, w)

    def ACT(out, in_, func, r, w, bias=None, scale=None, accum_out=None):
        kw = {}
        if bias is not None:
            kw["bias"] = bias
        if scale is not None:
            kw["scale"] = scale
        if accum_out is not None:
            kw["accum_out"] = accum_out
        P.op("act", lambda e, a=(out, in_, func), kw=kw: e.activation(a[0], a[1], a[2], **kw), r, w)

    def TT(out, in0, in1, op, r, w, eng="dve"):
        P.op(eng, lambda e, a=(out, in0, in1, op): e.tensor_tensor(a[0], a[1], a[2], a[3]), r, w)

    def TS(out, in0, s1, s2, op0, op1, r, w, eng="dve"):
        if s2 is None:
            P.op(eng, lambda e, a=(out, in0, s1, op0): e.tensor_scalar(a[0], a[1], a[2], None, a[3]), r, w)
        else:
            P.op(eng, lambda e, a=(out, in0, s1, s2, op0, op1): e.tensor_scalar(a[0], a[1], a[2], a[3], a[4], a[5]), r, w)

    def STT(out, in0, scalar, in1, op0, op1, r, w):
        P.op("dve", lambda e, a=(out, in0, scalar, in1, op0, op1): e.scalar_tensor_tensor(a[0], a[1], a[2], a[3], a[4], a[5]), r, w)

    def CP(out, in_, r, w, eng="dve"):
        if eng == "act":
            P.op("act", lambda e, a=(out, in_): e.copy(a[0], a[1]), r, w)
        else:
            P.op(eng, lambda e, a=(out, in_): e.tensor_copy(a[0], a[1]), r, w)

    def RECIP(out, in_, r, w):
        P.op("dve", lambda e, a=(out, in_): e.reciprocal(a[0], a[1]), r, w)

    def MEMSET(ap, val, r, w, eng="dve"):
        P.op(eng, lambda e, a=(ap, val): e.memset(a[0], a[1]), r, w)

    def col_load(dst, src1d, w, cls="m"):
        P.dma("sp", dst, src1d.rearrange("(p o) -> p o", o=1), (), w, cls=cls)

    with ExitStack() as top:
        uid = [0]

        def sbt(es, name, shape, dt):
            uid[0] += 1
            return es.enter_context(nc.sbuf_tensor("sb%d_%s" % (uid[0], name), list(shape), dt))

        ps = [None] * 8
        for i in range(2, 8):
            ps[i] = top.enter_context(nc.psum_tensor("ps%d" % i, [128, 512], F32))
        PSK = [("ps", i) for i in range(8)]
        ps01 = [ExitStack(), 0]

        def alloc_ps01():
            ps01[1] += 1
            for i in range(2):
                ps[i] = ps01[0].enter_context(nc.psum_tensor("ps%d_%d" % (i, ps01[1]), [128, 512], F32))

        def free_ps01():
            ps01[0].close()
            ps01[0] = ExitStack()
            ps[0] = ps[1] = None

        alloc_ps01()

        idf = sbt(top, "idf", [128, 128], F32)
        idb = sbt(top, "idb", [128, 128], BF16)
        masks = sbt(top, "masks", [128, 4, 128], F32)
        ones_f = sbt(top, "ones_f", [128, 128], F32)
        eps_ln = sbt(top, "eps_ln", [128, 1], F32)
        eps_rms = sbt(top, "eps_rms", [128, 1], F32)
        P.dma("sp", idf[:], ident_d, (), ["idf"], cls="m")
        P.dma("sp", masks[:], mask_d.rearrange("m p c -> p m c"), (), ["masks"], cls="m")
        CP(idb[:], idf[:], ["idf"], ["idb"])
        MEMSET(ones_f[:], 1.0, (), ["ones_f"])
        MEMSET(eps_ln[:], LN_EPS, (), ["eps"])
        MEMSET(eps_rms[:], RMS_EPS, (), ["eps"])

        with ExitStack() as es:
            scin = sbt(es, "scin", [128, 2, 8], F32)
            sce = sbt(es, "sce", [128, 2, 8], F32)
            scT = sbt(es, "scT", [128, 8, 2], BF16)
            wr = Ring([sbt(es, "adaw%d" % i, [128, 8, 512], BF16) for i in range(3)], "adaw")
            mrow = sbt(es, "mrow", [2, 6 * D], F32)
            brow = sbt(es, "brow", [2, 6 * D], F32)
            for r in range(2):
                P.dma("sp", scin[:, r, :], c2[r].rearrange("(p k) -> p k", k=8), (), ["scin"], cls="m")
            ACT(sce[:], scin[:], AF.Exp, ["scin"], ["sce"], scale=-1.0)
            TS(sce[:], sce[:], 1.0, None, ALU.add, None, ["sce"], ["sce"])
            RECIP(sce[:], sce[:], ["sce"], ["sce"])
            TT(scT[:].rearrange("p k r -> p r k"), scin[:], sce[:], ALU.mult, ["scin", "sce"], ["scT"])
            bi = 0
            for L in range(2):
                for r in range(2):
                    P.dma("sp", brow[r:r + 1, :], ada_b[L:L + 1, :], ["mrow"], ["brow"], cls="m")
                for n in range(12):
                    slot, sk = wr.next()
                    P.dma("pool", slot[:], ada_w[L][:, n * 512:(n + 1) * 512].rearrange("(p k) n -> p k n", k=8), (), [sk], cls="w")
                    b = bi % 2
                    bi += 1
                    for k in range(8):
                        MM(ps[b][0:2, :], scT[:, k, :], slot[:, k, :], k == 0, k == 7, ["scT", sk], [PSK[b]])
                    TT(mrow[:, n * 512:(n + 1) * 512], ps[b][0:2, :], brow[:, n * 512:(n + 1) * 512], ALU.add,
                       [PSK[b], "brow"], ["mrow"])
                for i in (1, 4):
                    TS(mrow[:, i * D:(i + 1) * D], mrow[:, i * D:(i + 1) * D], 1.0, None, ALU.add, None, ["mrow"], ["mrow"])
                P.dma("sp", mrow_d[L], mrow[:], ["mrow"], [("mrow_d", L)], cls="st")
                if L == 0:
                    dump("mrow", mrow[:], ["mrow"])
                    dump("scin", scin[:].rearrange("p r k -> p (r k)"), ["scin"])
                    dump("sce", sce[:].rearrange("p r k -> p (r k)"), ["sce"])

        def bload(tile, L, r, i):
            P.dma("sp", tile[:], mrow_d[L, r, i * D:(i + 1) * D].partition_broadcast(128), [("mrow_d", L)], [tile.name], cls="m")

        def bload_vec(tile, src1d):
            P.dma("sp", tile[:], src1d.partition_broadcast(128), (), [tile.name], cls="m")

        def mod_transpose(src, skey, mA, mB, hT, t, work, pbanks, logits=None):
            h, hk = work.next()
            TT(h[:], src, mA[:], ALU.mult, [skey, mA.name], [hk])
            TT(h[:], h[:], mB[:], ALU.add, [hk, mB.name], [hk])
            if t == 2:
                dump("h2", h[:], [hk])
                dump("mA", mA[:], [mA.name])
            ba, bb = pbanks
            for k in range(8):
                b = ba if k < 4 else bb
                TR(ps[b][:, (k % 4) * 128:(k % 4 + 1) * 128], h[:, k * 128:(k + 1) * 128], idf[:], [hk, "idf"], [PSK[b]])
            if logits is None:
                for j, b in enumerate((ba, bb)):
                    CP(hT[:, j * 4:(j + 1) * 4, t * 128:(t + 1) * 128], ps[b][:].rearrange("p (k n) -> p k n", k=4),
                       [PSK[b]], [("hT", t)], eng="act")
            if logits is not None:
                h2T, wrt, comb, lb = logits
                sstop(10)
                for j, b in enumerate((ba, bb)):
                    CP(h2T[:, j * 4:(j + 1) * 4, :], ps[b][:].rearrange("p (k n) -> p k n", k=4), [PSK[b]], ["h2T"])
                CP(hT[:, :, t * 128:(t + 1) * 128], h2T[:], ["h2T"], [("hT", t)], eng="act")
                sstop(11)
                for k in range(8):
                    MM(ps[lb][:, 0:8], h2T[:, k, :], wrt[:, k, :], k == 0, k == 7, ["h2T", "wrt"], [PSK[lb]])
                sstop(12)
                route(ps[lb][:, 0:8], PSK[lb], comb, t)

        def route(lg_ps, lgk, comb, t):
            lg, m8, msk, ex, ssum = comb["lg"], comb["m8"], comb["msk"], comb["ex"], comb["ssum"]
            CP(lg[:], lg_ps, [lgk], ["r_lg"])
            sstop(13)
            P.op("dve", lambda e: e.max(out=m8[:], in_=lg[:]), ["r_lg"], ["r_m8"])
            sstop(14)
            TS(msk[:], lg[:], m8[:, 1:2], None, ALU.is_ge, None, ["r_lg", "r_m8"], ["r_msk"])
            sstop(15)
            TS(ex[:], lg[:], m8[:, 0:1], None, ALU.subtract, None, ["r_lg", "r_m8"], ["r_ex"])
            ACT(ex[:], ex[:], AF.Exp, ["r_ex"], ["r_ex"])
            TT(ex[:], ex[:], msk[:], ALU.mult, ["r_ex", "r_msk"], ["r_ex"])
            P.op("dve", lambda e: e.reduce_sum(ssum[:], ex[:], AX.X), ["r_ex"], ["r_ss"])
            RECIP(ssum[:], ssum[:], ["r_ss"], ["r_ss"])
            TS(comb["comb"][:, t - NTC, :], ex[:], ssum[:, 0:1], None, ALU.mult, None, ["r_ex", "r_ss"], [("comb", t)])

        def layernorm_tile(tl, tk, gT, bT, dst, dk, small, sk):
            st, mv, rstd = small
            tv = tl[:].rearrange("p (c f) -> p c f", f=512)
            for c in range(2):
                P.op("dve", lambda e, c=c: e.bn_stats(st[:, c, :], tv[:, c, :]), [tk], [sk])
            P.op("dve", lambda e: e.bn_aggr(mv[:], st[:]), [sk], [sk])
            ACT(rstd[:], mv[:, 1:2], AF.Ln, [sk], [sk], bias=eps_ln[:], scale=1.0)
            ACT(rstd[:], rstd[:], AF.Exp, [sk], [sk], scale=-0.5)
            TS(tl[:], tl[:], mv[:, 0:1], rstd[:, 0:1], ALU.subtract, ALU.mult, [tk, sk], [tk])
            TT(tl[:], tl[:], gT[:], ALU.mult, [tk, gT.name], [tk])
            TT(dst, tl[:], bT[:], ALU.add, [tk, bT.name], [dk])

        def stream_w(ring, src_ap, eng="pool"):
            slot, sk = ring.next()
            P.dma(eng, slot[:] if src_ap.shape[-1] == slot.shape[-1] else slot[:, :, 0:src_ap.shape[-1]], src_ap, (), [sk], cls="w")
            return slot, sk

        def proj_fm(hT, ntok, slot, sk, c0, ncols, rows0, stg, banks, bctr):
            for (a, w) in tgroups(0, ntok):
                b = banks[bctr[0] % len(banks)]
                bctr[0] += 1
                for k in range(8):
                    MM(ps[b][0:ncols, 0:w], slot[:, k, c0:c0 + ncols], hT[:, k, a:a + w], k == 0, k == 7,
                       [sk] + tkeys("hT", a, w), [PSK[b]])
                s, stk = stg.next()
                CP(s[0:ncols, 0:w], ps[b][0:ncols, 0:w], [PSK[b]], [stk], eng="act")
                P.dma("sp", pfm[rows0:rows0 + ncols, a:a + w], s[0:ncols, 0:w], [stk], tkeys(("pfm", rows0 // 128), a, w), cls="st")

        def proj_tm(hT, tiles, slot, sk, ncols, col0, stg, banks, bctr):
            for t in tiles:
                b = banks[bctr[0] % len(banks)]
                bctr[0] += 1
                for k in range(8):
                    MM(ps[b][:, 0:ncols], hT[:, k, t * 128:(t + 1) * 128], slot[:, k, 0:ncols], k == 0, k == 7,
                       [sk, ("hT", t)], [PSK[b]])
                s, stk = stg.next()
                CP(s[:, 0:ncols], ps[b][:, 0:ncols], [PSK[b]], [stk], eng="act")
                P.dma("sp", ptm[t * 128:(t + 1) * 128, col0:col0 + ncols], s[:, 0:ncols], [stk], [("ptm", col0 // 512, t)], cls="st")

        def wsrc(w_ap, c0, n):
            return w_ap[:, c0:c0 + n].rearrange("(k p) n -> p k n", p=128)

        def attention(es, maps, Vaug, qranges, scale, finish, sbanks, abanks, tag=""):
            ptr = Ring([sbt(es, "pT%s_%d" % (tag, i), [128, 512], BF16) for i in range(3)], "pT" + tag)
            sctr = 0
            actr = 0
            for (q0, qw, ktiles) in qranges:
                nsub = qw // 128
                for mi, parts in enumerate(maps):
                    accs = abanks[actr % len(abanks)]
                    actr += 1
                    def score(idx_):
                        kt_ = ktiles[idx_]
                        sbk = sbanks[(sctr + idx_) % len(sbanks)]
                        for pi, (kfn, qfn, rk) in enumerate(parts):
                            MM(ps[sbk][:, 0:qw], kfn(kt_), qfn(q0, qw), pi == 0, pi == len(parts) - 1, rk, [PSK[sbk]])
                        return sbk
                    sb_cur = score(0)
                    for idx, kt in enumerate(ktiles):
                        sb_next = score(idx + 1) if idx + 1 < len(ktiles) else None
                        sb_ = sb_cur
                        pt, ptk = ptr.next()
                        ACT(pt[:, 0:qw], ps[sb_][:, 0:qw], AF.Exp, [PSK[sb_]], [ptk], scale=scale)
                        for s in range(nsub):
                            bk = accs[s // 2]
                            c0 = (s % 2) * 256
                            MM(ps[bk][:, c0:c0 + 130], pt[:, s * 128:(s + 1) * 128], Vaug(kt), idx == 0 and s % 2 == 0,
                               idx == len(ktiles) - 1, [ptk, ("Vaug", kt)], [PSK[bk]])
                        sb_cur = sb_next
                    sctr += len(ktiles)
                    for s in range(nsub):
                        bk = accs[s // 2]
                        c0 = (s % 2) * 256
                        finish(mi, s, q0, ps[bk][:, c0:c0 + 130], PSK[bk])

        def scan_phase(es, cfg, mixT):
            nch, hpc, dk, C, gsc = cfg["nch"], cfg["hpc"], cfg["dk"], cfg["C"], cfg["gsc"]
            nchunks = T // C
            cpt = 128 // C
            out_t0 = cfg["out_t0"]
            NG = 2
            ncg = nch // NG
            big = lambda n: sbt(es, n, [128, T], F32)
            ng, Cs, Ce, ee, qf, kf = big("sc_ng"), big("sc_Cs"), big("sc_Ce"), big("sc_ee"), big("sc_qf"), big("sc_kf")
            onesT = sbt(es, "sc_ones", [128, T], BF16)
            MEMSET(onesT[:], 1.0, (), ["sc_ones"])
            qh = [sbt(es, "sc_qh%d" % i, [128, T], BF16) for i in range(ncg)]
            kh = [sbt(es, "sc_kh%d" % i, [128, T], BF16) for i in range(ncg)]
            qm = None
            if hpc == 2:
                qm = [[sbt(es, "sc_qm%d_%d" % (i, j), [128, T], BF16) for j in range(2)] for i in range(ncg)]
                for i in range(ncg):
                    for j in range(2):
                        MEMSET(qm[i][j][:], 0.0, (), [("sc_qh", i)])
            c_out0 = out_t0 * cpt
            vbt = sbt(es, "sc_v", [128, nchunks, 256], BF16)
            oacc = sbt(es, "sc_oacc", [128, nchunks - c_out0, 256], F32)
            if C < 128:
                MEMSET(vbt[:], 0.0, (), [("sc_v", c) for c in range(nchunks)])
            cols = [[sbt(es, "sc_c%d_%d" % (i, j), [128, nchunks], F32) for j in range(6)] for i in range(ncg)]
            Sst = [sbt(es, "sc_S%d" % i, [128, 128], F32) for i in range(ncg)]
            Sef = [[sbt(es, "sc_Se%d_%d" % (i, j), [128, 128], BF16) for j in range(2)] for i in range(ncg)]
            tmpS = sbt(es, "sc_tmpS", [128, 128], F32)
            khtm = Ring([sbt(es, "sc_khtm%d" % i, [128, 256], BF16) for i in range(2)], "khtm")
            Am = Ring([sbt(es, "sc_Am%d" % i, [128, 2 * C], BF16) for i in range(2)], "Am")
            ldr = Ring([sbt(es, "sc_ld%d" % i, [128, 256], F32) for i in range(3)], "scld")
            for tl_ in khtm.tiles:
                MEMSET(tl_[:], 0.0, (), [("khtm", 0), ("khtm", 1)])
            for tl_ in Am.tiles:
                MEMSET(tl_[:], 0.0, (), [("Am", 0), ("Am", 1)])
            gT = sbt(es, "sc_gT", [128, 128], F32)
            bload_vec(gT, cfg["norm_g"])
            wk = Ring([sbt(es, "sc_mw%d" % i, [128, 256], F32) for i in range(2)], "scmw")
            gk = Ring([sbt(es, "sc_mg%d" % i, [128, 256], F32) for i in range(2)], "scmg")
            ssq = sbt(es, "sc_ssq", [128, 2], F32)
            for hg in range(NG):
                vc0 = cfg["vcol"] + hg * 256
                for c in range(nchunks):
                    l, lk = ldr.next()
                    P.dma("sp", l[0:C, :], ptm[c * C:(c + 1) * C, vc0:vc0 + 256], [("ptm", cfg["vcol"] // 512, (c * C) // 128)], [lk])
                    CP(vbt[0:C, c, :], l[0:C, :], [lk], [("sc_v", c)], eng="pool")
                for d in range(2):
                    maskF = masks[:, (0 if C == 128 else 2) + d, 0:C]
                    for lc in range(ncg):
                        cc = hg * ncg + lc
                        cfg["load_q"](cc, qf)
                        cfg["make_ng_k"](cc, d, ng, kf, ee)
                        sstop(-3)
                        P.op("dve", lambda e: e.tensor_tensor_scan(Cs[:], onesT[:], ng[:], 0.0, ALU.mult, ALU.add),
                             ["sc_ones", "sc_ng"], ["sc_Cs"])
                        TT(Ce[:], Cs[:], ng[:], ALU.subtract, ["sc_Cs", "sc_ng"], ["sc_Ce"])
                        sstop(-2)
                        Cs3 = Cs[:].rearrange("p (n c) -> p n c", c=C)
                        Ce3 = Ce[:].rearrange("p (n c) -> p n c", c=C)
                        cA, cZ, cR, c1, c2_, c3 = cols[lc]
                        ck = ("sc_cols", lc)
                        CP(cA[:], Cs3[:, :, C - 1], ["sc_Cs"], [ck])
                        MEMSET(cZ[:, 0:1], 0.0, (), [ck])
                        CP(cZ[:, 1:nchunks], cA[:, 0:nchunks - 1], [ck], [ck])
                        if d == 0:
                            CP(cR[:], Cs3[:, :, C // 2 - 1], ["sc_Cs"], [ck])
                            base3 = Cs3
                        else:
                            CP(cR[:], Ce3[:, :, C // 2], ["sc_Ce"], [ck])
                            base3 = Ce3
                        TT(c1[:], cR[:], cZ[:], ALU.subtract, [ck], [ck])
                        TT(c2_[:], cA[:], cZ[:], ALU.subtract, [ck], [ck])
                        TT(c3[:], cA[:], cR[:], ALU.subtract, [ck], [ck])
                        for c_ in (c1, c2_, c3):
                            ACT(c_[:], c_[:], AF.Exp, [ck], [ck], scale=-gsc)
                        sstop(-1)
                        rel = ee
                        TT(rel[:].rearrange("p (n c) -> p n c", c=C), base3, cR[:].unsqueeze(2).to_broadcast([128, nchunks, C]),
                           ALU.subtract, ["sc_Cs", "sc_Ce", ck], ["sc_ee"])
                        sq = -gsc if d == 0 else gsc
                        ACT(Ce[:], rel[:], AF.Exp, ["sc_ee"], ["sc_Ce"], scale=sq)
                        TT(qh[lc][:], qf[:], Ce[:], ALU.mult, ["sc_qf", "sc_Ce"], [("sc_qh", lc)])
                        if hpc == 2:
                            for j in range(2):
                                CP(qm[lc][j][j * 64:(j + 1) * 64, :], qh[lc][j * 64:(j + 1) * 64, :], [("sc_qh", lc)], [("sc_qh", lc)], eng="pool")
                        ACT(Ce[:], rel[:], AF.Exp, ["sc_ee"], ["sc_Ce"], scale=-sq)
                        TT(kh[lc][:], kf[:], Ce[:], ALU.mult, ["sc_kf", "sc_Ce"], [("sc_kh", lc)])
                    sstop(0)
                    if d == 0:
                        order = list(range(nchunks))
                    else:
                        nctx = CTX // C
                        order = list(range(nctx - 1, -1, -1)) + list(range(nchunks - 1, nctx - 1, -1))
                    for lc in range(ncg):
                        MEMSET(Sst[lc][:], 0.0, (), [("sc_S", lc)])
                        MEMSET(Sef[lc][0][:], 0.0, (), [("sc_Se", lc, 0)])
                    bK, bA, bO, bD = [0, 1], [2, 3], [4, 5], [6, 7]
                    eM = [cols[lc][3 if d == 0 else 5] for lc in range(ncg)]
                    eL = [cols[lc][4] for lc in range(ncg)]
                    eLM = [cols[lc][5 if d == 0 else 3] for lc in range(ncg)]
                    sstop(2)
                    def qsel(lc, hl, tok0_):
                        return qm[lc][hl % 2][:, tok0_:tok0_ + C] if hpc == 2 else qh[lc][:, tok0_:tok0_ + C]

                    def front(oi_):
                        tok0_ = order[oi_] * C
                        kb = bK[oi_ % 2]
                        for lc in range(ncg):
                            MM(ps[kb][0:C, lc * 128:(lc + 1) * 128], kh[lc][:, tok0_:tok0_ + C], idb[:], lc == 0, True,
                               [("sc_kh", lc), "idb"], [PSK[kb]])
                        ktm_, ktk_ = khtm.next()
                        CP(ktm_[0:C, 0:ncg * 128], ps[kb][0:C, 0:ncg * 128], [PSK[kb]], [ktk_], eng="act")
                        ab = bA[oi_ % 2]
                        for hl in range(2):
                            lc = hl // hpc
                            MM(ps[ab][0:C, hl * C:(hl + 1) * C], kh[lc][:, tok0_:tok0_ + C],
                               qsel(lc, hl, tok0_), hl == 0, True, [("sc_kh", lc), ("sc_qh", lc)], [PSK[ab]])
                        am_, amk_ = Am.next()
                        TT(am_[0:C, :].rearrange("p (h c) -> p h c", c=C),
                           ps[ab][0:C, 0:2 * C].rearrange("p (h c) -> p h c", c=C),
                           maskF[0:C, None, :].to_broadcast([C, 2, C]), ALU.mult, [PSK[ab], "masks"], [amk_])
                        return ktm_, ktk_, am_, amk_

                    cur = front(0)
                    for oi, c in enumerate(order):
                        pb = 0
                        t = (c * C) // 128
                        tok0 = c * C
                        r2 = oi % 2
                        nxt = front(oi + 1) if oi + 1 < len(order) else None
                        ktm, ktk, am, amk = cur
                        cur = nxt
                        for _once in (0,):
                            if oi == len(order) - 1:
                                break
                            sstop(5)
                            db = bD[r2]
                            for hl in range(2):
                                lc, r0 = hl // hpc, (hl % hpc) * dk
                                MM(ps[db][:, hl * 128:(hl + 1) * 128], ktm[:, lc * 128:(lc + 1) * 128],
                                   vbt[:, c, hl * 128:(hl + 1) * 128], hl == 0, True, [ktk, ("sc_v", c)], [PSK[db]])
                            cn = order[oi + 1]
                            for lc in range(ncg):
                                for j in range(hpc):
                                    hl = lc * hpc + j
                                    r0 = j * dk
                                    TS(tmpS[r0:r0 + dk, :], ps[db][r0:r0 + dk, hl * 128:(hl + 1) * 128], eLM[lc][r0:r0 + dk, c:c + 1], None, ALU.mult, None,
                                       [PSK[db], ("sc_cols", lc)], ["sc_tmpS"])
                                STT(Sst[lc][:], Sst[lc][:], eL[lc][:, c:c + 1], tmpS[:], ALU.mult, ALU.add,
                                    [("sc_S", lc), "sc_tmpS", ("sc_cols", lc)], [("sc_S", lc)])
                                TS(Sef[lc][(oi + 1) % 2][:], Sst[lc][:], eM[lc][:, cn:cn + 1], None, ALU.mult, None,
                                   [("sc_S", lc), ("sc_cols", lc)], [("sc_Se", lc, (oi + 1) % 2)])
                        sstop(4)
                        need_out = t >= out_t0
                        ob = bO[r2]
                        if need_out:
                            for hl in range(2):
                                lc, r0 = hl // hpc, (hl % hpc) * dk
                                MM(ps[ob][pb:pb + C, hl * 128:(hl + 1) * 128], am[:, hl * C:(hl + 1) * C],
                                   vbt[:, c, hl * 128:(hl + 1) * 128], hl == 0, False, [amk, ("sc_v", c)], [PSK[ob]])
                                MM(ps[ob][pb:pb + C, hl * 128:(hl + 1) * 128], qsel(lc, hl, tok0),
                                   Sef[lc][oi % 2][:, :], False, True, [("sc_qh", lc), ("sc_Se", lc, oi % 2)], [PSK[ob]])
                            okey = ("sc_oacc", c)
                            if d == 0:
                                CP(oacc[0:C, c - c_out0, :], ps[ob][0:C, 0:256], [PSK[ob]], [okey], eng="act")
                            else:
                                TT(oacc[0:C, c - c_out0, :], oacc[0:C, c - c_out0, :], ps[ob][0:C, 0:256], ALU.add,
                                   [PSK[ob], okey], [okey])
                sstop(6)
                gc0 = cfg["gcol"] + hg * 256
                for c in range(c_out0, nchunks):
                    t = (c * C) // 128
                    o = oacc[0:C, c - c_out0, :]
                    okeys = [("sc_oacc", c)]
                    w_, wk_ = wk.next()
                    g_, gk_ = gk.next()
                    P.dma("sp", g_[0:C, :], ptm[c * C:(c + 1) * C, gc0:gc0 + 256], [("ptm", cfg["gcol"] // 512, t)], [gk_])
                    TT(w_[0:C, :], o, o, ALU.mult, okeys, [wk_])
                    P.op("dve", lambda e, w_=w_: e.reduce_sum(ssq[0:C, :], w_[0:C, :].rearrange("p (h v) -> p h v", v=128), AX.X), [wk_], ["sc_ssq"])
                    ACT(ssq[0:C, :], ssq[0:C, :], AF.Ln, ["sc_ssq"], ["sc_ssq"], bias=eps_rms[0:C, :], scale=1.0 / 128)
                    ACT(ssq[0:C, :], ssq[0:C, :], AF.Exp, ["sc_ssq"], ["sc_ssq"], scale=-0.5)
                    TT(w_[0:C, :].rearrange("p (h v) -> p h v", v=128), o.rearrange("p (h v) -> p h v", v=128),
                       ssq[0:C, :].unsqueeze(2).to_broadcast([C, 2, 128]), ALU.mult, okeys + ["sc_ssq"], [wk_])
                    TT(w_[0:C, :].rearrange("p (h v) -> p h v", v=128), w_[0:C, :].rearrange("p (h v) -> p h v", v=128),
                       gT[0:C, None, :].to_broadcast([C, 2, 128]), ALU.mult, [wk_, gT.name], [wk_])
                    e_, ek_ = ldr.next()
                    ACT(e_[0:C, :], g_[0:C, :], AF.Exp, [gk_], [ek_], scale=-1.0)
                    ACT(e_[0:C, :], e_[0:C, :], AF.Ln, [ek_], [ek_], bias=ones_f[0:C, 0:1], scale=1.0)
                    ACT(e_[0:C, :], e_[0:C, :], AF.Exp, [ek_], [ek_], scale=-1.0)
                    TT(g_[0:C, :], g_[0:C, :], e_[0:C, :], ALU.mult, [gk_, ek_], [gk_])
                    TT(w_[0:C, :], w_[0:C, :], g_[0:C, :], ALU.mult, [wk_, gk_], [wk_])
                    b = 6 + (c % 2)
                    for hl in range(2):
                        TR(ps[b][:, hl * C:(hl + 1) * C], w_[0:C, hl * 128:(hl + 1) * 128], idf[0:C, 0:C], [wk_, "idf"], [PSK[b]])
                    CP(mixT[:, 4 + 2 * hg:6 + 2 * hg, c * C:(c + 1) * C], ps[b][:, 0:2 * C].rearrange("p (h n) -> p h n", h=2),
                       [PSK[b]], [("mixT", 1, t)], eng="act")

        def outproj_phase(es, L, mixT, hT, w_out, xsrc, xdst, t0, route_cfg=None):
            wo = sbt(es, "wo", [128, 8, D], BF16)
            for hlf in range(2):
                P.dma("pool", wo[:, :, hlf * 512:(hlf + 1) * 512], wsrc(w_out, hlf * 512, 512), (), [("wo", hlf)], cls="w")
            names = ["m2"]
            mb = {}
            for r in range(2):
                if r == 1 and t0 >= NTC:
                    continue
                for i, nm in zip((2,), names):
                    tl = sbt(es, "ob_%s_%d" % (nm, r), [128, D], F32)
                    bload(tl, L, 0 if r == 0 else 1, i)
                    mb[(nm, r)] = tl
            gT = sbt(es, "ob_g", [128, D], F32)
            bT = sbt(es, "ob_b", [128, D], F32)
            bload_vec(gT, ln_g[L, 0])
            bload_vec(bT, ln_b[L, 0])
            xr = Ring([sbt(es, "ob_x%d" % i, [128, D], F32) for i in range(2)], "obx")
            yr = Ring([sbt(es, "ob_y%d" % i, [128, D], F32) for i in range(2)], "oby")
            x1r = Ring([sbt(es, "ob_x1%d" % i, [128, D], F32) for i in range(2)], "obx1")
            st = sbt(es, "ob_st", [128, 2, 6], F32)
            mv = sbt(es, "ob_mv", [128, 2], F32)
            rstd = sbt(es, "ob_rstd", [128, 1], F32)
            for t in range(t0, NT):
                r = 1 if t < NTC else 0
                xt, xk = xr.next()
                P.dma("sp", xt[:], xsrc(t), [("xsrc", L, t)], [xk])
                y, yk = yr.next()
                for hlf in range(2):
                    b = 2 * (t % 2) + hlf
                    for k in range(8):
                        MM(ps[b][:, :], mixT[:, k, t * 128:(t + 1) * 128], wo[:, k, hlf * 512:(hlf + 1) * 512], k == 0, k == 7,
                           [("mixT", k // 4, t), ("wo", hlf)], [PSK[b]])
                    TT(y[:, hlf * 512:(hlf + 1) * 512], ps[b][:, :], mb[("m2", r)][:, hlf * 512:(hlf + 1) * 512], ALU.mult,
                       [PSK[b], mb[("m2", r)].name], [yk])
                STT(y[:], xt[:], ALU_ALPHA, y[:], ALU.mult, ALU.add, [xk, yk], [yk])
                x1, x1k = x1r.next()
                layernorm_tile(y, yk, gT, bT, x1[:], x1k, (st, mv, rstd), "ob_small")
                P.dma("sp", xdst[t * 128:(t + 1) * 128, :], x1[:], [x1k], [("xdst", L, 0, t)], cls="st")

        ALU_ALPHA = float(ALPHA)

        class View:
            def __init__(self, ap, name):
                self.ap, self.name = ap, name

            def __getitem__(self, k):
                return self.ap[k]

        def ffn_phase(es, L, hT, experts, groups, xsrc, xdst, comb=None, final=False):
            maxw = max(w for _, w in groups)
            hidraw = sbt(es, "ff_hid", [128, NFF * maxw], BF16)
            hid = hidraw[:].rearrange("p (f w) -> p f w", w=maxw)
            hf32 = hidraw[:].bitcast(F32)
            ev = [View(hf32[:, i * D:(i + 1) * D], "ff_ev%d" % i) for i in range(10)]
            acc = sbt(es, "ff_acc", [128, 8, maxw], F32)
            wring = Ring([sbt(es, "ff_w_%d" % i, [128, 8192], BF16) for i in range(3)], "ffw")
            sil = Ring([sbt(es, "ff_sil%d" % i, [128, 512], F32) for i in range(2)], "ffsil")
            tmp = Ring([sbt(es, "ff_tmp%d" % i, [128, 512], F32) for i in range(2)], "fftmp")
            st = sbt(es, "ff_st", [128, 2, 6], F32)
            mv = sbt(es, "ff_mv", [128, 2], F32)
            rstd = sbt(es, "ff_rstd", [128, 1], F32)
            cbt = None
            if comb is not None:
                cbt = sbt(es, "ff_cb", [128, maxw], F32)
            hb = [0, 1, 2, 3]
            hbc = 0
            for gi, (g0, gw) in enumerate(groups):
                subs = tgroups(g0, g0 + gw)
                for ei, (w1, w2) in enumerate(experts):
                    for f4 in range(NFF // 4):
                        if comb is not None and f4 == 0:
                            P.dma("sp", cbt[:, 0:gw], comb["comb_d"][ei, g0 - CTX:g0 - CTX + gw].partition_broadcast(128),
                                  ["comb_d"], ["ff_cb"], cls="m")
                        slot_, sk = wring.next()
                        slot = slot_[:].rearrange("p (k n) -> p k n", k=8)
                        P.dma("pool", slot[:, :, 0:512], wsrc(w1, f4 * 512, 512), (), [sk], cls="w")
                        P.dma("pool", slot[:, :, 512:1024], wsrc(w1, DFF + f4 * 512, 512), (), [sk], cls="w")
                        for fj in range(4):
                            f = f4 * 4 + fj
                            for (a, w) in subs:
                                bg = hb[hbc % 4]
                                bu = hb[(hbc + 1) % 4]
                                hbc += 2
                                for k in range(8):
                                    MM(ps[bg][:, 0:w], slot[:, k, fj * 128:(fj + 1) * 128], hT[:, k, a:a + w], k == 0, k == 7,
                                       [sk] + tkeys("hT", a, w), [PSK[bg]])
                                for k in range(8):
                                    MM(ps[bu][:, 0:w], slot[:, k, 512 + fj * 128:512 + (fj + 1) * 128], hT[:, k, a:a + w], k == 0, k == 7,
                                       [sk] + tkeys("hT", a, w), [PSK[bu]])
                                s_, sk_ = sil.next()
                                ACT(s_[:, 0:w], ps[bg][:, 0:w], AF.Silu, [PSK[bg]], [sk_])
                                TT(hid[:, f, a - g0:a - g0 + w], s_[:, 0:w], ps[bu][:, 0:w], ALU.mult, [sk_, PSK[bu]], [("ff_hid", f)])
                    HF = NFF // 2
                    for c4 in range(2):
                        for fh in range(2):
                            slot_, sk = wring.next()
                            slot = slot_[:, 0:HF * 512].rearrange("p (f n) -> p f n", n=512)
                            P.dma("pool", slot, w2[fh * HF * 128:(fh + 1) * HF * 128, c4 * 512:(c4 + 1) * 512].rearrange("(f p) n -> p f n", p=128),
                                  (), [sk], cls="w")
                            first = ei == 0 and fh == 0
                            for cj in range(4):
                                c = c4 * 4 + cj
                                for (a, w) in subs:
                                    b = 4 + (hbc % 2)
                                    hbc += 1
                                    for fi in range(HF):
                                        f = fh * HF + fi
                                        MM(ps[b][:, 0:w], slot[:, fi, cj * 128:(cj + 1) * 128], hid[:, f, a - g0:a - g0 + w], fi == 0, fi == HF - 1,
                                           [sk, ("ff_hid", f)], [PSK[b]])
                                    av = acc[:, c, a - g0:a - g0 + w]
                                    ak = ("ff_acc", c, a)
                                    if comb is None:
                                        if first:
                                            CP(av, ps[b][:, 0:w], [PSK[b]], [ak], eng="act")
                                        else:
                                            TT(av, av, ps[b][:, 0:w], ALU.add, [ak, PSK[b]], [ak])
                                    elif first:
                                        TT(av, ps[b][:, 0:w], cbt[:, a - g0:a - g0 + w], ALU.mult, [PSK[b], "ff_cb"], [ak])
                                    else:
                                        t_, tk_ = tmp.next()
                                        TT(t_[:, 0:w], ps[b][:, 0:w], cbt[:, a - g0:a - g0 + w], ALU.mult, [PSK[b], "ff_cb"], [tk_])
                                        TT(av, av, t_[:, 0:w], ALU.add, [ak, tk_], [ak])
                P.barrier()
                tiles = list(range(g0 // 128, (g0 + gw) // 128))
                mb = {}
                for r in sorted(set(1 if t < NTC else 0 for t in tiles)):
                    mb[r] = ev[5 + r]
                    bload(mb[r], L, 0 if r == 0 else 1, 5)
                gT, bT = ev[3], ev[4]
                bload_vec(gT, ln_g[L, 1])
                bload_vec(bT, ln_b[L, 1])
                for ti, t in enumerate(tiles):
                    r = 1 if t < NTC else 0
                    a512 = g0 + ((t * 128 - g0) // 512) * 512
                    eb = 0 if ti % 2 == 0 else 7
                    xt, xk = ev[eb], ev[eb].name
                    P.dma("sp", xt[:], xsrc[t * 128:(t + 1) * 128, :], [("xdst", L, 0, t)], [xk])
                    y, yk = ev[eb + 1], ev[eb + 1].name
                    for hlf in range(2):
                        bq = (6 if ti % 2 == 0 else 4) + hlf
                        for cq in range(4):
                            c = hlf * 4 + cq
                            TR(ps[bq][:, cq * 128:(cq + 1) * 128], acc[:, c, t * 128 - g0:(t + 1) * 128 - g0], idf[:],
                               [("ff_acc", c, a512), "idf"], [PSK[bq]])
                        TT(y[:, hlf * 512:(hlf + 1) * 512], ps[bq][:, :], mb[r][:, hlf * 512:(hlf + 1) * 512], ALU.mult,
                           [PSK[bq], mb[r].name], [yk])
                    STT(y[:], xt[:], ALU_ALPHA, y[:], ALU.mult, ALU.add, [xk, yk], [yk])
                    x2, x2k = ev[eb + 2], ev[eb + 2].name
                    layernorm_tile(y, yk, gT, bT, x2[:], x2k, (st, mv, rstd), "ff_small")
                    if final:
                        P.dma("sp", out_d[(t - NTC) * 128:(t - NTC + 1) * 128, :], x2[:], [x2k], [("out", t)], cls="st")
                    else:
                        P.dma("sp", xdst[t * 128:(t + 1) * 128, :], x2[:], [x2k], [("xsrc", L + 1, t)], cls="st")
                P.barrier()

        def p1_phase(es, L, hT, xsrc_fn, idx=(0, 1), t0=0, rkey=None, route_cfg=None):
            mbs = {}
            for r in range(2):
                if r == 1 and t0 >= NTC:
                    continue
                for i in idx:
                    tl = sbt(es, "p1_m%d_%d" % (i, r), [128, D], F32)
                    bload(tl, L, r, i)
                    mbs[(i, r)] = tl
            xr = Ring([sbt(es, "p1_x%d" % i, [128, D], F32) for i in range(2)], "p1x")
            wk = Ring([sbt(es, "p1_h%d" % i, [128, D], F32) for i in range(2)], "p1h")
            for t in range(t0, NT):
                r = 1 if t < NTC else 0
                xt, xk = xr.next()
                P.dma("sp", xt[:], xsrc_fn(t), [rkey(t) if rkey else ("xsrc", L, t)], [xk])
                mod_transpose(xt[:], xk, mbs[(idx[1], r)], mbs[(idx[0], r)], hT, t, wk, (2 * (t % 2), 2 * (t % 2) + 1), logits=route_cfg)

        def xsrc0(t):
            return ctx_in[t * 128:(t + 1) * 128, :] if t < NTC else x_in[(t - NTC) * 128:(t - NTC + 1) * 128, :]

        upto = None
        if debug is not None:
            for d_ in debug:
                if d_.startswith("upto:"):
                    upto = d_[5:]

        ORDER = ["inproj", "attn", "gla", "outproj", "l0", "l1inproj", "mla", "hgrn", "l1out", "route", "all"]
        lvl = ORDER.index(upto) if upto else len(ORDER) - 1

        SL = 99
        if debug is not None:
            for d_ in debug:
                if d_.startswith("scan:"):
                    SL = int(d_[5:])

        def sstop(k):
            if SL < k:
                P.mute = True

        def stage(ph):
            if lvl < ORDER.index(ph):
                P.mute = True

        def inproj(hT, fm, tm):
            P.barrier()
            with ExitStack() as e2:
                wr = Ring([sbt(e2, "ipw%d" % i, [128, 8, 512], BF16) for i in range(3)], "ipw")
                stg = Ring([sbt(e2, "ipstg%d" % i, [128, 512], F32) for i in range(3)], "ipstg")
                bctr = [0]
                banks = [4, 5, 6, 7]
                for (wap, c0, n, rc0, cw) in fm:
                    slot, sk = stream_w(wr, wsrc(wap, c0, n))
                    for j in range(n // cw):
                        proj_fm(hT, T, slot, sk, j * cw, cw, (rc0 + j) * 128, stg, banks, bctr)
                for j, (wap, c0) in enumerate(tm):
                    slot, sk = stream_w(wr, wsrc(wap, c0, 512))
                    proj_tm(hT, range(NT), slot, sk, 512, j * 512, stg, banks, bctr)

        def rope_prep(ea, ld, cs, items, a, w):
            P.dma("sp", cs[0][:, 0:w], cos_d[:, a:a + w], (), ["cs0"])
            P.dma("sp", cs[1][:, 0:w], sin_d[:, a:a + w], (), ["cs1"])
            for (dst, rows, rcq, rcs) in items:
                l1, k1 = ld.next()
                l2, k2 = ld.next()
                P.dma("sp", l1[0:rows, 0:w], pfm[rcq * 128:rcq * 128 + rows, a:a + w], tkeys(("pfm", rcq), a, w), [k1])
                P.dma("sp", l2[0:rows, 0:w], pfm[rcs * 128:rcs * 128 + rows, a:a + w], tkeys(("pfm", rcs), a, w), [k2])
                TT(l1[0:rows, 0:w], l1[0:rows, 0:w], cs[0][0:rows, 0:w], ALU.mult, [k1, "cs0"], [k1])
                TT(l2[0:rows, 0:w], l2[0:rows, 0:w], cs[1][0:rows, 0:w], ALU.mult, [k2, "cs1"], [k2])
                TT(dst, l1[0:rows, 0:w], l2[0:rows, 0:w], ALU.add, [k1, k2], ["ropeout"])

        def vaug_prep(Vaug, ld, col0):
            MEMSET(Vaug[:], 1.0, (), [("Vaug", t) for t in range(NT)])
            for t in range(NT):
                l, lk = ld.next()
                P.dma("sp", l[:], ptm[t * 128:(t + 1) * 128, col0:col0 + 512], [("ptm", col0 // 512, t)], [lk])
                CP(Vaug[:, t, :, 0:128], l[:].rearrange("p (h v) -> p h v", v=128), [lk], [("Vaug", t)], eng="act")

        LATQ = [(CTX + 512 * g, 512, list(range(NT))) for g in range(4)]

        with ExitStack() as l0:
            mixT = sbt(l0, "mixT", [128, 8, T], BF16)
            with ExitStack() as es:
                hT = sbt(es, "hT", [128, 8, T], BF16)
                P.barrier()
                with ExitStack() as e1:
                    p1_phase(e1, 0, hT, xsrc0)
                fm = [(ev_w_in, 0, 512, 0, 128), (ev_w_in_sw, 0, 512, 4, 128), (ev_w_in, 512, 512, 8, 128),
                      (ev_w_in_sw, 512, 512, 12, 128), (ev_w_in, 1536, 512, 16, 128), (ev_w_in, 3072, 32, 20, 16)]
                inproj(hT, fm, [(ev_w_in, 1024), (ev_w_in, 2048), (ev_w_in, 2560)])
            dump("inproj", pfm[0:128, :], tkeys(("pfm", 0), 0, T))
            stage("attn")
            P.barrier()
            with ExitStack() as ea:
                qT = sbt(ea, "qT", [128, 4, T], BF16)
                kT = sbt(ea, "kT", [128, 4, T], BF16)
                Vaug = sbt(ea, "Vaug", [128, NT, 4, 130], BF16)
                ld = Ring([sbt(ea, "a0ld%d" % i, [128, 512], F32) for i in range(6)], "a0ld")
                cs = [sbt(ea, "a0cos", [128, 512], F32), sbt(ea, "a0sin", [128, 512], F32)]
                for (a, w) in tgroups(0, T):
                    items = []
                    for h in range(4):
                        items.append((qT[:, h, a:a + w], 128, h, 4 + h))
                        items.append((kT[:, h, a:a + w], 128, 8 + h, 12 + h))
                    rope_prep(ea, ld, cs, items, a, w)
                vaug_prep(Vaug, ld, 0)
                lv = sbt(ea, "lv", [1, 256], F32)
                lp = sbt(ea, "lp", [1, 128], F32)
                ls = sbt(ea, "ls", [1, 2], F32)
                lam2 = sbt(ea, "lam2", [1, 2], F32)
                nlam = sbt(ea, "nlam", [128, 2], F32)
                P.dma("sp", lv[:], ev_lam, (), ["lv"], cls="m")
                TT(lp[:, 0:64], lv[:, 0:64], lv[:, 64:128], ALU.mult, ["lv"], ["lp"])
                TT(lp[:, 64:128], lv[:, 128:192], lv[:, 192:256], ALU.mult, ["lv"], ["lp"])
                P.op("dve", lambda e: e.reduce_sum(ls[:], lp[:].rearrange("p (a b) -> p a b", b=64), AX.X), ["lp"], ["ls"])
                ACT(ls[:], ls[:], AF.Exp, ["ls"], ["ls"])
                TT(lam2[:, 0:1], ls[:, 1:2], ls[:, 0:1], ALU.subtract, ["ls"], ["lam2"])
                TS(lam2[:, 0:1], lam2[:, 0:1], -0.2, None, ALU.add, None, ["lam2"], ["lam2"])
                CP(lam2[:, 1:2], lam2[:, 0:1], ["lam2"], ["lam2"])
                MM(ps[7][:, 0:2], ones_f[0:1, :], lam2[0:1, 0:2], True, True, ["ones_f", "lam2"], [PSK[7]])
                CP(nlam[:], ps[7][:, 0:2], [PSK[7]], ["nlam"])
                gsub = sbt(ea, "gsub", [128, 128], F32)
                bload_vec(gsub, ev_subln_g[0])
                TS(gsub[:], gsub[:], 0.8, None, ALU.mult, None, [gsub.name], [gsub.name])
                o1n = sbt(ea, "o1n", [128, 4, 128], F32)
                r1 = sbt(ea, "a0r1", [128, 1], F32)
                ss = sbt(ea, "a0ss", [128, 1], F32)
                ofr = Ring([sbt(ea, "a0of%d" % i, [128, 128], F32) for i in range(2)], "a0of")
                junk = sbt(ea, "a0junk", [128, 128], F32)
                dump("qT0", qT[:, 0, :], ["ropeout"], eng="pool")
                for h in range(4 if not (debug and "noattn" in debug) else 0):
                    def finish(mi, s, q0, acc, acck, h=h):
                        t = (q0 + s * 128) // 128
                        if mi == 0:
                            RECIP(r1[:], acc[:, 128:129], [acck], ["a0r"])
                            TS(o1n[:, s, :], acc[:, 0:128], r1[:, 0:1], None, ALU.mult, None, [acck, "a0r"], [("o1n", s)])
                            return
                        RECIP(r1[:], acc[:, 128:129], [acck], ["a0r"])
                        TT(r1[:], r1[:], nlam[:, 0:1], ALU.mult, ["a0r", "nlam"], ["a0r"])
                        of, ofk = ofr.next()
                        STT(of[:], acc[:, 0:128], r1[:, 0:1], o1n[:, s, :], ALU.mult, ALU.add, [acck, "a0r", ("o1n", s)], [ofk])
                        TT(junk[:], of[:], of[:], ALU.mult, [ofk], ["a0junk"])
                        P.op("dve", lambda e: e.reduce_sum(ss[:], junk[:], AX.X), ["a0junk"], ["a0ss"])
                        ACT(ss[:], ss[:], AF.Ln, ["a0ss"], ["a0ss"], bias=eps_rms[:], scale=1.0 / 128)
                        ACT(ss[:], ss[:], AF.Exp, ["a0ss"], ["a0ss"], scale=-0.5)
                        STT(of[:], of[:], ss[:, 0:1], gsub[:], ALU.mult, ALU.mult, [ofk, "a0ss", gsub.name], [ofk])
                        TR(ps[7][:, 0:128], of[:], idf[:], [ofk, "idf"], [PSK[7]])
                        CP(mixT[:, h, t * 128:(t + 1) * 128], ps[7][:, 0:128], [PSK[7]], [("mixT", 0, t)])
                    maps = []
                    for m in range(2):
                        pb = 64 * m
                        maps.append([(lambda kt, pb=pb, h=h: kT[pb:pb + 64, h, kt * 128:(kt + 1) * 128],
                                      lambda q0, qw, pb=pb, h=h: qT[pb:pb + 64, h, q0:q0 + qw], ["ropeout"])])
                    qr_ = [(0, CTX, list(range(NTC)))] + LATQ
                    attention(ea, maps, lambda kt, h=h: Vaug[:, kt, h, :], qr_, 0.125, finish, [0, 1, 2], [[3, 4], [5, 6]], tag="a%d" % h)
            stage("gla")
            P.barrier()
            with ExitStack() as eg:
                lrf = sbt(eg, "g_lrf", [16, 2, T], F32)
                lrT = sbt(eg, "g_lrT", [16, 2, T], BF16)
                gkf = sbt(eg, "g_gkf", [16, 2, 256], F32)
                gkw = sbt(eg, "g_gkw", [16, 2, 256], BF16)
                nb = sbt(eg, "g_nb", [128, 4], F32)
                for d in range(2):
                    P.dma("sp", lrf[:, d, :], pfm[(20 + d) * 128:(20 + d) * 128 + 16, :], tkeys(("pfm", 20 + d), 0, T), ["g_lrf"])
                    P.dma("sp", gkf[:, d, :], ev_gk_w2[d], (), ["g_gkf"], cls="m")
                    for cc in range(2):
                        col_load(nb[:, d * 2 + cc:d * 2 + cc + 1], ev_gk_b[d, cc * 128:(cc + 1) * 128], ["g_nb"])
                CP(lrT[:], lrf[:], ["g_lrf"], ["g_lrT"])
                CP(gkw[:], gkf[:], ["g_gkf"], ["g_gkw"])
                TS(nb[:], nb[:], -1.0, None, ALU.mult, None, ["g_nb"], ["g_nb"])

                def load_q(cc, qf):
                    P.dma("sp", qf[:], pfm[(16 + cc) * 128:(17 + cc) * 128, :], tkeys(("pfm", 16 + cc), 0, T), ["sc_qf"])
                    TS(qf[:], qf[:], 0.125, None, ALU.mult, None, ["sc_qf"], ["sc_qf"])

                def make_ng_k(cc, d, ng, kf, ee):
                    P.dma("sp", kf[:], pfm[(18 + cc) * 128:(19 + cc) * 128, :], tkeys(("pfm", 18 + cc), 0, T), ["sc_kf"])
                    for gi, (a, w) in enumerate(tgroups(0, T)):
                        b = gi % 2
                        MM(ps[b][:, 0:w], gkw[:, d, cc * 128:(cc + 1) * 128], lrT[:, d, a:a + w], True, True, ["g_gkw", "g_lrT"], [PSK[b]])
                        ACT(ee[:, a:a + w], ps[b][:, 0:w], AF.Exp, [PSK[b], "g_nb"], ["sc_ee"], bias=nb[:, d * 2 + cc:d * 2 + cc + 1], scale=-1.0)
                    ACT(ng[:], ee[:], AF.Ln, ["sc_ee"], ["sc_ng"], bias=ones_f[:, 0:1], scale=1.0)

                cfg = dict(nch=2, hpc=2, dk=64, C=128, gsc=1.0 / 16, out_t0=0, vcol=512, gcol=1024, norm_g=ev_gla_g[0],
                           load_q=load_q, make_ng_k=make_ng_k)
                scan_phase(eg, cfg, mixT)
            dump("mixT0", mixT[:, 0, :], [("mixT", 0, t) for t in range(NT)], eng="pool")
            dump("mixT4", mixT[:, 4, :], [("mixT", 1, t) for t in range(NT)], eng="pool")
            stage("outproj")
            P.barrier()
            with ExitStack() as eo:
                outproj_phase(eo, 0, mixT, None, ev_w_out, xsrc0, xa, 0)
            dump("x1", xa[CTX:CTX + 128, :], [("xdst", 0, 0, NTC)])
        stage("l0")
        P.barrier()
        with ExitStack() as es:
            hT = sbt(es, "hTb", [128, 8, T], BF16)
            with ExitStack() as e1:
                p1_phase(e1, 0, hT, lambda t: xa[t * 128:(t + 1) * 128, :], idx=(3, 4), rkey=lambda t: ("xdst", 0, 0, t))
            P.barrier()
            with ExitStack() as ef:
                ffn_phase(ef, 0, hT, [(ev_w1, ev_w2)], [(0, 768), (768, 768), (1536, 768)], xa, xb)
        dump("x2", xb[CTX:CTX + 128, :], [("xsrc", 1, NTC)])

        stage("l1inproj")
        if True:
          with ExitStack() as l1:
            mixT = sbt(l1, "mixT1", [128, 8, T], BF16)
            P.barrier()
            with ExitStack() as es:
                hT = sbt(es, "hT1", [128, 8, T], BF16)
                with ExitStack() as e1:
                    p1_phase(e1, 1, hT, lambda t: xb[t * 128:(t + 1) * 128, :])
                fm = [(od_w_in, 0, 256, 0, 128), (od_w_in, 256, 128, 2, 128), (od_w_in, 384, 64, 3, 64), (od_kr_sw, 0, 64, 4, 64),
                      (od_w_in, 448, 512, 5, 128), (od_w_in, 960, 512, 9, 128), (od_w_in, 1472, 512, 13, 128)]
                inproj(hT, fm, [(od_w_in, 1984), (od_w_in, 2496)])
            stage("mla")
            P.barrier()
            with ExitStack() as em:
                qn = sbt(em, "m_qn", [128, 4, T], BF16)
                qr = sbt(em, "m_qr", [128, 4, T], BF16)
                kn = sbt(em, "m_kn", [128, 4, T], BF16)
                kr = sbt(em, "m_kr", [128, T], BF16)
                MEMSET(qr[64:128, :, :], 0.0, (), ["m_qpad"])
                MEMSET(kr[64:128, :], 0.0, (), ["m_kpad"])
                Vaug = sbt(em, "m_Vaug", [128, NT, 4, 130], BF16)
                cqn = sbt(em, "m_cqn", [128, 2, T], BF16)
                ckvn = sbt(em, "m_ckvn", [128, T], BF16)
                wuq = sbt(em, "m_wuq", [128, 2, 768], BF16)
                wuqs = sbt(em, "m_wuqs", [128, 2, 256], BF16)
                wukv = sbt(em, "m_wukv", [128, 1024], BF16)
                gcol = sbt(em, "m_gcol", [128, 3], F32)
                P.dma("pool", wuq[:], od_w_uq.rearrange("(k p) n -> p k n", p=128), (), ["m_wuq"], cls="w")
                P.dma("pool", wuqs[:], od_w_uq_sw.rearrange("(k p) n -> p k n", p=128), (), ["m_wuqs"], cls="w")
                P.dma("pool", wukv[:], od_w_ukv, (), ["m_wukv"], cls="w")
                for c in range(2):
                    col_load(gcol[:, c:c + 1], od_qg[c * 128:(c + 1) * 128], ["m_gcol"])
                col_load(gcol[:, 2:3], od_kvg, ["m_gcol"])
                ld = Ring([sbt(em, "m_ld%d" % i, [128, 512], F32) for i in range(6)], "m_ld")
                cs = [sbt(em, "m_cos", [128, 512], F32), sbt(em, "m_sin", [128, 512], F32)]
                rs = sbt(em, "m_rs", [128, 512], F32)
                for gi, (a, w) in enumerate(tgroups(0, T)):
                    for (rcs, nrm, dst, gc0) in (((0, 1), 256.0, lambda c: cqn[:, c, a:a + w], 0), ((2,), 128.0, lambda c: ckvn[:, a:a + w], 2)):
                        lt = []
                        b = gi % 2
                        for ci, rc in enumerate(rcs):
                            l, lk = ld.next()
                            s2, s2k = ld.next()
                            P.dma("sp", l[:, 0:w], pfm[rc * 128:(rc + 1) * 128, a:a + w], tkeys(("pfm", rc), a, w), [lk])
                            TT(s2[:, 0:w], l[:, 0:w], l[:, 0:w], ALU.mult, [lk], [s2k])
                            MM(ps[b][:, 0:w], ones_f[:], s2[:, 0:w], ci == 0, ci == len(rcs) - 1, ["ones_f", s2k], [PSK[b]])
                            lt.append((l, lk))
                        ACT(rs[:, 0:w], ps[b][:, 0:w], AF.Ln, [PSK[b]], ["m_rs"], bias=eps_rms[:], scale=1.0 / nrm)
                        ACT(rs[:, 0:w], rs[:, 0:w], AF.Exp, ["m_rs"], ["m_rs"], scale=-0.5)
                        for ci, (l, lk) in enumerate(lt):
                            STT(dst(ci), l[:, 0:w], gcol[:, gc0 + ci:gc0 + ci + 1], rs[:, 0:w], ALU.mult, ALU.mult,
                                [lk, "m_gcol", "m_rs"], tkeys("m_cn%d" % gc0, a, w))
                    rope_prep(em, ld, cs, [(kr[0:64, a:a + w], 64, 3, 4)], a, w)
                    for h in range(4):
                        b = 2 + (h % 2)
                        for c in range(2):
                            MM(ps[b][:, 0:w], wuq[:, c, h * 192:h * 192 + 128], cqn[:, c, a:a + w], c == 0, c == 1,
                               ["m_wuq"] + tkeys("m_cn0", a, w), [PSK[b]])
                        CP(qn[:, h, a:a + w], ps[b][:, 0:w], [PSK[b]], ["m_q"], eng="act")
                        MM(ps[b][:, 0:w], wukv[:, h * 256:h * 256 + 128], ckvn[:, a:a + w], True, True,
                           ["m_wukv"] + tkeys("m_cn2", a, w), [PSK[b]])
                        CP(kn[:, h, a:a + w], ps[b][:, 0:w], [PSK[b]], ["m_k"], eng="act")
                        for c in range(2):
                            MM(ps[4][0:64, 0:w], wuq[:, c, h * 192 + 128:h * 192 + 192], cqn[:, c, a:a + w], c == 0, c == 1,
                               ["m_wuq"] + tkeys("m_cn0", a, w), [PSK[4]])
                        for c in range(2):
                            MM(ps[5][0:64, 0:w], wuqs[:, c, h * 64:(h + 1) * 64], cqn[:, c, a:a + w], c == 0, c == 1,
                               ["m_wuqs"] + tkeys("m_cn0", a, w), [PSK[5]])
                        l1, k1 = ld.next()
                        l2, k2 = ld.next()
                        TT(l1[0:64, 0:w], ps[4][0:64, 0:w], cs[0][0:64, 0:w], ALU.mult, [PSK[4], "cs0"], [k1])
                        TT(l2[0:64, 0:w], ps[5][0:64, 0:w], cs[1][0:64, 0:w], ALU.mult, [PSK[5], "cs1"], [k2])
                        TT(qr[0:64, h, a:a + w], l1[0:64, 0:w], l2[0:64, 0:w], ALU.add, [k1, k2], ["m_q"])
                MEMSET(Vaug[:], 1.0, (), [("Vaug", t) for t in range(NT)])
                for t in range(NT):
                    b = 6 + (t % 2)
                    for h in range(4):
                        MM(ps[b][:, h * 128:(h + 1) * 128], ckvn[:, t * 128:(t + 1) * 128], wukv[:, h * 256 + 128:h * 256 + 256], h == 0, True,
                           ["m_wukv", ("m_cn2", t)], [PSK[b]])
                    CP(Vaug[:, t, :, 0:128], ps[b][:].rearrange("p (h v) -> p h v", v=128), [PSK[b]], [("Vaug", t)], eng="act")
                r1 = sbt(em, "m_r1", [128, 1], F32)
                ofr = Ring([sbt(em, "m_of%d" % i, [128, 128], F32) for i in range(2)], "m_of")
                for h in range(4):
                    def finish(mi, s, q0, acc, acck, h=h):
                        t = (q0 + s * 128) // 128
                        RECIP(r1[:], acc[:, 128:129], [acck], ["m_r"])
                        of, ofk = ofr.next()
                        TS(of[:], acc[:, 0:128], r1[:, 0:1], None, ALU.mult, None, [acck, "m_r"], [ofk])
                        TR(ps[7][:, 0:128], of[:], idf[:], [ofk, "idf"], [PSK[7]])
                        CP(mixT[:, h, t * 128:(t + 1) * 128], ps[7][:, 0:128], [PSK[7]], [("mixT", 0, t)])
                    maps = [[(lambda kt, h=h: kn[:, h, kt * 128:(kt + 1) * 128], lambda q0, qw, h=h: qn[:, h, q0:q0 + qw], ["m_k", "m_q"]),
                             (lambda kt: kr[:, kt * 128:(kt + 1) * 128], lambda q0, qw, h=h: qr[:, h, q0:q0 + qw], ["ropeout", "m_q", "m_qpad", "m_kpad"])]]
                    attention(em, maps, lambda kt, h=h: Vaug[:, kt, h, :], LATQ, 192.0 ** -0.5, finish, [0, 1, 2], [[3, 4], [5, 6]], tag="m%d" % h)
            stage("hgrn")
            P.barrier()
            with ExitStack() as eg:
                lbt = sbt(eg, "h_lbt", [128, 2, 4], F32)
                lbc = sbt(eg, "h_lbc", [128, 4], F32)
                oml = sbt(eg, "h_oml", [128, 4], F32)
                for l_ in range(2):
                    for cc in range(4):
                        col_load(lbt[:, l_, cc:cc + 1], lb_table[l_, cc * 128:(cc + 1) * 128], ["h_lbt"])
                TT(lbc[:], lbt[:, 0, :], lbt[:, 1, :], ALU.subtract, ["h_lbt"], ["h_lb"])
                ACT(lbc[:], lbc[:], AF.Exp, ["h_lb"], ["h_lb"])
                TS(lbc[:], lbc[:], 1.0, None, ALU.add, None, ["h_lb"], ["h_lb"])
                RECIP(lbc[:], lbc[:], ["h_lb"], ["h_lb"])
                TS(oml[:], lbc[:], -1.0, 1.0, ALU.mult, ALU.add, ["h_lb"], ["h_oml"])

                def load_q(cc, qf):
                    P.dma("sp", qf[:], pfm[(5 + cc) * 128:(6 + cc) * 128, :], tkeys(("pfm", 5 + cc), 0, T), ["sc_qf"])

                def make_ng_k(cc, d, ng, kf, ee):
                    rc = 9 + 4 * d + cc
                    P.dma("sp", ng[:], pfm[rc * 128:(rc + 1) * 128, :], tkeys(("pfm", rc), 0, T), ["sc_ng"])
                    ACT(ee[:], ng[:], AF.Exp, ["sc_ng"], ["sc_ee"], scale=-1.0)
                    ACT(ee[:], ee[:], AF.Ln, ["sc_ee"], ["sc_ee"], bias=ones_f[:, 0:1], scale=1.0)
                    ACT(ee[:], ee[:], AF.Exp, ["sc_ee"], ["sc_ee"], scale=-1.0)
                    TS(ee[:], ee[:], oml[:, cc:cc + 1], lbc[:, cc:cc + 1], ALU.mult, ALU.add, ["sc_ee", "h_lb", "h_oml"], ["sc_ee"])
                    TS(kf[:], ee[:], -1.0, 1.0, ALU.mult, ALU.add, ["sc_ee"], ["sc_kf"])
                    ACT(ng[:], ee[:], AF.Ln, ["sc_ee"], ["sc_ng"])
                    TS(ng[:], ng[:], -1.0, None, ALU.mult, None, ["sc_ng"], ["sc_ng"])

                cfg = dict(nch=4, hpc=1, dk=128, C=64, gsc=1.0, out_t0=NTC, vcol=0, gcol=512, norm_g=od_hg_g[0],
                           load_q=load_q, make_ng_k=make_ng_k)
                scan_phase(eg, cfg, mixT)
            dump("l1mixT0", mixT[:, 0, :], [("mixT", 0, t) for t in range(NTC, NT)], eng="pool")
            dump("l1mixT4", mixT[:, 4, :], [("mixT", 1, t) for t in range(NTC, NT)], eng="pool")
            stage("l1out")
            P.barrier()
            with ExitStack() as eo:
                outproj_phase(eo, 1, mixT, None, od_w_out, lambda t: xb[t * 128:(t + 1) * 128, :], xc, NTC)
            dump("x3", xc[CTX:CTX + 128, :], [("xdst", 1, 0, NTC)])
          stage("route")
          P.barrier()
          with ExitStack() as es:
                hT = sbt(es, "hT1b", [128, 8, T], BF16)
                combt = sbt(es, "r_comb", [128, NT - NTC, NE], F32)
                comb_d = dscr("comb_d", [NE, S])
                with ExitStack() as eo:
                    combT = sbt(eo, "r_combT", [128, S], F32)
                    MEMSET(combT[:], 0.0, (), [("combT", tt) for tt in range(NTC, NT)])
                    h2T = sbt(eo, "r_h2T", [128, 8, 128], F32)
                    wrt = sbt(eo, "r_wrt", [128, 8, NE], F32)
                    P.dma("sp", wrt[:], od_router.rearrange("(k p) e -> p k e", p=128), (), ["wrt"], cls="m")
                    comb = dict(comb=combt, lg=sbt(eo, "r_lg", [128, NE], F32), m8=sbt(eo, "r_m8", [128, 8], F32),
                                msk=sbt(eo, "r_msk", [128, NE], F32), ex=sbt(eo, "r_ex", [128, NE], F32), ssum=sbt(eo, "r_ss", [128, 1], F32))
                    p1_phase(eo, 1, hT, lambda t: xc[t * 128:(t + 1) * 128, :], idx=(3, 4), t0=NTC, rkey=lambda t: ("xdst", 1, 0, t),
                             route_cfg=(h2T, wrt, comb, 6))
                    cpad = sbt(eo, "r_cpad", [128, 128], F32)
                    MEMSET(cpad[:], 0.0, (), ["cpad"])
                    for t in range(NTC, NT):
                        i = t - NTC
                        b = 4 + i % 2
                        CP(cpad[:, 0:NE], combt[:, i, :], [("comb", t), "cpad"], ["cpad"])
                        MM(ps[b][:, 0:128], cpad[:], idf[:], True, True, ["cpad", "idf"], [PSK[b]])
                        CP(combT[0:NE, i * 128:(i + 1) * 128], ps[b][0:NE, 0:128], [PSK[b]], [("combT", t)])
                    P.dma("sp", comb_d, combT[0:NE, :], [("combT", tt) for tt in range(NTC, NT)], ["comb_d"], cls="st")
                dump("comb", combt[:].rearrange("p t e -> p (t e)"), [("comb", t) for t in range(NTC, NT)])
                stage("all")
                P.barrier()
                with ExitStack() as ef:
                    ffn_phase(ef, 1, hT, [(od_w1[e], od_w2[e]) for e in range(NE)], [(CTX, 1024), (CTX + 1024, 1024)], xc, None,
                              comb=dict(comb_d=comb_d), final=True)
          done_keys = [("out", t) for t in range(NTC, NT)]
        P.emit(done_keys + dbg_keys + [("mrow_d", 1)])
        free_ps01()
    return nc, P


def _host_consts():
    ident = np.eye(128, dtype=np.float32)
    n_freq = 16
    inv = (10000.0 ** (-np.arange(n_freq, dtype=np.float32) / n_freq)).astype(np.float32)
    rows = S // 64
    row = np.repeat(np.arange(rows, dtype=np.float32), 64)
    col = np.tile(np.arange(64, dtype=np.float32), rows)
    ang = np.concatenate([row[:, None] * inv, col[:, None] * inv], axis=-1).astype(np.float32)
    cos = np.cos(ang).astype(np.float32)
    sin = np.sin(ang).astype(np.float32)
    cosT = np.ones((128, T), np.float32)
    sinT = np.zeros((128, T), np.float32)
    for p in range(128):
        i = (p % 64) // 2
        cosT[p, CTX:] = cos[:, i]
        sinT[p, CTX:] = -sin[:, i] if p % 2 == 0 else sin[:, i]
    j = np.arange(128)[:, None]
    i = np.arange(128)[None, :]
    mF = (j <= i).astype(np.float32)
    mB = (j >= i).astype(np.float32)
    m64F = np.zeros((128, 128), np.float32)
    m64B = np.zeros((128, 128), np.float32)
    jj = (np.arange(128) % 64)[:, None]
    ii = np.arange(64)[None, :]
    m64F[:, :64] = (jj <= ii)
    m64B[:, :64] = (jj >= ii)
    masks = np.stack([mF, mB, m64F, m64B]).astype(np.float32)
    return ident, cosT, sinT, masks


def _prep_inputs(inp):
    f = lambda a: np.ascontiguousarray(np.asarray(a, dtype=np.float32))
    ident, cosT, sinT, masks = _host_consts()
    sw = np.arange(1024) ^ 1
    ev_w_in = f(inp["ev_w_in"][0])
    od_w_in = f(inp["od_w_in"][0])
    od_w_uq = f(inp["od_w_uq"][0])
    sw64 = np.arange(64) ^ 1
    uq_sw = np.concatenate([od_w_uq[:, h * 192 + 128:h * 192 + 192][:, sw64] for h in range(4)], axis=1)
    shared = {
        "ada_w": f(inp["ada_w"]), "ada_b": f(inp["ada_b"]), "post_ln_g": f(inp["post_ln_g"]), "post_ln_b": f(inp["post_ln_b"]),
        "lb_table": f(inp["lb_table"]), "ev_w_in": ev_w_in, "ev_w_in_sw": f(ev_w_in[:, :1024][:, sw]),
        "ev_lam": f(inp["ev_lam"][0].reshape(1, 256)), "ev_subln_g": f(inp["ev_subln_g"]), "ev_gk_w2": f(inp["ev_gk_w2"][0]),
        "ev_gk_b": f(inp["ev_gk_b"][0]), "ev_gla_norm_g": f(inp["ev_gla_norm_g"]), "ev_w_out": f(inp["ev_w_out"][0]),
        "ev_ffn_w1": f(inp["ev_ffn_w1"][0]), "ev_ffn_w2": f(inp["ev_ffn_w2"][0]), "od_w_in": od_w_in,
        "od_kr_sw": f(od_w_in[:, 384:448][:, sw64]), "od_q_norm_g": f(inp["od_q_norm_g"][0]), "od_kv_norm_g": f(inp["od_kv_norm_g"][0]),
        "od_w_uq": od_w_uq, "od_w_uq_sw": f(uq_sw), "od_w_ukv": f(inp["od_w_ukv"][0]), "od_hg_norm_g": f(inp["od_hg_norm_g"]),
        "od_w_out": f(inp["od_w_out"][0]), "od_router": f(inp["od_router"][0]), "od_exp_w1": f(inp["od_exp_w1"][0]),
        "od_exp_w2": f(inp["od_exp_w2"][0]), "ident": ident, "cosT": cosT, "sinT": sinT, "masks": masks,
    }
    maps = []
    for b in range(8):
        m = dict(shared)
        m["x"] = f(inp["x"][b])
        m["ctx"] = f(inp["ctx"][b])
        m["c2"] = f(np.stack([inp["c"][b], inp["c_ctx"]]))
        maps.append(m)
    return maps


def kernel(**inputs):
    nc, P = build()
    maps = _prep_inputs(inputs)
    res = run_bass_kernel_spmd(nc, maps, core_ids=list(range(8)))
    return np.stack([np.asarray(r["out"], dtype=np.float32) for r in res.results], axis=0)
```

```python
import bisect
import math
from contextlib import ExitStack

import numpy as np
import concourse.bass as bass
import concourse.mybir as mybir
from concourse.bass_utils import run_bass_kernel_spmd

F32 = mybir.dt.float32
BF16 = mybir.dt.bfloat16
AF = mybir.ActivationFunctionType
ALU = mybir.AluOpType
AX = mybir.AxisListType

ENGS = ("pe", "act", "dve", "pool", "sp")
SAME_ENG_SYNC = {"pe": False, "act": True, "dve": True, "pool": True, "sp": False}

D = 1024
S = 2048
CTX = 256
T = S + CTX
NT = T // 128
NTC = CTX // 128
DFF = 3584
NFF = DFF // 128
NE = 8
ALPHA = 4 ** 0.25
LN_EPS = 1e-5
RMS_EPS = 1e-6


class Prog:
    def __init__(self, nc):
        self.nc = nc
        self.ops = []
        self.state = {}
        self.cls = {"w": [0, 1, 2, 3], "ld": [4, 5, 6, 7], "st": [8, 9, 10, 11], "m": [12, 13]}
        self.rr = {k: 0 for k in self.cls}
        self.n_dma_sems = 14
        self.last_op = {}
        self.last_dma = {}
        self.cur_barrier = None
        self.mute = False

    def _rec(self, eng, fn, reads, writes, dma_sem=None):
        if self.mute:
            return -1
        oid = len(self.ops)
        deps = set()
        for k in reads:
            st = self.state.get(k)
            if st and st[0] is not None:
                deps.add(st[0])
        for k in writes:
            st = self.state.get(k)
            if st:
                if st[0] is not None:
                    deps.add(st[0])
                deps.update(st[1])
        if self.cur_barrier is not None:
            deps.add(self.cur_barrier)
        self.ops.append(dict(eng=eng, fn=fn, deps=deps, dma=dma_sem))
        if dma_sem is None:
            self.last_op[eng] = oid
        else:
            self.last_dma[dma_sem] = oid
        for k in reads:
            self.state.setdefault(k, [None, []])[1].append(oid)
        for k in writes:
            self.state[k] = [oid, []]
        return oid

    def op(self, eng, fn, reads=(), writes=()):
        return self._rec(eng, fn, tuple(reads), tuple(writes))

    def barrier(self):
        if self.mute:
            return -1
        deps = set(self.last_op.values()) | set(self.last_dma.values())
        if self.cur_barrier is not None:
            deps.add(self.cur_barrier)
        oid = len(self.ops)
        self.ops.append(dict(eng="sp", fn=lambda e: e.nop(), deps=deps, dma=None))
        self.last_op["sp"] = oid
        self.cur_barrier = oid
        return oid

    def dma(self, eng, out, in_, reads=(), writes=(), cls="ld"):
        lst = self.cls[cls]
        sem = lst[self.rr[cls] % len(lst)]
        self.rr[cls] += 1
        return self._rec(eng, lambda e, o=out, i=in_: e.dma_start(out=o, in_=i),
                         tuple(reads), tuple(writes), dma_sem=sem)

    def emit(self, final_keys):
        nc = self.nc
        ops = self.ops
        self.mute = False
        self.barrier()
        self._rec("sp", None, tuple(final_keys), ())
        needed = set()
        for o in ops:
            for d in o["deps"]:
                src = ops[d]
                if src["dma"] is None and src["eng"] == o["eng"] and not SAME_ENG_SYNC[o["eng"]]:
                    continue
                needed.add(d)
        cnt = {e: 0 for e in ENGS}
        dcnt = [0] * self.n_dma_sems
        dma_hist = [[] for _ in range(self.n_dma_sems)]
        for i, o in enumerate(ops):
            if o["dma"] is not None:
                s = o["dma"]
                dcnt[s] += 16
                o["val"] = dcnt[s]
                dma_hist[s].append((i, dcnt[s]))
            elif i in needed:
                cnt[o["eng"]] += 1
                o["val"] = cnt[o["eng"]]
        self.stats = dict(cnt=dict(cnt), dcnt=list(dcnt), nops=len(ops))
        per = {e: [] for e in ENGS}
        seen = {e: {} for e in ENGS}
        for i, o in enumerate(ops):
            e = o["eng"]
            req = {}
            for d in o["deps"]:
                src = ops[d]
                if src["dma"] is not None:
                    s = src["dma"]
                    hist = dma_hist[s]
                    j = bisect.bisect_left(hist, (i, -1)) - 1
                    v = hist[j][1]
                    key = ("d", s)
                else:
                    if src["eng"] == e and not SAME_ENG_SYNC[e]:
                        continue
                    v = src["val"]
                    key = ("e", src["eng"])
                if v > req.get(key, 0):
                    req[key] = v
            waits = []
            for key, v in req.items():
                if seen[e].get(key, 0) >= v:
                    continue
                seen[e][key] = v
                waits.append((key, v))
            per[e].append((i, o, waits))

        with ExitStack() as es:
            esem = {e: es.enter_context(nc.semaphore("s_" + e)) for e in ENGS}
            dsem = [es.enter_context(nc.semaphore("d_%d" % i)) for i in range(self.n_dma_sems)]
            block = es.enter_context(nc.Block())

            def run(engname):
                def body(eng):
                    for i, o, waits in per[engname]:
                        for key, v in waits:
                            sem = dsem[key[1]] if key[0] == "d" else esem[key[1]]
                            eng.wait_ge(sem, v)
                        if o["fn"] is None:
                            continue
                        ins = o["fn"](eng)
                        if o["dma"] is not None:
                            ins.then_inc(dsem[o["dma"]], 16)
                        elif i in needed:
                            ins.then_inc(esem[engname], 1)
                return body

            block.tensor(run("pe"))
            block.scalar(run("act"))
            block.vector(run("dve"))
            block.gpsimd(run("pool"))
            block.sync(run("sp"))


class Ring:
    def __init__(self, tiles, name):
        self.tiles, self.name, self.i = tiles, name, 0

    def next(self):
        j = self.i % len(self.tiles)
        self.i += 1
        return self.tiles[j], (self.name, j)


def tgroups(t0, t1, w=512):
    out = []
    a = t0
    while a < t1:
        b = min(a + w, t1)
        out.append((a, b - a))
        a = b
    return out


def tkeys(name, a, w):
    return [(name, t) for t in range(a // 128, (a + w + 127) // 128)]


def build(debug=None):
    nc = bass.Bass("TRN2", target_bir_lowering=False)
    P = Prog(nc)

    def din(name, shape):
        return nc.dram_tensor(name, list(shape), F32, kind="ExternalInput").ap()

    x_in = din("x", [S, D])
    ctx_in = din("ctx", [CTX, D])
    c2 = din("c2", [2, D])
    ada_w = din("ada_w", [2, D, 6 * D])
    ada_b = din("ada_b", [2, 6 * D])
    ln_g = din("post_ln_g", [2, 2, D])
    ln_b = din("post_ln_b", [2, 2, D])
    lb_table = din("lb_table", [2, 512])
    ev_w_in = din("ev_w_in", [D, 3104])
    ev_w_in_sw = din("ev_w_in_sw", [D, 1024])
    ev_lam = din("ev_lam", [1, 256])
    ev_subln_g = din("ev_subln_g", [1, 128])
    ev_gk_w2 = din("ev_gk_w2", [2, 16, 256])
    ev_gk_b = din("ev_gk_b", [2, 256])
    ev_gla_g = din("ev_gla_norm_g", [1, 128])
    ev_w_out = din("ev_w_out", [D, D])
    ev_w1 = din("ev_ffn_w1", [D, 2 * DFF])
    ev_w2 = din("ev_ffn_w2", [DFF, D])
    od_w_in = din("od_w_in", [D, 3008])
    od_kr_sw = din("od_kr_sw", [D, 64])
    od_qg = din("od_q_norm_g", [256])
    od_kvg = din("od_kv_norm_g", [128])
    od_w_uq = din("od_w_uq", [256, 768])
    od_w_uq_sw = din("od_w_uq_sw", [256, 256])
    od_w_ukv = din("od_w_ukv", [128, 1024])
    od_hg_g = din("od_hg_norm_g", [1, 128])
    od_w_out = din("od_w_out", [D, D])
    od_router = din("od_router", [D, NE])
    od_w1 = din("od_exp_w1", [NE, D, 2 * DFF])
    od_w2 = din("od_exp_w2", [NE, DFF, D])
    ident_d = din("ident", [128, 128])
    cos_d = din("cosT", [128, T])
    sin_d = din("sinT", [128, T])
    mask_d = din("masks", [4, 128, 128])
    out_d = nc.dram_tensor("out", [S, D], F32, kind="ExternalOutput").ap()

    def dscr(name, shape, dt=F32):
        return nc.dram_tensor(name, list(shape), dt).ap()

    mrow_d = dscr("mrow_d", [2, 2, 6 * D])
    pfm = dscr("pfm", [22 * 128, T])
    ptm = dscr("ptm", [T, 1536])
    xa = dscr("xa", [T, D])
    xb = dscr("xb", [T, D])
    xc = dscr("xc", [T, D])
    dbg_keys = []

    def dump(name, src_ap, rk, eng="sp"):
        if debug is None or name not in debug:
            return
        d_ = nc.dram_tensor("dbg_" + name, list(src_ap.shape), F32, kind="ExternalOutput").ap()
        P.dma(eng, d_, src_ap, rk, [("dbg", name)], cls="st")
        dbg_keys.append(("dbg", name))

    def MM(out, lhsT, rhs, start, stop, r, w):
        P.op("pe", lambda e, a=(out, lhsT, rhs, start, stop): e.matmul(a[0], a[1], a[2], start=a[3], stop=a[4]), r, w)

    def TR(out, in_, ident, r, w):
        P.op("pe", lambda e, a=(out, in_, ident): e.transpose(a[0], a[1], a[2]), r, w)

    def ACT(out, in_, func, r, w, bias=None, scale=None, accum_out=None):
        kw = {}
        if bias is not None:
            kw["bias"] = bias
        if scale is not None:
            kw["scale"] = scale
        if accum_out is not None:
            kw["accum_out"] = accum_out
        P.op("act", lambda e, a=(out, in_, func), kw=kw: e.activation(a[0], a[1], a[2], **kw), r, w)

    def TT(out, in0, in1, op, r, w, eng="dve"):
        P.op(eng, lambda e, a=(out, in0, in1, op): e.tensor_tensor(a[0], a[1], a[2], a[3]), r, w)

    def TS(out, in0, s1, s2, op0, op1, r, w, eng="dve"):
        if s2 is None:
            P.op(eng, lambda e, a=(out, in0, s1, op0): e.tensor_scalar(a[0], a[1], a[2], None, a[3]), r, w)
        else:
            P.op(eng, lambda e, a=(out, in0, s1, s2, op0, op1): e.tensor_scalar(a[0], a[1], a[2], a[3], a[4], a[5]), r, w)

    def STT(out, in0, scalar, in1, op0, op1, r, w):
        P.op("dve", lambda e, a=(out, in0, scalar, in1, op0, op1): e.scalar_tensor_tensor(a[0], a[1], a[2], a[3], a[4], a[5]), r, w)

    def CP(out, in_, r, w, eng="dve"):
        if eng == "act":
            P.op("act", lambda e, a=(out, in_): e.copy(a[0], a[1]), r, w)
        else:
            P.op(eng, lambda e, a=(out, in_): e.tensor_copy(a[0], a[1]), r, w)

    def RECIP(out, in_, r, w):
        P.op("dve", lambda e, a=(out, in_): e.reciprocal(a[0], a[1]), r, w)

    def MEMSET(ap, val, r, w, eng="dve"):
        P.op(eng, lambda e, a=(ap, val): e.memset(a[0], a[1]), r, w)

    def col_load(dst, src1d, w, cls="m"):
        P.dma("sp", dst, src1d.rearrange("(p o) -> p o", o=1), (), w, cls=cls)

    with ExitStack() as top:
        uid = [0]

        def sbt(es, name, shape, dt):
            uid[0] += 1
            return es.enter_context(nc.sbuf_tensor("sb%d_%s" % (uid[0], name), list(shape), dt))

        ps = [None] * 8
        for i in range(2, 8):
            ps[i] = top.enter_context(nc.psum_tensor("ps%d" % i, [128, 512], F32))
        PSK = [("ps", i) for i in range(8)]
        ps01 = [ExitStack(), 0]

        def alloc_ps01():
            ps01[1] += 1
            for i in range(2):
                ps[i] = ps01[0].enter_context(nc.psum_tensor("ps%d_%d" % (i, ps01[1]), [128, 512], F32))

        def free_ps01():
            ps01[0].close()
            ps01[0] = ExitStack()
            ps[0] = ps[1] = None

        alloc_ps01()

        idf = sbt(top, "idf", [128, 128], F32)
        idb = sbt(top, "idb", [128, 128], BF16)
        masks = sbt(top, "masks", [128, 4, 128], F32)
        ones_f = sbt(top, "ones_f", [128, 128], F32)
        eps_ln = sbt(top, "eps_ln", [128, 1], F32)
        eps_rms = sbt(top, "eps_rms", [128, 1], F32)
        P.dma("sp", idf[:], ident_d, (), ["idf"], cls="m")
        P.dma("sp", masks[:], mask_d.rearrange("m p c -> p m c"), (), ["masks"], cls="m")
        CP(idb[:], idf[:], ["idf"], ["idb"])
        MEMSET(ones_f[:], 1.0, (), ["ones_f"])
        MEMSET(eps_ln[:], LN_EPS, (), ["eps"])
        MEMSET(eps_rms[:], RMS_EPS, (), ["eps"])

        with ExitStack() as es:
            scin = sbt(es, "scin", [128, 2, 8], F32)
            sce = sbt(es, "sce", [128, 2, 8], F32)
            scT = sbt(es, "scT", [128, 8, 2], BF16)
            wr = Ring([sbt(es, "adaw%d" % i, [128, 8, 512], BF16) for i in range(3)], "adaw")
            mrow = sbt(es, "mrow", [2, 6 * D], F32)
            brow = sbt(es, "brow", [2, 6 * D], F32)
            for r in range(2):
                P.dma("sp", scin[:, r, :], c2[r].rearrange("(p k) -> p k", k=8), (), ["scin"], cls="m")
            ACT(sce[:], scin[:], AF.Exp, ["scin"], ["sce"], scale=-1.0)
            TS(sce[:], sce[:], 1.0, None, ALU.add, None, ["sce"], ["sce"])
            RECIP(sce[:], sce[:], ["sce"], ["sce"])
            TT(scT[:].rearrange("p k r -> p r k"), scin[:], sce[:], ALU.mult, ["scin", "sce"], ["scT"])
            bi = 0
            for L in range(2):
                for r in range(2):
                    P.dma("sp", brow[r:r + 1, :], ada_b[L:L + 1, :], ["mrow"], ["brow"], cls="m")
                for n in range(12):
                    slot, sk = wr.next()
                    P.dma("pool", slot[:], ada_w[L][:, n * 512:(n + 1) * 512].rearrange("(p k) n -> p k n", k=8), (), [sk], cls="w")
                    b = bi % 2
                    bi += 1
                    for k in range(8):
                        MM(ps[b][0:2, :], scT[:, k, :], slot[:, k, :], k == 0, k == 7, ["scT", sk], [PSK[b]])
                    TT(mrow[:, n * 512:(n + 1) * 512], ps[b][0:2, :], brow[:, n * 512:(n + 1) * 512], ALU.add,
                       [PSK[b], "brow"], ["mrow"])
                for i in (1, 4):
                    TS(mrow[:, i * D:(i + 1) * D], mrow[:, i * D:(i + 1) * D], 1.0, None, ALU.add, None, ["mrow"], ["mrow"])
                P.dma("sp", mrow_d[L], mrow[:], ["mrow"], [("mrow_d", L)], cls="st")
                if L == 0:
                    dump("mrow", mrow[:], ["mrow"])
                    dump("scin", scin[:].rearrange("p r k -> p (r k)"), ["scin"])
                    dump("sce", sce[:].rearrange("p r k -> p (r k)"), ["sce"])

        def bload(tile, L, r, i):
            P.dma("sp", tile[:], mrow_d[L, r, i * D:(i + 1) * D].partition_broadcast(128), [("mrow_d", L)], [tile.name], cls="m")

        def bload_vec(tile, src1d):
            P.dma("sp", tile[:], src1d.partition_broadcast(128), (), [tile.name], cls="m")

        def mod_transpose(src, skey, mA, mB, hT, t, work, pbanks, logits=None):
            h, hk = work.next()
            TT(h[:], src, mA[:], ALU.mult, [skey, mA.name], [hk])
            TT(h[:], h[:], mB[:], ALU.add, [hk, mB.name], [hk])
            if t == 2:
                dump("h2", h[:], [hk])
                dump("mA", mA[:], [mA.name])
            ba, bb = pbanks
            for k in range(8):
                b = ba if k < 4 else bb
                TR(ps[b][:, (k % 4) * 128:(k % 4 + 1) * 128], h[:, k * 128:(k + 1) * 128], idf[:], [hk, "idf"], [PSK[b]])
            if logits is None:
                for j, b in enumerate((ba, bb)):
                    CP(hT[:, j * 4:(j + 1) * 4, t * 128:(t + 1) * 128], ps[b][:].rearrange("p (k n) -> p k n", k=4),
                       [PSK[b]], [("hT", t)], eng="act")
            if logits is not None:
                h2T, wrt, comb, lb = logits
                sstop(10)
                for j, b in enumerate((ba, bb)):
                    CP(h2T[:, j * 4:(j + 1) * 4, :], ps[b][:].rearrange("p (k n) -> p k n", k=4), [PSK[b]], ["h2T"])
                CP(hT[:, :, t * 128:(t + 1) * 128], h2T[:], ["h2T"], [("hT", t)], eng="act")
                sstop(11)
                for k in range(8):
                    MM(ps[lb][:, 0:8], h2T[:, k, :], wrt[:, k, :], k == 0, k == 7, ["h2T", "wrt"], [PSK[lb]])
                sstop(12)
                route(ps[lb][:, 0:8], PSK[lb], comb, t)

        def route(lg_ps, lgk, comb, t):
            lg, m8, msk, ex, ssum = comb["lg"], comb["m8"], comb["msk"], comb["ex"], comb["ssum"]
            CP(lg[:], lg_ps, [lgk], ["r_lg"])
            sstop(13)
            P.op("dve", lambda e: e.max(out=m8[:], in_=lg[:]), ["r_lg"], ["r_m8"])
            sstop(14)
            TS(msk[:], lg[:], m8[:, 1:2], None, ALU.is_ge, None, ["r_lg", "r_m8"], ["r_msk"])
            sstop(15)
            TS(ex[:], lg[:], m8[:, 0:1], None, ALU.subtract, None, ["r_lg", "r_m8"], ["r_ex"])
            ACT(ex[:], ex[:], AF.Exp, ["r_ex"], ["r_ex"])
            TT(ex[:], ex[:], msk[:], ALU.mult, ["r_ex", "r_msk"], ["r_ex"])
            P.op("dve", lambda e: e.reduce_sum(ssum[:], ex[:], AX.X), ["r_ex"], ["r_ss"])
            RECIP(ssum[:], ssum[:], ["r_ss"], ["r_ss"])
            TS(comb["comb"][:, t - NTC, :], ex[:], ssum[:, 0:1], None, ALU.mult, None, ["r_ex", "r_ss"], [("comb", t)])

        def layernorm_tile(tl, tk, gT, bT, dst, dk, small, sk):
            st, mv, rstd = small
            tv = tl[:].rearrange("p (c f) -> p c f", f=512)
            for c in range(2):
                P.op("dve", lambda e, c=c: e.bn_stats(st[:, c, :], tv[:, c, :]), [tk], [sk])
            P.op("dve", lambda e: e.bn_aggr(mv[:], st[:]), [sk], [sk])
            ACT(rstd[:], mv[:, 1:2], AF.Ln, [sk], [sk], bias=eps_ln[:], scale=1.0)
            ACT(rstd[:], rstd[:], AF.Exp, [sk], [sk], scale=-0.5)
            TS(tl[:], tl[:], mv[:, 0:1], rstd[:, 0:1], ALU.subtract, ALU.mult, [tk, sk], [tk])
            TT(tl[:], tl[:], gT[:], ALU.mult, [tk, gT.name], [tk])
            TT(dst, tl[:], bT[:], ALU.add, [tk, bT.name], [dk], eng="pool")

        def stream_w(ring, src_ap, eng="pool"):
            slot, sk = ring.next()
            P.dma(eng, slot[:] if src_ap.shape[-1] == slot.shape[-1] else slot[:, :, 0:src_ap.shape[-1]], src_ap, (), [sk], cls="w")
            return slot, sk

        def proj_fm(hT, ntok, slot, sk, c0, ncols, rows0, stg, banks, bctr):
            for (a, w) in tgroups(0, ntok):
                b = banks[bctr[0] % len(banks)]
                bctr[0] += 1
                for k in range(8):
                    MM(ps[b][0:ncols, 0:w], slot[:, k, c0:c0 + ncols], hT[:, k, a:a + w], k == 0, k == 7,
                       [sk] + tkeys("hT", a, w), [PSK[b]])
                s, stk = stg.next()
                CP(s[0:ncols, 0:w], ps[b][0:ncols, 0:w], [PSK[b]], [stk], eng="act")
                P.dma("sp", pfm[rows0:rows0 + ncols, a:a + w], s[0:ncols, 0:w], [stk], tkeys(("pfm", rows0 // 128), a, w), cls="st")

        def proj_tm(hT, tiles, slot, sk, ncols, col0, stg, banks, bctr):
            for t in tiles:
                b = banks[bctr[0] % len(banks)]
                bctr[0] += 1
                for k in range(8):
                    MM(ps[b][:, 0:ncols], hT[:, k, t * 128:(t + 1) * 128], slot[:, k, 0:ncols], k == 0, k == 7,
                       [sk, ("hT", t)], [PSK[b]])
                s, stk = stg.next()
                CP(s[:, 0:ncols], ps[b][:, 0:ncols], [PSK[b]], [stk], eng="act")
                P.dma("sp", ptm[t * 128:(t + 1) * 128, col0:col0 + ncols], s[:, 0:ncols], [stk], [("ptm", col0 // 512, t)], cls="st")

        def wsrc(w_ap, c0, n):
            return w_ap[:, c0:c0 + n].rearrange("(k p) n -> p k n", p=128)

        def attention(es, maps, Vaug, qranges, scale, finish, sbanks, abanks, tag=""):
            ptr = Ring([sbt(es, "pT%s_%d" % (tag, i), [128, 512], BF16) for i in range(3)], "pT" + tag)
            sctr = 0
            actr = 0
            for (q0, qw, ktiles) in qranges:
                nsub = qw // 128
                for mi, parts in enumerate(maps):
                    accs = abanks[actr % len(abanks)]
                    actr += 1
                    def score(idx_):
                        kt_ = ktiles[idx_]
                        sbk = sbanks[(sctr + idx_) % len(sbanks)]
                        for pi, (kfn, qfn, rk) in enumerate(parts):
                            MM(ps[sbk][:, 0:qw], kfn(kt_), qfn(q0, qw), pi == 0, pi == len(parts) - 1, rk, [PSK[sbk]])
                        return sbk
                    sb_cur = score(0)
                    for idx, kt in enumerate(ktiles):
                        sb_next = score(idx + 1) if idx + 1 < len(ktiles) else None
                        sb_ = sb_cur
                        pt, ptk = ptr.next()
                        ACT(pt[:, 0:qw], ps[sb_][:, 0:qw], AF.Exp, [PSK[sb_]], [ptk], scale=scale)
                        for s in range(nsub):
                            bk = accs[s // 2]
                            c0 = (s % 2) * 256
                            MM(ps[bk][:, c0:c0 + 130], pt[:, s * 128:(s + 1) * 128], Vaug(kt), idx == 0 and s % 2 == 0,
                               idx == len(ktiles) - 1, [ptk, ("Vaug", kt)], [PSK[bk]])
                        sb_cur = sb_next
                    sctr += len(ktiles)
                    for s in range(nsub):
                        bk = accs[s // 2]
                        c0 = (s % 2) * 256
                        finish(mi, s, q0, ps[bk][:, c0:c0 + 130], PSK[bk])

        def scan_phase(es, cfg, mixT):
            nch, hpc, dk, C, gsc = cfg["nch"], cfg["hpc"], cfg["dk"], cfg["C"], cfg["gsc"]
            nchunks = T // C
            cpt = 128 // C
            out_t0 = cfg["out_t0"]
            NG = 2
            ncg = nch // NG
            big = lambda n: sbt(es, n, [128, T], F32)
            ng, Cs, Ce, ee, qf, kf = big("sc_ng"), big("sc_Cs"), big("sc_Ce"), big("sc_ee"), big("sc_qf"), big("sc_kf")
            onesT = sbt(es, "sc_ones", [128, T], BF16)
            MEMSET(onesT[:], 1.0, (), ["sc_ones"])
            qh = [sbt(es, "sc_qh%d" % i, [128, T], BF16) for i in range(ncg)]
            kh = [sbt(es, "sc_kh%d" % i, [128, T], BF16) for i in range(ncg)]
            qm = None
            if hpc == 2:
                qm = [[sbt(es, "sc_qm%d_%d" % (i, j), [128, T], BF16) for j in range(2)] for i in range(ncg)]
                for i in range(ncg):
                    for j in range(2):
                        MEMSET(qm[i][j][:], 0.0, (), [("sc_qh", i)])
            c_out0 = out_t0 * cpt
            vbt = sbt(es, "sc_v", [128, nchunks, 256], BF16)
            oacc = sbt(es, "sc_oacc", [128, nchunks - c_out0, 256], F32)
            if C < 128:
                MEMSET(vbt[:], 0.0, (), [("sc_v", c) for c in range(nchunks)])
            cols = [[sbt(es, "sc_c%d_%d" % (i, j), [128, nchunks], F32) for j in range(6)] for i in range(ncg)]
            Sst = [sbt(es, "sc_S%d" % i, [128, 128], F32) for i in range(ncg)]
            Sef = [[sbt(es, "sc_Se%d_%d" % (i, j), [128, 128], BF16) for j in range(2)] for i in range(ncg)]
            tmpS = sbt(es, "sc_tmpS", [128, 128], F32)
            khtm = Ring([sbt(es, "sc_khtm%d" % i, [128, 256], BF16) for i in range(2)], "khtm")
            Am = Ring([sbt(es, "sc_Am%d" % i, [128, 2 * C], BF16) for i in range(2)], "Am")
            ldr = Ring([sbt(es, "sc_ld%d" % i, [128, 256], F32) for i in range(3)], "scld")
            for tl_ in khtm.tiles:
                MEMSET(tl_[:], 0.0, (), [("khtm", 0), ("khtm", 1)])
            for tl_ in Am.tiles:
                MEMSET(tl_[:], 0.0, (), [("Am", 0), ("Am", 1)])
            gT = sbt(es, "sc_gT", [128, 128], F32)
            bload_vec(gT, cfg["norm_g"])
            wk = Ring([sbt(es, "sc_mw%d" % i, [128, 256], F32) for i in range(2)], "scmw")
            gk = Ring([sbt(es, "sc_mg%d" % i, [128, 256], F32) for i in range(2)], "scmg")
            ssq = sbt(es, "sc_ssq", [128, 2], F32)
            for hg in range(NG):
                vc0 = cfg["vcol"] + hg * 256
                for c in range(nchunks):
                    l, lk = ldr.next()
                    P.dma("sp", l[0:C, :], ptm[c * C:(c + 1) * C, vc0:vc0 + 256], [("ptm", cfg["vcol"] // 512, (c * C) // 128)], [lk])
                    CP(vbt[0:C, c, :], l[0:C, :], [lk], [("sc_v", c)], eng="pool")
                for d in range(2):
                    maskF = masks[:, (0 if C == 128 else 2) + d, 0:C]
                    for lc in range(ncg):
                        cc = hg * ncg + lc
                        cfg["load_q"](cc, qf)
                        cfg["make_ng_k"](cc, d, ng, kf, ee)
                        sstop(-3)
                        P.op("dve", lambda e: e.tensor_tensor_scan(Cs[:], onesT[:], ng[:], 0.0, ALU.mult, ALU.add),
                             ["sc_ones", "sc_ng"], ["sc_Cs"])
                        TT(Ce[:], Cs[:], ng[:], ALU.subtract, ["sc_Cs", "sc_ng"], ["sc_Ce"])
                        sstop(-2)
                        Cs3 = Cs[:].rearrange("p (n c) -> p n c", c=C)
                        Ce3 = Ce[:].rearrange("p (n c) -> p n c", c=C)
                        cA, cZ, cR, c1, c2_, c3 = cols[lc]
                        ck = ("sc_cols", lc)
                        CP(cA[:], Cs3[:, :, C - 1], ["sc_Cs"], [ck])
                        MEMSET(cZ[:, 0:1], 0.0, (), [ck])
                        CP(cZ[:, 1:nchunks], cA[:, 0:nchunks - 1], [ck], [ck])
                        if d == 0:
                            CP(cR[:], Cs3[:, :, C // 2 - 1], ["sc_Cs"], [ck])
                            base3 = Cs3
                        else:
                            CP(cR[:], Ce3[:, :, C // 2], ["sc_Ce"], [ck])
                            base3 = Ce3
                        TT(c1[:], cR[:], cZ[:], ALU.subtract, [ck], [ck])
                        TT(c2_[:], cA[:], cZ[:], ALU.subtract, [ck], [ck])
                        TT(c3[:], cA[:], cR[:], ALU.subtract, [ck], [ck])
                        for c_ in (c1, c2_, c3):
                            ACT(c_[:], c_[:], AF.Exp, [ck], [ck], scale=-gsc)
                        sstop(-1)
                        rel = ee
                        TT(rel[:].rearrange("p (n c) -> p n c", c=C), base3, cR[:].unsqueeze(2).to_broadcast([128, nchunks, C]),
                           ALU.subtract, ["sc_Cs", "sc_Ce", ck], ["sc_ee"])
                        sq = -gsc if d == 0 else gsc
                        ACT(Ce[:], rel[:], AF.Exp, ["sc_ee"], ["sc_Ce"], scale=sq)
                        TT(qh[lc][:], qf[:], Ce[:], ALU.mult, ["sc_qf", "sc_Ce"], [("sc_qh", lc)])
                        if hpc == 2:
                            for j in range(2):
                                CP(qm[lc][j][j * 64:(j + 1) * 64, :], qh[lc][j * 64:(j + 1) * 64, :], [("sc_qh", lc)], [("sc_qh", lc)], eng="pool")
                        ACT(Ce[:], rel[:], AF.Exp, ["sc_ee"], ["sc_Ce"], scale=-sq)
                        TT(kh[lc][:], kf[:], Ce[:], ALU.mult, ["sc_kf", "sc_Ce"], [("sc_kh", lc)])
                    sstop(0)
                    if d == 0:
                        order = list(range(nchunks))
                    else:
                        nctx = CTX // C
                        order = list(range(nctx - 1, -1, -1)) + list(range(nchunks - 1, nctx - 1, -1))
                    for lc in range(ncg):
                        MEMSET(Sst[lc][:], 0.0, (), [("sc_S", lc)])
                        MEMSET(Sef[lc][0][:], 0.0, (), [("sc_Se", lc, 0)])
                    bK, bA, bO, bD = [0, 1], [2, 3], [4, 5], [6, 7]
                    eM = [cols[lc][3 if d == 0 else 5] for lc in range(ncg)]
                    eL = [cols[lc][4] for lc in range(ncg)]
                    eLM = [cols[lc][5 if d == 0 else 3] for lc in range(ncg)]
                    sstop(2)
                    def qsel(lc, hl, tok0_):
                        return qm[lc][hl % 2][:, tok0_:tok0_ + C] if hpc == 2 else qh[lc][:, tok0_:tok0_ + C]

                    def front(oi_):
                        tok0_ = order[oi_] * C
                        kb = bK[oi_ % 2]
                        for lc in range(ncg):
                            MM(ps[kb][0:C, lc * 128:(lc + 1) * 128], kh[lc][:, tok0_:tok0_ + C], idb[:], lc == 0, True,
                               [("sc_kh", lc), "idb"], [PSK[kb]])
                        ktm_, ktk_ = khtm.next()
                        CP(ktm_[0:C, 0:ncg * 128], ps[kb][0:C, 0:ncg * 128], [PSK[kb]], [ktk_], eng="act")
                        ab = bA[oi_ % 2]
                        for hl in range(2):
                            lc = hl // hpc
                            MM(ps[ab][0:C, hl * C:(hl + 1) * C], kh[lc][:, tok0_:tok0_ + C],
                               qsel(lc, hl, tok0_), hl == 0, True, [("sc_kh", lc), ("sc_qh", lc)], [PSK[ab]])
                        am_, amk_ = Am.next()
                        TT(am_[0:C, :].rearrange("p (h c) -> p h c", c=C),
                           ps[ab][0:C, 0:2 * C].rearrange("p (h c) -> p h c", c=C),
                           maskF[0:C, None, :].to_broadcast([C, 2, C]), ALU.mult, [PSK[ab], "masks"], [amk_])
                        return ktm_, ktk_, am_, amk_

                    cur = front(0)
                    for oi, c in enumerate(order):
                        pb = 0
                        t = (c * C) // 128
                        tok0 = c * C
                        r2 = oi % 2
                        nxt = front(oi + 1) if oi + 1 < len(order) else None
                        ktm, ktk, am, amk = cur
                        cur = nxt
                        for _once in (0,):
                            if oi == len(order) - 1:
                                break
                            sstop(5)
                            db = bD[r2]
                            for hl in range(2):
                                lc, r0 = hl // hpc, (hl % hpc) * dk
                                MM(ps[db][:, hl * 128:(hl + 1) * 128], ktm[:, lc * 128:(lc + 1) * 128],
                                   vbt[:, c, hl * 128:(hl + 1) * 128], hl == 0, True, [ktk, ("sc_v", c)], [PSK[db]])
                            cn = order[oi + 1]
                            for lc in range(ncg):
                                for j in range(hpc):
                                    hl = lc * hpc + j
                                    r0 = j * dk
                                    TS(tmpS[r0:r0 + dk, :], ps[db][r0:r0 + dk, hl * 128:(hl + 1) * 128], eLM[lc][r0:r0 + dk, c:c + 1], None, ALU.mult, None,
                                       [PSK[db], ("sc_cols", lc)], ["sc_tmpS"])
                                STT(Sst[lc][:], Sst[lc][:], eL[lc][:, c:c + 1], tmpS[:], ALU.mult, ALU.add,
                                    [("sc_S", lc), "sc_tmpS", ("sc_cols", lc)], [("sc_S", lc)])
                                TS(Sef[lc][(oi + 1) % 2][:], Sst[lc][:], eM[lc][:, cn:cn + 1], None, ALU.mult, None,
                                   [("sc_S", lc), ("sc_cols", lc)], [("sc_Se", lc, (oi + 1) % 2)])
                        sstop(4)
                        need_out = t >= out_t0
                        ob = bO[r2]
                        if need_out:
                            for hl in range(2):
                                lc, r0 = hl // hpc, (hl % hpc) * dk
                                MM(ps[ob][pb:pb + C, hl * 128:(hl + 1) * 128], am[:, hl * C:(hl + 1) * C],
                                   vbt[:, c, hl * 128:(hl + 1) * 128], hl == 0, False, [amk, ("sc_v", c)], [PSK[ob]])
                                MM(ps[ob][pb:pb + C, hl * 128:(hl + 1) * 128], qsel(lc, hl, tok0),
                                   Sef[lc][oi % 2][:, :], False, True, [("sc_qh", lc), ("sc_Se", lc, oi % 2)], [PSK[ob]])
                            okey = ("sc_oacc", c)
                            if d == 0:
                                CP(oacc[0:C, c - c_out0, :], ps[ob][0:C, 0:256], [PSK[ob]], [okey], eng="act")
                            else:
                                TT(oacc[0:C, c - c_out0, :], oacc[0:C, c - c_out0, :], ps[ob][0:C, 0:256], ALU.add,
                                   [PSK[ob], okey], [okey])
                sstop(6)
                gc0 = cfg["gcol"] + hg * 256
                for c in range(c_out0, nchunks):
                    t = (c * C) // 128
                    o = oacc[0:C, c - c_out0, :]
                    okeys = [("sc_oacc", c)]
                    w_, wk_ = wk.next()
                    g_, gk_ = gk.next()
                    P.dma("sp", g_[0:C, :], ptm[c * C:(c + 1) * C, gc0:gc0 + 256], [("ptm", cfg["gcol"] // 512, t)], [gk_])
                    TT(w_[0:C, :], o, o, ALU.mult, okeys, [wk_])
                    P.op("dve", lambda e, w_=w_: e.reduce_sum(ssq[0:C, :], w_[0:C, :].rearrange("p (h v) -> p h v", v=128), AX.X), [wk_], ["sc_ssq"])
                    ACT(ssq[0:C, :], ssq[0:C, :], AF.Ln, ["sc_ssq"], ["sc_ssq"], bias=eps_rms[0:C, :], scale=1.0 / 128)
                    ACT(ssq[0:C, :], ssq[0:C, :], AF.Exp, ["sc_ssq"], ["sc_ssq"], scale=-0.5)
                    TT(w_[0:C, :].rearrange("p (h v) -> p h v", v=128), o.rearrange("p (h v) -> p h v", v=128),
                       ssq[0:C, :].unsqueeze(2).to_broadcast([C, 2, 128]), ALU.mult, okeys + ["sc_ssq"], [wk_])
                    TT(w_[0:C, :].rearrange("p (h v) -> p h v", v=128), w_[0:C, :].rearrange("p (h v) -> p h v", v=128),
                       gT[0:C, None, :].to_broadcast([C, 2, 128]), ALU.mult, [wk_, gT.name], [wk_])
                    e_, ek_ = ldr.next()
                    ACT(e_[0:C, :], g_[0:C, :], AF.Exp, [gk_], [ek_], scale=-1.0)
                    ACT(e_[0:C, :], e_[0:C, :], AF.Ln, [ek_], [ek_], bias=ones_f[0:C, 0:1], scale=1.0)
                    ACT(e_[0:C, :], e_[0:C, :], AF.Exp, [ek_], [ek_], scale=-1.0)
                    TT(g_[0:C, :], g_[0:C, :], e_[0:C, :], ALU.mult, [gk_, ek_], [gk_])
                    TT(w_[0:C, :], w_[0:C, :], g_[0:C, :], ALU.mult, [wk_, gk_], [wk_])
                    b = 6 + (c % 2)
                    for hl in range(2):
                        TR(ps[b][:, hl * C:(hl + 1) * C], w_[0:C, hl * 128:(hl + 1) * 128], idf[0:C, 0:C], [wk_, "idf"], [PSK[b]])
                    CP(mixT[:, 4 + 2 * hg:6 + 2 * hg, c * C:(c + 1) * C], ps[b][:, 0:2 * C].rearrange("p (h n) -> p h n", h=2),
                       [PSK[b]], [("mixT", 1, t)], eng="act")

        def outproj_phase(es, L, mixT, hT, w_out, xsrc, xdst, t0, route_cfg=None):
            wo = sbt(es, "wo", [128, 8, D], BF16)
            for hlf in range(2):
                P.dma("pool", wo[:, :, hlf * 512:(hlf + 1) * 512], wsrc(w_out, hlf * 512, 512), (), [("wo", hlf)], cls="w")
            names = ["m2"]
            mb = {}
            for r in range(2):
                if r == 1 and t0 >= NTC:
                    continue
                for i, nm in zip((2,), names):
                    tl = sbt(es, "ob_%s_%d" % (nm, r), [128, D], F32)
                    bload(tl, L, 0 if r == 0 else 1, i)
                    mb[(nm, r)] = tl
            gT = sbt(es, "ob_g", [128, D], F32)
            bT = sbt(es, "ob_b", [128, D], F32)
            bload_vec(gT, ln_g[L, 0])
            bload_vec(bT, ln_b[L, 0])
            xr = Ring([sbt(es, "ob_x%d" % i, [128, D], F32) for i in range(2)], "obx")
            yr = Ring([sbt(es, "ob_y%d" % i, [128, D], F32) for i in range(2)], "oby")
            x1r = Ring([sbt(es, "ob_x1%d" % i, [128, D], F32) for i in range(2)], "obx1")
            st = sbt(es, "ob_st", [128, 2, 6], F32)
            mv = sbt(es, "ob_mv", [128, 2], F32)
            rstd = sbt(es, "ob_rstd", [128, 1], F32)
            for t in range(t0, NT):
                r = 1 if t < NTC else 0
                xt, xk = xr.next()
                P.dma("sp", xt[:], xsrc(t), [("xsrc", L, t)], [xk])
                y, yk = yr.next()
                for hlf in range(2):
                    b = 2 * (t % 2) + hlf
                    for k in range(8):
                        MM(ps[b][:, :], mixT[:, k, t * 128:(t + 1) * 128], wo[:, k, hlf * 512:(hlf + 1) * 512], k == 0, k == 7,
                           [("mixT", k // 4, t), ("wo", hlf)], [PSK[b]])
                    TT(y[:, hlf * 512:(hlf + 1) * 512], ps[b][:, :], mb[("m2", r)][:, hlf * 512:(hlf + 1) * 512], ALU.mult,
                       [PSK[b], mb[("m2", r)].name], [yk])
                STT(y[:], xt[:], ALU_ALPHA, y[:], ALU.mult, ALU.add, [xk, yk], [yk])
                x1, x1k = x1r.next()
                layernorm_tile(y, yk, gT, bT, x1[:], x1k, (st, mv, rstd), "ob_small")
                P.dma("sp", xdst[t * 128:(t + 1) * 128, :], x1[:], [x1k], [("xdst", L, 0, t)], cls="st")

        ALU_ALPHA = float(ALPHA)

        class View:
            def __init__(self, ap, name):
                self.ap, self.name = ap, name

            def __getitem__(self, k):
                return self.ap[k]

        def ffn_phase(es, L, hT, experts, groups, xsrc, xdst, comb=None, final=False):
            maxw = max(w for _, w in groups)
            hidraw = sbt(es, "ff_hid", [128, NFF * maxw], BF16)
            hid = hidraw[:].rearrange("p (f w) -> p f w", w=maxw)
            hf32 = hidraw[:].bitcast(F32)
            ev = [View(hf32[:, i * D:(i + 1) * D], "ff_ev%d" % i) for i in range(10)]
            acc = sbt(es, "ff_acc", [128, 8, maxw], F32)
            wring = Ring([sbt(es, "ff_w_%d" % i, [128, 8192], BF16) for i in range(3)], "ffw")
            sil = Ring([sbt(es, "ff_sil%d" % i, [128, 512], F32) for i in range(2)], "ffsil")
            tmp = Ring([sbt(es, "ff_tmp%d" % i, [128, 512], F32) for i in range(2)], "fftmp")
            st = sbt(es, "ff_st", [128, 2, 6], F32)
            mv = sbt(es, "ff_mv", [128, 2], F32)
            rstd = sbt(es, "ff_rstd", [128, 1], F32)
            cbt = None
            if comb is not None:
                cbt = sbt(es, "ff_cb", [128, maxw], F32)
            hb = [0, 1, 2, 3]
            hbc = 0
            for gi, (g0, gw) in enumerate(groups):
                subs = tgroups(g0, g0 + gw)
                for ei, (w1, w2) in enumerate(experts):
                    for f4 in range(NFF // 4):
                        if comb is not None and f4 == 0:
                            P.dma("sp", cbt[:, 0:gw], comb["comb_d"][ei, g0 - CTX:g0 - CTX + gw].partition_broadcast(128),
                                  ["comb_d"], ["ff_cb"], cls="m")
                        slot_, sk = wring.next()
                        slot = slot_[:].rearrange("p (k n) -> p k n", k=8)
                        P.dma("pool", slot[:, :, 0:512], wsrc(w1, f4 * 512, 512), (), [sk], cls="w")
                        P.dma("pool", slot[:, :, 512:1024], wsrc(w1, DFF + f4 * 512, 512), (), [sk], cls="w")
                        for fj in range(4):
                            f = f4 * 4 + fj
                            for (a, w) in subs:
                                bg = hb[hbc % 4]
                                bu = hb[(hbc + 1) % 4]
                                hbc += 2
                                for k in range(8):
                                    MM(ps[bg][:, 0:w], slot[:, k, fj * 128:(fj + 1) * 128], hT[:, k, a:a + w], k == 0, k == 7,
                                       [sk] + tkeys("hT", a, w), [PSK[bg]])
                                for k in range(8):
                                    MM(ps[bu][:, 0:w], slot[:, k, 512 + fj * 128:512 + (fj + 1) * 128], hT[:, k, a:a + w], k == 0, k == 7,
                                       [sk] + tkeys("hT", a, w), [PSK[bu]])
                                s_, sk_ = sil.next()
                                ACT(s_[:, 0:w], ps[bg][:, 0:w], AF.Silu, [PSK[bg]], [sk_])
                                TT(hid[:, f, a - g0:a - g0 + w], s_[:, 0:w], ps[bu][:, 0:w], ALU.mult, [sk_, PSK[bu]], [("ff_hid", f)])
                    HF = NFF // 2
                    for c4 in range(2):
                        for fh in range(2):
                            slot_, sk = wring.next()
                            slot = slot_[:, 0:HF * 512].rearrange("p (f n) -> p f n", n=512)
                            P.dma("pool", slot, w2[fh * HF * 128:(fh + 1) * HF * 128, c4 * 512:(c4 + 1) * 512].rearrange("(f p) n -> p f n", p=128),
                                  (), [sk], cls="w")
                            first = ei == 0 and fh == 0
                            for cj in range(4):
                                c = c4 * 4 + cj
                                for (a, w) in subs:
                                    b = 4 + (hbc % 2)
                                    hbc += 1
                                    for fi in range(HF):
                                        f = fh * HF + fi
                                        MM(ps[b][:, 0:w], slot[:, fi, cj * 128:(cj + 1) * 128], hid[:, f, a - g0:a - g0 + w], fi == 0, fi == HF - 1,
                                           [sk, ("ff_hid", f)], [PSK[b]])
                                    av = acc[:, c, a - g0:a - g0 + w]
                                    ak = ("ff_acc", c, a)
                                    if comb is None:
                                        if first:
                                            CP(av, ps[b][:, 0:w], [PSK[b]], [ak], eng="act")
                                        else:
                                            TT(av, av, ps[b][:, 0:w], ALU.add, [ak, PSK[b]], [ak])
                                    elif first:
                                        TT(av, ps[b][:, 0:w], cbt[:, a - g0:a - g0 + w], ALU.mult, [PSK[b], "ff_cb"], [ak])
                                    else:
                                        t_, tk_ = tmp.next()
                                        TT(t_[:, 0:w], ps[b][:, 0:w], cbt[:, a - g0:a - g0 + w], ALU.mult, [PSK[b], "ff_cb"], [tk_])
                                        TT(av, av, t_[:, 0:w], ALU.add, [ak, tk_], [ak])
                P.barrier()
                tiles = list(range(g0 // 128, (g0 + gw) // 128))
                mb = {}
                for r in sorted(set(1 if t < NTC else 0 for t in tiles)):
                    mb[r] = ev[5 + r]
                    bload(mb[r], L, 0 if r == 0 else 1, 5)
                gT, bT = ev[3], ev[4]
                bload_vec(gT, ln_g[L, 1])
                bload_vec(bT, ln_b[L, 1])
                for ti, t in enumerate(tiles):
                    r = 1 if t < NTC else 0
                    a512 = g0 + ((t * 128 - g0) // 512) * 512
                    eb = 0 if ti % 2 == 0 else 7
                    xt, xk = ev[eb], ev[eb].name
                    P.dma("sp", xt[:], xsrc[t * 128:(t + 1) * 128, :], [("xdst", L, 0, t)], [xk])
                    y, yk = ev[eb + 1], ev[eb + 1].name
                    for hlf in range(2):
                        bq = (6 if ti % 2 == 0 else 4) + hlf
                        for cq in range(4):
                            c = hlf * 4 + cq
                            TR(ps[bq][:, cq * 128:(cq + 1) * 128], acc[:, c, t * 128 - g0:(t + 1) * 128 - g0], idf[:],
                               [("ff_acc", c, a512), "idf"], [PSK[bq]])
                        TT(y[:, hlf * 512:(hlf + 1) * 512], ps[bq][:, :], mb[r][:, hlf * 512:(hlf + 1) * 512], ALU.mult,
                           [PSK[bq], mb[r].name], [yk])
                    STT(y[:], xt[:], ALU_ALPHA, y[:], ALU.mult, ALU.add, [xk, yk], [yk])
                    x2, x2k = ev[eb + 2], ev[eb + 2].name
                    layernorm_tile(y, yk, gT, bT, x2[:], x2k, (st, mv, rstd), "ff_small")
                    if final:
                        P.dma("sp", out_d[(t - NTC) * 128:(t - NTC + 1) * 128, :], x2[:], [x2k], [("out", t)], cls="st")
                    else:
                        P.dma("sp", xdst[t * 128:(t + 1) * 128, :], x2[:], [x2k], [("xsrc", L + 1, t)], cls="st")
                P.barrier()

        def p1_phase(es, L, hT, xsrc_fn, idx=(0, 1), t0=0, rkey=None, route_cfg=None):
            mbs = {}
            for r in range(2):
                if r == 1 and t0 >= NTC:
                    continue
                for i in idx:
                    tl = sbt(es, "p1_m%d_%d" % (i, r), [128, D], F32)
                    bload(tl, L, r, i)
                    mbs[(i, r)] = tl
            xr = Ring([sbt(es, "p1_x%d" % i, [128, D], F32) for i in range(2)], "p1x")
            wk = Ring([sbt(es, "p1_h%d" % i, [128, D], F32) for i in range(2)], "p1h")
            for t in range(t0, NT):
                r = 1 if t < NTC else 0
                xt, xk = xr.next()
                P.dma("sp", xt[:], xsrc_fn(t), [rkey(t) if rkey else ("xsrc", L, t)], [xk])
                mod_transpose(xt[:], xk, mbs[(idx[1], r)], mbs[(idx[0], r)], hT, t, wk, (2 * (t % 2), 2 * (t % 2) + 1), logits=route_cfg)

        def xsrc0(t):
            return ctx_in[t * 128:(t + 1) * 128, :] if t < NTC else x_in[(t - NTC) * 128:(t - NTC + 1) * 128, :]

        upto = None
        if debug is not None:
            for d_ in debug:
                if d_.startswith("upto:"):
                    upto = d_[5:]

        ORDER = ["inproj", "attn", "gla", "outproj", "l0", "l1inproj", "mla", "hgrn", "l1out", "route", "all"]
        lvl = ORDER.index(upto) if upto else len(ORDER) - 1

        SL = 99
        if debug is not None:
            for d_ in debug:
                if d_.startswith("scan:"):
                    SL = int(d_[5:])

        def sstop(k):
            if SL < k:
                P.mute = True

        def stage(ph):
            if lvl < ORDER.index(ph):
                P.mute = True

        def inproj(hT, fm, tm):
            P.barrier()
            with ExitStack() as e2:
                wr = Ring([sbt(e2, "ipw%d" % i, [128, 8, 512], BF16) for i in range(3)], "ipw")
                stg = Ring([sbt(e2, "ipstg%d" % i, [128, 512], F32) for i in range(3)], "ipstg")
                bctr = [0]
                banks = [4, 5, 6, 7]
                for (wap, c0, n, rc0, cw) in fm:
                    slot, sk = stream_w(wr, wsrc(wap, c0, n))
                    for j in range(n // cw):
                        proj_fm(hT, T, slot, sk, j * cw, cw, (rc0 + j) * 128, stg, banks, bctr)
                for j, (wap, c0) in enumerate(tm):
                    slot, sk = stream_w(wr, wsrc(wap, c0, 512))
                    proj_tm(hT, range(NT), slot, sk, 512, j * 512, stg, banks, bctr)

        def rope_prep(ea, ld, cs, items, a, w):
            P.dma("sp", cs[0][:, 0:w], cos_d[:, a:a + w], (), ["cs0"])
            P.dma("sp", cs[1][:, 0:w], sin_d[:, a:a + w], (), ["cs1"])
            for (dst, rows, rcq, rcs) in items:
                l1, k1 = ld.next()
                l2, k2 = ld.next()
                P.dma("sp", l1[0:rows, 0:w], pfm[rcq * 128:rcq * 128 + rows, a:a + w], tkeys(("pfm", rcq), a, w), [k1])
                P.dma("sp", l2[0:rows, 0:w], pfm[rcs * 128:rcs * 128 + rows, a:a + w], tkeys(("pfm", rcs), a, w), [k2])
                TT(l1[0:rows, 0:w], l1[0:rows, 0:w], cs[0][0:rows, 0:w], ALU.mult, [k1, "cs0"], [k1])
                TT(l2[0:rows, 0:w], l2[0:rows, 0:w], cs[1][0:rows, 0:w], ALU.mult, [k2, "cs1"], [k2])
                TT(dst, l1[0:rows, 0:w], l2[0:rows, 0:w], ALU.add, [k1, k2], ["ropeout"])

        def vaug_prep(Vaug, ld, col0):
            MEMSET(Vaug[:], 1.0, (), [("Vaug", t) for t in range(NT)])
            for t in range(NT):
                l, lk = ld.next()
                P.dma("sp", l[:], ptm[t * 128:(t + 1) * 128, col0:col0 + 512], [("ptm", col0 // 512, t)], [lk])
                CP(Vaug[:, t, :, 0:128], l[:].rearrange("p (h v) -> p h v", v=128), [lk], [("Vaug", t)], eng="act")

        LATQ = [(CTX + 512 * g, 512, list(range(NT))) for g in range(4)]

        with ExitStack() as l0:
            mixT = sbt(l0, "mixT", [128, 8, T], BF16)
            with ExitStack() as es:
                hT = sbt(es, "hT", [128, 8, T], BF16)
                P.barrier()
                with ExitStack() as e1:
                    p1_phase(e1, 0, hT, xsrc0)
                fm = [(ev_w_in, 0, 512, 0, 128), (ev_w_in_sw, 0, 512, 4, 128), (ev_w_in, 512, 512, 8, 128),
                      (ev_w_in_sw, 512, 512, 12, 128), (ev_w_in, 1536, 512, 16, 128), (ev_w_in, 3072, 32, 20, 16)]
                inproj(hT, fm, [(ev_w_in, 1024), (ev_w_in, 2048), (ev_w_in, 2560)])
            dump("inproj", pfm[0:128, :], tkeys(("pfm", 0), 0, T))
            stage("attn")
            P.barrier()
            with ExitStack() as ea:
                qT = sbt(ea, "qT", [128, 4, T], BF16)
                kT = sbt(ea, "kT", [128, 4, T], BF16)
                Vaug = sbt(ea, "Vaug", [128, NT, 4, 130], BF16)
                ld = Ring([sbt(ea, "a0ld%d" % i, [128, 512], F32) for i in range(6)], "a0ld")
                cs = [sbt(ea, "a0cos", [128, 512], F32), sbt(ea, "a0sin", [128, 512], F32)]
                for (a, w) in tgroups(0, T):
                    items = []
                    for h in range(4):
                        items.append((qT[:, h, a:a + w], 128, h, 4 + h))
                        items.append((kT[:, h, a:a + w], 128, 8 + h, 12 + h))
                    rope_prep(ea, ld, cs, items, a, w)
                vaug_prep(Vaug, ld, 0)
                lv = sbt(ea, "lv", [1, 256], F32)
                lp = sbt(ea, "lp", [1, 128], F32)
                ls = sbt(ea, "ls", [1, 2], F32)
                lam2 = sbt(ea, "lam2", [1, 2], F32)
                nlam = sbt(ea, "nlam", [128, 2], F32)
                P.dma("sp", lv[:], ev_lam, (), ["lv"], cls="m")
                TT(lp[:, 0:64], lv[:, 0:64], lv[:, 64:128], ALU.mult, ["lv"], ["lp"])
                TT(lp[:, 64:128], lv[:, 128:192], lv[:, 192:256], ALU.mult, ["lv"], ["lp"])
                P.op("dve", lambda e: e.reduce_sum(ls[:], lp[:].rearrange("p (a b) -> p a b", b=64), AX.X), ["lp"], ["ls"])
                ACT(ls[:], ls[:], AF.Exp, ["ls"], ["ls"])
                TT(lam2[:, 0:1], ls[:, 1:2], ls[:, 0:1], ALU.subtract, ["ls"], ["lam2"])
                TS(lam2[:, 0:1], lam2[:, 0:1], -0.2, None, ALU.add, None, ["lam2"], ["lam2"])
                CP(lam2[:, 1:2], lam2[:, 0:1], ["lam2"], ["lam2"])
                MM(ps[7][:, 0:2], ones_f[0:1, :], lam2[0:1, 0:2], True, True, ["ones_f", "lam2"], [PSK[7]])
                CP(nlam[:], ps[7][:, 0:2], [PSK[7]], ["nlam"])
                gsub = sbt(ea, "gsub", [128, 128], F32)
                bload_vec(gsub, ev_subln_g[0])
                TS(gsub[:], gsub[:], 0.8, None, ALU.mult, None, [gsub.name], [gsub.name])
                o1n = sbt(ea, "o1n", [128, 4, 128], F32)
                r1 = sbt(ea, "a0r1", [128, 1], F32)
                ss = sbt(ea, "a0ss", [128, 1], F32)
                ofr = Ring([sbt(ea, "a0of%d" % i, [128, 128], F32) for i in range(2)], "a0of")
                junk = sbt(ea, "a0junk", [128, 128], F32)
                dump("qT0", qT[:, 0, :], ["ropeout"], eng="pool")
                for h in range(4 if not (debug and "noattn" in debug) else 0):
                    def finish(mi, s, q0, acc, acck, h=h):
                        t = (q0 + s * 128) // 128
                        if mi == 0:
                            RECIP(r1[:], acc[:, 128:129], [acck], ["a0r"])
                            TS(o1n[:, s, :], acc[:, 0:128], r1[:, 0:1], None, ALU.mult, None, [acck, "a0r"], [("o1n", s)])
                            return
                        RECIP(r1[:], acc[:, 128:129], [acck], ["a0r"])
                        TT(r1[:], r1[:], nlam[:, 0:1], ALU.mult, ["a0r", "nlam"], ["a0r"])
                        of, ofk = ofr.next()
                        STT(of[:], acc[:, 0:128], r1[:, 0:1], o1n[:, s, :], ALU.mult, ALU.add, [acck, "a0r", ("o1n", s)], [ofk])
                        TT(junk[:], of[:], of[:], ALU.mult, [ofk], ["a0junk"])
                        P.op("dve", lambda e: e.reduce_sum(ss[:], junk[:], AX.X), ["a0junk"], ["a0ss"])
                        ACT(ss[:], ss[:], AF.Ln, ["a0ss"], ["a0ss"], bias=eps_rms[:], scale=1.0 / 128)
                        ACT(ss[:], ss[:], AF.Exp, ["a0ss"], ["a0ss"], scale=-0.5)
                        STT(of[:], of[:], ss[:, 0:1], gsub[:], ALU.mult, ALU.mult, [ofk, "a0ss", gsub.name], [ofk])
                        TR(ps[7][:, 0:128], of[:], idf[:], [ofk, "idf"], [PSK[7]])
                        CP(mixT[:, h, t * 128:(t + 1) * 128], ps[7][:, 0:128], [PSK[7]], [("mixT", 0, t)])
                    maps = []
                    for m in range(2):
                        pb = 64 * m
                        maps.append([(lambda kt, pb=pb, h=h: kT[pb:pb + 64, h, kt * 128:(kt + 1) * 128],
                                      lambda q0, qw, pb=pb, h=h: qT[pb:pb + 64, h, q0:q0 + qw], ["ropeout"])])
                    qr_ = [(0, CTX, list(range(NTC)))] + LATQ
                    attention(ea, maps, lambda kt, h=h: Vaug[:, kt, h, :], qr_, 0.125, finish, [0, 1, 2], [[3, 4], [5, 6]], tag="a%d" % h)
            stage("gla")
            P.barrier()
            with ExitStack() as eg:
                lrf = sbt(eg, "g_lrf", [16, 2, T], F32)
                lrT = sbt(eg, "g_lrT", [16, 2, T], BF16)
                gkf = sbt(eg, "g_gkf", [16, 2, 256], F32)
                gkw = sbt(eg, "g_gkw", [16, 2, 256], BF16)
                nb = sbt(eg, "g_nb", [128, 4], F32)
                for d in range(2):
                    P.dma("sp", lrf[:, d, :], pfm[(20 + d) * 128:(20 + d) * 128 + 16, :], tkeys(("pfm", 20 + d), 0, T), ["g_lrf"])
                    P.dma("sp", gkf[:, d, :], ev_gk_w2[d], (), ["g_gkf"], cls="m")
                    for cc in range(2):
                        col_load(nb[:, d * 2 + cc:d * 2 + cc + 1], ev_gk_b[d, cc * 128:(cc + 1) * 128], ["g_nb"])
                CP(lrT[:], lrf[:], ["g_lrf"], ["g_lrT"])
                CP(gkw[:], gkf[:], ["g_gkf"], ["g_gkw"])
                TS(nb[:], nb[:], -1.0, None, ALU.mult, None, ["g_nb"], ["g_nb"])

                def load_q(cc, qf):
                    P.dma("sp", qf[:], pfm[(16 + cc) * 128:(17 + cc) * 128, :], tkeys(("pfm", 16 + cc), 0, T), ["sc_qf"])
                    TS(qf[:], qf[:], 0.125, None, ALU.mult, None, ["sc_qf"], ["sc_qf"])

                def make_ng_k(cc, d, ng, kf, ee):
                    P.dma("sp", kf[:], pfm[(18 + cc) * 128:(19 + cc) * 128, :], tkeys(("pfm", 18 + cc), 0, T), ["sc_kf"])
                    for gi, (a, w) in enumerate(tgroups(0, T)):
                        b = gi % 2
                        MM(ps[b][:, 0:w], gkw[:, d, cc * 128:(cc + 1) * 128], lrT[:, d, a:a + w], True, True, ["g_gkw", "g_lrT"], [PSK[b]])
                        ACT(ee[:, a:a + w], ps[b][:, 0:w], AF.Exp, [PSK[b], "g_nb"], ["sc_ee"], bias=nb[:, d * 2 + cc:d * 2 + cc + 1], scale=-1.0)
                    ACT(ng[:], ee[:], AF.Ln, ["sc_ee"], ["sc_ng"], bias=ones_f[:, 0:1], scale=1.0)

                cfg = dict(nch=2, hpc=2, dk=64, C=128, gsc=1.0 / 16, out_t0=0, vcol=512, gcol=1024, norm_g=ev_gla_g[0],
                           load_q=load_q, make_ng_k=make_ng_k)
                scan_phase(eg, cfg, mixT)
            dump("mixT0", mixT[:, 0, :], [("mixT", 0, t) for t in range(NT)], eng="pool")
            dump("mixT4", mixT[:, 4, :], [("mixT", 1, t) for t in range(NT)], eng="pool")
            stage("outproj")
            P.barrier()
            with ExitStack() as eo:
                outproj_phase(eo, 0, mixT, None, ev_w_out, xsrc0, xa, 0)
            dump("x1", xa[CTX:CTX + 128, :], [("xdst", 0, 0, NTC)])
        stage("l0")
        P.barrier()
        with ExitStack() as es:
            hT = sbt(es, "hTb", [128, 8, T], BF16)
            with ExitStack() as e1:
                p1_phase(e1, 0, hT, lambda t: xa[t * 128:(t + 1) * 128, :], idx=(3, 4), rkey=lambda t: ("xdst", 0, 0, t))
            P.barrier()
            with ExitStack() as ef:
                ffn_phase(ef, 0, hT, [(ev_w1, ev_w2)], [(0, 768), (768, 768), (1536, 768)], xa, xb)
        dump("x2", xb[CTX:CTX + 128, :], [("xsrc", 1, NTC)])

        stage("l1inproj")
        if True:
          with ExitStack() as l1:
            mixT = sbt(l1, "mixT1", [128, 8, T], BF16)
            P.barrier()
            with ExitStack() as es:
                hT = sbt(es, "hT1", [128, 8, T], BF16)
                with ExitStack() as e1:
                    p1_phase(e1, 1, hT, lambda t: xb[t * 128:(t + 1) * 128, :])
                fm = [(od_w_in, 0, 256, 0, 128), (od_w_in, 256, 128, 2, 128), (od_w_in, 384, 64, 3, 64), (od_kr_sw, 0, 64, 4, 64),
                      (od_w_in, 448, 512, 5, 128), (od_w_in, 960, 512, 9, 128), (od_w_in, 1472, 512, 13, 128)]
                inproj(hT, fm, [(od_w_in, 1984), (od_w_in, 2496)])
            stage("mla")
            P.barrier()
            with ExitStack() as em:
                qn = sbt(em, "m_qn", [128, 4, T], BF16)
                qr = sbt(em, "m_qr", [128, 4, T], BF16)
                kn = sbt(em, "m_kn", [128, 4, T], BF16)
                kr = sbt(em, "m_kr", [128, T], BF16)
                MEMSET(qr[64:128, :, :], 0.0, (), ["m_qpad"])
                MEMSET(kr[64:128, :], 0.0, (), ["m_kpad"])
                Vaug = sbt(em, "m_Vaug", [128, NT, 4, 130], BF16)
                cqn = sbt(em, "m_cqn", [128, 2, T], BF16)
                ckvn = sbt(em, "m_ckvn", [128, T], BF16)
                wuq = sbt(em, "m_wuq", [128, 2, 768], BF16)
                wuqs = sbt(em, "m_wuqs", [128, 2, 256], BF16)
                wukv = sbt(em, "m_wukv", [128, 1024], BF16)
                gcol = sbt(em, "m_gcol", [128, 3], F32)
                P.dma("pool", wuq[:], od_w_uq.rearrange("(k p) n -> p k n", p=128), (), ["m_wuq"], cls="w")
                P.dma("pool", wuqs[:], od_w_uq_sw.rearrange("(k p) n -> p k n", p=128), (), ["m_wuqs"], cls="w")
                P.dma("pool", wukv[:], od_w_ukv, (), ["m_wukv"], cls="w")
                for c in range(2):
                    col_load(gcol[:, c:c + 1], od_qg[c * 128:(c + 1) * 128], ["m_gcol"])
                col_load(gcol[:, 2:3], od_kvg, ["m_gcol"])
                ld = Ring([sbt(em, "m_ld%d" % i, [128, 512], F32) for i in range(6)], "m_ld")
                cs = [sbt(em, "m_cos", [128, 512], F32), sbt(em, "m_sin", [128, 512], F32)]
                rs = sbt(em, "m_rs", [128, 512], F32)
                for gi, (a, w) in enumerate(tgroups(0, T)):
                    for (rcs, nrm, dst, gc0) in (((0, 1), 256.0, lambda c: cqn[:, c, a:a + w], 0), ((2,), 128.0, lambda c: ckvn[:, a:a + w], 2)):
                        lt = []
                        b = gi % 2
                        for ci, rc in enumerate(rcs):
                            l, lk = ld.next()
                            s2, s2k = ld.next()
                            P.dma("sp", l[:, 0:w], pfm[rc * 128:(rc + 1) * 128, a:a + w], tkeys(("pfm", rc), a, w), [lk])
                            TT(s2[:, 0:w], l[:, 0:w], l[:, 0:w], ALU.mult, [lk], [s2k])
                            MM(ps[b][:, 0:w], ones_f[:], s2[:, 0:w], ci == 0, ci == len(rcs) - 1, ["ones_f", s2k], [PSK[b]])
                            lt.append((l, lk))
                        ACT(rs[:, 0:w], ps[b][:, 0:w], AF.Ln, [PSK[b]], ["m_rs"], bias=eps_rms[:], scale=1.0 / nrm)
                        ACT(rs[:, 0:w], rs[:, 0:w], AF.Exp, ["m_rs"], ["m_rs"], scale=-0.5)
                        for ci, (l, lk) in enumerate(lt):
                            STT(dst(ci), l[:, 0:w], gcol[:, gc0 + ci:gc0 + ci + 1], rs[:, 0:w], ALU.mult, ALU.mult,
                                [lk, "m_gcol", "m_rs"], tkeys("m_cn%d" % gc0, a, w))
                    rope_prep(em, ld, cs, [(kr[0:64, a:a + w], 64, 3, 4)], a, w)
                    for h in range(4):
                        b = 2 + (h % 2)
                        for c in range(2):
                            MM(ps[b][:, 0:w], wuq[:, c, h * 192:h * 192 + 128], cqn[:, c, a:a + w], c == 0, c == 1,
                               ["m_wuq"] + tkeys("m_cn0", a, w), [PSK[b]])
                        CP(qn[:, h, a:a + w], ps[b][:, 0:w], [PSK[b]], ["m_q"], eng="act")
                        MM(ps[b][:, 0:w], wukv[:, h * 256:h * 256 + 128], ckvn[:, a:a + w], True, True,
                           ["m_wukv"] + tkeys("m_cn2", a, w), [PSK[b]])
                        CP(kn[:, h, a:a + w], ps[b][:, 0:w], [PSK[b]], ["m_k"], eng="act")
                        for c in range(2):
                            MM(ps[4][0:64, 0:w], wuq[:, c, h * 192 + 128:h * 192 + 192], cqn[:, c, a:a + w], c == 0, c == 1,
                               ["m_wuq"] + tkeys("m_cn0", a, w), [PSK[4]])
                        for c in range(2):
                            MM(ps[5][0:64, 0:w], wuqs[:, c, h * 64:(h + 1) * 64], cqn[:, c, a:a + w], c == 0, c == 1,
                               ["m_wuqs"] + tkeys("m_cn0", a, w), [PSK[5]])
                        l1, k1 = ld.next()
                        l2, k2 = ld.next()
                        TT(l1[0:64, 0:w], ps[4][0:64, 0:w], cs[0][0:64, 0:w], ALU.mult, [PSK[4], "cs0"], [k1])
                        TT(l2[0:64, 0:w], ps[5][0:64, 0:w], cs[1][0:64, 0:w], ALU.mult, [PSK[5], "cs1"], [k2])
                        TT(qr[0:64, h, a:a + w], l1[0:64, 0:w], l2[0:64, 0:w], ALU.add, [k1, k2], ["m_q"])
                MEMSET(Vaug[:], 1.0, (), [("Vaug", t) for t in range(NT)])
                for t in range(NT):
                    b = 6 + (t % 2)
                    for h in range(4):
                        MM(ps[b][:, h * 128:(h + 1) * 128], ckvn[:, t * 128:(t + 1) * 128], wukv[:, h * 256 + 128:h * 256 + 256], h == 0, True,
                           ["m_wukv", ("m_cn2", t)], [PSK[b]])
                    CP(Vaug[:, t, :, 0:128], ps[b][:].rearrange("p (h v) -> p h v", v=128), [PSK[b]], [("Vaug", t)], eng="act")
                r1 = sbt(em, "m_r1", [128, 1], F32)
                ofr = Ring([sbt(em, "m_of%d" % i, [128, 128], F32) for i in range(2)], "m_of")
                for h in range(4):
                    def finish(mi, s, q0, acc, acck, h=h):
                        t = (q0 + s * 128) // 128
                        RECIP(r1[:], acc[:, 128:129], [acck], ["m_r"])
                        of, ofk = ofr.next()
                        TS(of[:], acc[:, 0:128], r1[:, 0:1], None, ALU.mult, None, [acck, "m_r"], [ofk])
                        TR(ps[7][:, 0:128], of[:], idf[:], [ofk, "idf"], [PSK[7]])
                        CP(mixT[:, h, t * 128:(t + 1) * 128], ps[7][:, 0:128], [PSK[7]], [("mixT", 0, t)])
                    maps = [[(lambda kt, h=h: kn[:, h, kt * 128:(kt + 1) * 128], lambda q0, qw, h=h: qn[:, h, q0:q0 + qw], ["m_k", "m_q"]),
                             (lambda kt: kr[:, kt * 128:(kt + 1) * 128], lambda q0, qw, h=h: qr[:, h, q0:q0 + qw], ["ropeout", "m_q", "m_qpad", "m_kpad"])]]
                    attention(em, maps, lambda kt, h=h: Vaug[:, kt, h, :], LATQ, 192.0 ** -0.5, finish, [0, 1, 2], [[3, 4], [5, 6]], tag="m%d" % h)
            stage("hgrn")
            P.barrier()
            with ExitStack() as eg:
                lbt = sbt(eg, "h_lbt", [128, 2, 4], F32)
                lbc = sbt(eg, "h_lbc", [128, 4], F32)
                oml = sbt(eg, "h_oml", [128, 4], F32)
                for l_ in range(2):
                    for cc in range(4):
                        col_load(lbt[:, l_, cc:cc + 1], lb_table[l_, cc * 128:(cc + 1) * 128], ["h_lbt"])
                TT(lbc[:], lbt[:, 0, :], lbt[:, 1, :], ALU.subtract, ["h_lbt"], ["h_lb"])
                ACT(lbc[:], lbc[:], AF.Exp, ["h_lb"], ["h_lb"])
                TS(lbc[:], lbc[:], 1.0, None, ALU.add, None, ["h_lb"], ["h_lb"])
                RECIP(lbc[:], lbc[:], ["h_lb"], ["h_lb"])
                TS(oml[:], lbc[:], -1.0, 1.0, ALU.mult, ALU.add, ["h_lb"], ["h_oml"])

                def load_q(cc, qf):
                    P.dma("sp", qf[:], pfm[(5 + cc) * 128:(6 + cc) * 128, :], tkeys(("pfm", 5 + cc), 0, T), ["sc_qf"])

                def make_ng_k(cc, d, ng, kf, ee):
                    rc = 9 + 4 * d + cc
                    P.dma("sp", ng[:], pfm[rc * 128:(rc + 1) * 128, :], tkeys(("pfm", rc), 0, T), ["sc_ng"])
                    ACT(ee[:], ng[:], AF.Exp, ["sc_ng"], ["sc_ee"], scale=-1.0)
                    ACT(ee[:], ee[:], AF.Ln, ["sc_ee"], ["sc_ee"], bias=ones_f[:, 0:1], scale=1.0)
                    ACT(ee[:], ee[:], AF.Exp, ["sc_ee"], ["sc_ee"], scale=-1.0)
                    TS(ee[:], ee[:], oml[:, cc:cc + 1], lbc[:, cc:cc + 1], ALU.mult, ALU.add, ["sc_ee", "h_lb", "h_oml"], ["sc_ee"])
                    TS(kf[:], ee[:], -1.0, 1.0, ALU.mult, ALU.add, ["sc_ee"], ["sc_kf"])
                    ACT(ng[:], ee[:], AF.Ln, ["sc_ee"], ["sc_ng"])
                    TS(ng[:], ng[:], -1.0, None, ALU.mult, None, ["sc_ng"], ["sc_ng"])

                cfg = dict(nch=4, hpc=1, dk=128, C=64, gsc=1.0, out_t0=NTC, vcol=0, gcol=512, norm_g=od_hg_g[0],
                           load_q=load_q, make_ng_k=make_ng_k)
                scan_phase(eg, cfg, mixT)
            dump("l1mixT0", mixT[:, 0, :], [("mixT", 0, t) for t in range(NTC, NT)], eng="pool")
            dump("l1mixT4", mixT[:, 4, :], [("mixT", 1, t) for t in range(NTC, NT)], eng="pool")
            stage("l1out")
            P.barrier()
            with ExitStack() as eo:
                outproj_phase(eo, 1, mixT, None, od_w_out, lambda t: xb[t * 128:(t + 1) * 128, :], xc, NTC)
            dump("x3", xc[CTX:CTX + 128, :], [("xdst", 1, 0, NTC)])
          stage("route")
          P.barrier()
          with ExitStack() as es:
                hT = sbt(es, "hT1b", [128, 8, T], BF16)
                combt = sbt(es, "r_comb", [128, NT - NTC, NE], F32)
                comb_d = dscr("comb_d", [NE, S])
                with ExitStack() as eo:
                    combT = sbt(eo, "r_combT", [128, S], F32)
                    MEMSET(combT[:], 0.0, (), [("combT", tt) for tt in range(NTC, NT)])
                    h2T = sbt(eo, "r_h2T", [128, 8, 128], F32)
                    wrt = sbt(eo, "r_wrt", [128, 8, NE], F32)
                    P.dma("sp", wrt[:], od_router.rearrange("(k p) e -> p k e", p=128), (), ["wrt"], cls="m")
                    comb = dict(comb=combt, lg=sbt(eo, "r_lg", [128, NE], F32), m8=sbt(eo, "r_m8", [128, 8], F32),
                                msk=sbt(eo, "r_msk", [128, NE], F32), ex=sbt(eo, "r_ex", [128, NE], F32), ssum=sbt(eo, "r_ss", [128, 1], F32))
                    p1_phase(eo, 1, hT, lambda t: xc[t * 128:(t + 1) * 128, :], idx=(3, 4), t0=NTC, rkey=lambda t: ("xdst", 1, 0, t),
                             route_cfg=(h2T, wrt, comb, 6))
                    cpad = sbt(eo, "r_cpad", [128, 128], F32)
                    MEMSET(cpad[:], 0.0, (), ["cpad"])
                    for t in range(NTC, NT):
                        i = t - NTC
                        b = 4 + i % 2
                        CP(cpad[:, 0:NE], combt[:, i, :], [("comb", t), "cpad"], ["cpad"])
                        MM(ps[b][:, 0:128], cpad[:], idf[:], True, True, ["cpad", "idf"], [PSK[b]])
                        CP(combT[0:NE, i * 128:(i + 1) * 128], ps[b][0:NE, 0:128], [PSK[b]], [("combT", t)])
                    P.dma("sp", comb_d, combT[0:NE, :], [("combT", tt) for tt in range(NTC, NT)], ["comb_d"], cls="st")
                dump("comb", combt[:].rearrange("p t e -> p (t e)"), [("comb", t) for t in range(NTC, NT)])
                stage("all")
                P.barrier()
                with ExitStack() as ef:
                    ffn_phase(ef, 1, hT, [(od_w1[e], od_w2[e]) for e in range(NE)], [(CTX, 1024), (CTX + 1024, 1024)], xc, None,
                              comb=dict(comb_d=comb_d), final=True)
          done_keys = [("out", t) for t in range(NTC, NT)]
        P.emit(done_keys + dbg_keys + [("mrow_d", 1)])
        free_ps01()
    return nc, P


def _host_consts():
    ident = np.eye(128, dtype=np.float32)
    n_freq = 16
    inv = (10000.0 ** (-np.arange(n_freq, dtype=np.float32) / n_freq)).astype(np.float32)
    rows = S // 64
    row = np.repeat(np.arange(rows, dtype=np.float32), 64)
    col = np.tile(np.arange(64, dtype=np.float32), rows)
    ang = np.concatenate([row[:, None] * inv, col[:, None] * inv], axis=-1).astype(np.float32)
    cos = np.cos(ang).astype(np.float32)
    sin = np.sin(ang).astype(np.float32)
    cosT = np.ones((128, T), np.float32)
    sinT = np.zeros((128, T), np.float32)
    for p in range(128):
        i = (p % 64) // 2
        cosT[p, CTX:] = cos[:, i]
        sinT[p, CTX:] = -sin[:, i] if p % 2 == 0 else sin[:, i]
    j = np.arange(128)[:, None]
    i = np.arange(128)[None, :]
    mF = (j <= i).astype(np.float32)
    mB = (j >= i).astype(np.float32)
    m64F = np.zeros((128, 128), np.float32)
    m64B = np.zeros((128, 128), np.float32)
    jj = (np.arange(128) % 64)[:, None]
    ii = np.arange(64)[None, :]
    m64F[:, :64] = (jj <= ii)
    m64B[:, :64] = (jj >= ii)
    masks = np.stack([mF, mB, m64F, m64B]).astype(np.float32)
    return ident, cosT, sinT, masks


def _prep_inputs(inp):
    f = lambda a: np.ascontiguousarray(np.asarray(a, dtype=np.float32))
    ident, cosT, sinT, masks = _host_consts()
    sw = np.arange(1024) ^ 1
    ev_w_in = f(inp["ev_w_in"][0])
    od_w_in = f(inp["od_w_in"][0])
    od_w_uq = f(inp["od_w_uq"][0])
    sw64 = np.arange(64) ^ 1
    uq_sw = np.concatenate([od_w_uq[:, h * 192 + 128:h * 192 + 192][:, sw64] for h in range(4)], axis=1)
    shared = {
        "ada_w": f(inp["ada_w"]), "ada_b": f(inp["ada_b"]), "post_ln_g": f(inp["post_ln_g"]), "post_ln_b": f(inp["post_ln_b"]),
        "lb_table": f(inp["lb_table"]), "ev_w_in": ev_w_in, "ev_w_in_sw": f(ev_w_in[:, :1024][:, sw]),
        "ev_lam": f(inp["ev_lam"][0].reshape(1, 256)), "ev_subln_g": f(inp["ev_subln_g"]), "ev_gk_w2": f(inp["ev_gk_w2"][0]),
        "ev_gk_b": f(inp["ev_gk_b"][0]), "ev_gla_norm_g": f(inp["ev_gla_norm_g"]), "ev_w_out": f(inp["ev_w_out"][0]),
        "ev_ffn_w1": f(inp["ev_ffn_w1"][0]), "ev_ffn_w2": f(inp["ev_ffn_w2"][0]), "od_w_in": od_w_in,
        "od_kr_sw": f(od_w_in[:, 384:448][:, sw64]), "od_q_norm_g": f(inp["od_q_norm_g"][0]), "od_kv_norm_g": f(inp["od_kv_norm_g"][0]),
        "od_w_uq": od_w_uq, "od_w_uq_sw": f(uq_sw), "od_w_ukv": f(inp["od_w_ukv"][0]), "od_hg_norm_g": f(inp["od_hg_norm_g"]),
        "od_w_out": f(inp["od_w_out"][0]), "od_router": f(inp["od_router"][0]), "od_exp_w1": f(inp["od_exp_w1"][0]),
        "od_exp_w2": f(inp["od_exp_w2"][0]), "ident": ident, "cosT": cosT, "sinT": sinT, "masks": masks,
    }
    maps = []
    for b in range(8):
        m = dict(shared)
        m["x"] = f(inp["x"][b])
        m["ctx"] = f(inp["ctx"][b])
        m["c2"] = f(np.stack([inp["c"][b], inp["c_ctx"]]))
        maps.append(m)
    return maps


def kernel(**inputs):
    nc, P = build()
    maps = _prep_inputs(inputs)
    res = run_bass_kernel_spmd(nc, maps, core_ids=list(range(8)))
    return np.stack([np.asarray(r["out"], dtype=np.float32) for r in res.results], axis=0)
```

```python
import bisect
import math
from contextlib import ExitStack

import numpy as np
import concourse.bass as bass
import concourse.mybir as mybir
from concourse.bass_utils import run_bass_kernel_spmd

F32 = mybir.dt.float32
BF16 = mybir.dt.bfloat16
AF = mybir.ActivationFunctionType
ALU = mybir.AluOpType
AX = mybir.AxisListType

ENGS = ("pe", "act", "dve", "pool", "sp")
SAME_ENG_SYNC = {"pe": False, "act": True, "dve": True, "pool": True, "sp": False}

D = 1024
S = 2048
CTX = 256
T = S + CTX
NT = T // 128
NTC = CTX // 128
DFF = 3584
NFF = DFF // 128
NE = 8
ALPHA = 4 ** 0.25
LN_EPS = 1e-5
RMS_EPS = 1e-6


class Prog:
    def __init__(self, nc):
        self.nc = nc
        self.ops = []
        self.state = {}
        self.cls = {"w": [0, 1, 2, 3], "ld": [4, 5, 6, 7], "st": [8, 9, 10, 11], "m": [12, 13]}
        self.rr = {k: 0 for k in self.cls}
        self.n_dma_sems = 14
        self.last_op = {}
        self.last_dma = {}
        self.cur_barrier = None
        self.mute = False

    def _rec(self, eng, fn, reads, writes, dma_sem=None):
        if self.mute:
            return -1
        oid = len(self.ops)
        deps = set()
        for k in reads:
            st = self.state.get(k)
            if st and st[0] is not None:
                deps.add(st[0])
        for k in writes:
            st = self.state.get(k)
            if st:
                if st[0] is not None:
                    deps.add(st[0])
                deps.update(st[1])
        if self.cur_barrier is not None:
            deps.add(self.cur_barrier)
        self.ops.append(dict(eng=eng, fn=fn, deps=deps, dma=dma_sem))
        if dma_sem is None:
            self.last_op[eng] = oid
        else:
            self.last_dma[dma_sem] = oid
        for k in reads:
            self.state.setdefault(k, [None, []])[1].append(oid)
        for k in writes:
            self.state[k] = [oid, []]
        return oid

    def op(self, eng, fn, reads=(), writes=()):
        return self._rec(eng, fn, tuple(reads), tuple(writes))

    def barrier(self):
        if self.mute:
            return -1
        deps = set(self.last_op.values()) | set(self.last_dma.values())
        if self.cur_barrier is not None:
            deps.add(self.cur_barrier)
        oid = len(self.ops)
        self.ops.append(dict(eng="sp", fn=lambda e: e.nop(), deps=deps, dma=None))
        self.last_op["sp"] = oid
        self.cur_barrier = oid
        return oid

    def dma(self, eng, out, in_, reads=(), writes=(), cls="ld"):
        lst = self.cls[cls]
        sem = lst[self.rr[cls] % len(lst)]
        self.rr[cls] += 1
        return self._rec(eng, lambda e, o=out, i=in_: e.dma_start(out=o, in_=i),
                         tuple(reads), tuple(writes), dma_sem=sem)

    def emit(self, final_keys):
        nc = self.nc
        ops = self.ops
        self.mute = False
        self.barrier()
        self._rec("sp", None, tuple(final_keys), ())
        needed = set()
        for o in ops:
            for d in o["deps"]:
                src = ops[d]
                if src["dma"] is None and src["eng"] == o["eng"] and not SAME_ENG_SYNC[o["eng"]]:
                    continue
                needed.add(d)
        cnt = {e: 0 for e in ENGS}
        dcnt = [0] * self.n_dma_sems
        dma_hist = [[] for _ in range(self.n_dma_sems)]
        for i, o in enumerate(ops):
            if o["dma"] is not None:
                s = o["dma"]
                dcnt[s] += 16
                o["val"] = dcnt[s]
                dma_hist[s].append((i, dcnt[s]))
            elif i in needed:
                cnt[o["eng"]] += 1
                o["val"] = cnt[o["eng"]]
        self.stats = dict(cnt=dict(cnt), dcnt=list(dcnt), nops=len(ops))
        per = {e: [] for e in ENGS}
        seen = {e: {} for e in ENGS}
        for i, o in enumerate(ops):
            e = o["eng"]
            req = {}
            for d in o["deps"]:
                src = ops[d]
                if src["dma"] is not None:
                    s = src["dma"]
                    hist = dma_hist[s]
                    j = bisect.bisect_left(hist, (i, -1)) - 1
                    v = hist[j][1]
                    key = ("d", s)
                else:
                    if src["eng"] == e and not SAME_ENG_SYNC[e]:
                        continue
                    v = src["val"]
                    key = ("e", src["eng"])
                if v > req.get(key, 0):
                    req[key] = v
            waits = []
            for key, v in req.items():
                if seen[e].get(key, 0) >= v:
                    continue
                seen[e][key] = v
                waits.append((key, v))
            per[e].append((i, o, waits))

        with ExitStack() as es:
            esem = {e: es.enter_context(nc.semaphore("s_" + e)) for e in ENGS}
            dsem = [es.enter_context(nc.semaphore("d_%d" % i)) for i in range(self.n_dma_sems)]
            block = es.enter_context(nc.Block())

            def run(engname):
                def body(eng):
                    for i, o, waits in per[engname]:
                        for key, v in waits:
                            sem = dsem[key[1]] if key[0] == "d" else esem[key[1]]
                            eng.wait_ge(sem, v)
                        if o["fn"] is None:
                            continue
                        ins = o["fn"](eng)
                        if o["dma"] is not None:
                            ins.then_inc(dsem[o["dma"]], 16)
                        elif i in needed:
                            ins.then_inc(esem[engname], 1)
                return body

            block.tensor(run("pe"))
            block.scalar(run("act"))
            block.vector(run("dve"))
            block.gpsimd(run("pool"))
            block.sync(run("sp"))


class Ring:
    def __init__(self, tiles, name):
        self.tiles, self.name, self.i = tiles, name, 0

    def next(self):
        j = self.i % len(self.tiles)
        self.i += 1
        return self.tiles[j], (self.name, j)


def tgroups(t0, t1, w=512):
    out = []
    a = t0
    while a < t1:
        b = min(a + w, t1)
        out.append((a, b - a))
        a = b
    return out


def tkeys(name, a, w):
    return [(name, t) for t in range(a // 128, (a + w + 127) // 128)]


def build(debug=None):
    nc = bass.Bass("TRN2", target_bir_lowering=False)
    P = Prog(nc)

    def din(name, shape):
        return nc.dram_tensor(name, list(shape), F32, kind="ExternalInput").ap()

    x_in = din("x", [S, D])
    ctx_in = din("ctx", [CTX, D])
    c2 = din("c2", [2, D])
    ada_w = din("ada_w", [2, D, 6 * D])
    ada_b = din("ada_b", [2, 6 * D])
    ln_g = din("post_ln_g", [2, 2, D])
    ln_b = din("post_ln_b", [2, 2, D])
    lb_table = din("lb_table", [2, 512])
    ev_w_in = din("ev_w_in", [D, 3104])
    ev_w_in_sw = din("ev_w_in_sw", [D, 1024])
    ev_lam = din("ev_lam", [1, 256])
    ev_subln_g = din("ev_subln_g", [1, 128])
    ev_gk_w2 = din("ev_gk_w2", [2, 16, 256])
    ev_gk_b = din("ev_gk_b", [2, 256])
    ev_gla_g = din("ev_gla_norm_g", [1, 128])
    ev_w_out = din("ev_w_out", [D, D])
    ev_w1 = din("ev_ffn_w1", [D, 2 * DFF])
    ev_w2 = din("ev_ffn_w2", [DFF, D])
    od_w_in = din("od_w_in", [D, 3008])
    od_kr_sw = din("od_kr_sw", [D, 64])
    od_qg = din("od_q_norm_g", [256])
    od_kvg = din("od_kv_norm_g", [128])
    od_w_uq = din("od_w_uq", [256, 768])
    od_w_uq_sw = din("od_w_uq_sw", [256, 256])
    od_w_ukv = din("od_w_ukv", [128, 1024])
    od_hg_g = din("od_hg_norm_g", [1, 128])
    od_w_out = din("od_w_out", [D, D])
    od_router = din("od_router", [D, NE])
    od_w1 = din("od_exp_w1", [NE, D, 2 * DFF])
    od_w2 = din("od_exp_w2", [NE, DFF, D])
    ident_d = din("ident", [128, 128])
    cos_d = din("cosT", [128, T])
    sin_d = din("sinT", [128, T])
    mask_d = din("masks", [4, 128, 128])
    out_d = nc.dram_tensor("out", [S, D], F32, kind="ExternalOutput").ap()

    def dscr(name, shape, dt=F32):
        return nc.dram_tensor(name, list(shape), dt).ap()

    mrow_d = dscr("mrow_d", [2, 2, 6 * D])
    pfm = dscr("pfm", [22 * 128, T])
    ptm = dscr("ptm", [T, 1536])
    xa = dscr("xa", [T, D])
    xb = dscr("xb", [T, D])
    xc = dscr("xc", [T, D])
    dbg_keys = []

    def dump(name, src_ap, rk, eng="sp"):
        if debug is None or name not in debug:
            return
        d_ = nc.dram_tensor("dbg_" + name, list(src_ap.shape), F32, kind="ExternalOutput").ap()
        P.dma(eng, d_, src_ap, rk, [("dbg", name)], cls="st")
        dbg_keys.append(("dbg", name))

    def MM(out, lhsT, rhs, start, stop, r, w):
        P.op("pe", lambda e, a=(out, lhsT, rhs, start, stop): e.matmul(a[0], a[1], a[2], start=a[3], stop=a[4]), r, w)

    def TR(out, in_, ident, r, w):
        P.op("pe", lambda e, a=(out, in_, ident): e.transpose(a[0], a[1], a[2]), r, w)

    def ACT(out, in_, func, r, w, bias=None, scale=None, accum_out=None):
        kw = {}
        if bias is not None:
            kw["bias"] = bias
        if scale is not None:
            kw["scale"] = scale
        if accum_out is not None:
            kw["accum_out"] = accum_out
        P.op("act", lambda e, a=(out, in_, func), kw=kw: e.activation(a[0], a[1], a[2], **kw), r, w)

    def TT(out, in0, in1, op, r, w, eng="dve"):
        P.op(eng, lambda e, a=(out, in0, in1, op): e.tensor_tensor(a[0], a[1], a[2], a[3]), r, w)

    def TS(out, in0, s1, s2, op0, op1, r, w, eng="dve"):
        if s2 is None:
            P.op(eng, lambda e, a=(out, in0, s1, op0): e.tensor_scalar(a[0], a[1], a[2], None, a[3]), r, w)
        else:
            P.op(eng, lambda e, a=(out, in0, s1, s2, op0, op1): e.tensor_scalar(a[0], a[1], a[2], a[3], a[4], a[5]), r, w)

    def STT(out, in0, scalar, in1, op0, op1, r, w):
        P.op("dve", lambda e, a=(out, in0, scalar, in1, op0, op1): e.scalar_tensor_tensor(a[0], a[1], a[2], a[3], a[4], a[5]), r, w)

    def CP(out, in_, r, w, eng="dve"):
        if eng == "act":
            P.op("act", lambda e, a=(out, in_): e.copy(a[0], a[1]), r, w)
        else:
            P.op(eng, lambda e, a=(out, in_): e.tensor_copy(a[0], a[1]), r, w)

    def RECIP(out, in_, r, w):
        P.op("dve", lambda e, a=(out, in_): e.reciprocal(a[0], a[1]), r, w)

    def MEMSET(ap, val, r, w, eng="dve"):
        P.op(eng, lambda e, a=(ap, val): e.memset(a[0], a[1]), r, w)

    def col_load(dst, src1d, w, cls="m"):
        P.dma("sp", dst, src1d.rearrange("(p o) -> p o", o=1), (), w, cls=cls)

    with ExitStack() as top:
        uid = [0]

        def sbt(es, name, shape, dt):
            uid[0] += 1
            return es.enter_context(nc.sbuf_tensor("sb%d_%s" % (uid[0], name), list(shape), dt))

        ps = [None] * 8
        for i in range(2, 8):
            ps[i] = top.enter_context(nc.psum_tensor("ps%d" % i, [128, 512], F32))
        PSK = [("ps", i) for i in range(8)]
        ps01 = [ExitStack(), 0]

        def alloc_ps01():
            ps01[1] += 1
            for i in range(2):
                ps[i] = ps01[0].enter_context(nc.psum_tensor("ps%d_%d" % (i, ps01[1]), [128, 512], F32))

        def free_ps01():
            ps01[0].close()
            ps01[0] = ExitStack()
            ps[0] = ps[1] = None

        alloc_ps01()

        idf = sbt(top, "idf", [128, 128], F32)
        idb = sbt(top, "idb", [128, 128], BF16)
        masks = sbt(top, "masks", [128, 4, 128], F32)
        ones_f = sbt(top, "ones_f", [128, 128], F32)
        eps_ln = sbt(top, "eps_ln", [128, 1], F32)
        eps_rms = sbt(top, "eps_rms", [128, 1], F32)
        P.dma("sp", idf[:], ident_d, (), ["idf"], cls="m")
        P.dma("sp", masks[:], mask_d.rearrange("m p c -> p m c"), (), ["masks"], cls="m")
        CP(idb[:], idf[:], ["idf"], ["idb"])
        MEMSET(ones_f[:], 1.0, (), ["ones_f"])
        MEMSET(eps_ln[:], LN_EPS, (), ["eps"])
        MEMSET(eps_rms[:], RMS_EPS, (), ["eps"])

        with ExitStack() as es:
            scin = sbt(es, "scin", [128, 2, 8], F32)
            sce = sbt(es, "sce", [128, 2, 8], F32)
            scT = sbt(es, "scT", [128, 8, 2], BF16)
            wr = Ring([sbt(es, "adaw%d" % i, [128, 8, 512], BF16) for i in range(3)], "adaw")
            mrow = sbt(es, "mrow", [2, 6 * D], F32)
            brow = sbt(es, "brow", [2, 6 * D], F32)
            for r in range(2):
                P.dma("sp", scin[:, r, :], c2[r].rearrange("(p k) -> p k", k=8), (), ["scin"], cls="m")
            ACT(sce[:], scin[:], AF.Exp, ["scin"], ["sce"], scale=-1.0)
            TS(sce[:], sce[:], 1.0, None, ALU.add, None, ["sce"], ["sce"])
            RECIP(sce[:], sce[:], ["sce"], ["sce"])
            TT(scT[:].rearrange("p k r -> p r k"), scin[:], sce[:], ALU.mult, ["scin", "sce"], ["scT"])
            bi = 0
            for L in range(2):
                for r in range(2):
                    P.dma("sp", brow[r:r + 1, :], ada_b[L:L + 1, :], ["mrow"], ["brow"], cls="m")
                for n in range(12):
                    slot, sk = wr.next()
                    P.dma("pool", slot[:], ada_w[L][:, n * 512:(n + 1) * 512].rearrange("(p k) n -> p k n", k=8), (), [sk], cls="w")
                    b = bi % 2
                    bi += 1
                    for k in range(8):
                        MM(ps[b][0:2, :], scT[:, k, :], slot[:, k, :], k == 0, k == 7, ["scT", sk], [PSK[b]])
                    TT(mrow[:, n * 512:(n + 1) * 512], ps[b][0:2, :], brow[:, n * 512:(n + 1) * 512], ALU.add,
                       [PSK[b], "brow"], ["mrow"])
                for i in (1, 4):
                    TS(mrow[:, i * D:(i + 1) * D], mrow[:, i * D:(i + 1) * D], 1.0, None, ALU.add, None, ["mrow"], ["mrow"])
                P.dma("sp", mrow_d[L], mrow[:], ["mrow"], [("mrow_d", L)], cls="st")
                if L == 0:
                    dump("mrow", mrow[:], ["mrow"])
                    dump("scin", scin[:].rearrange("p r k -> p (r k)"), ["scin"])
                    dump("sce", sce[:].rearrange("p r k -> p (r k)"), ["sce"])

        def bload(tile, L, r, i):
            P.dma("sp", tile[:], mrow_d[L, r, i * D:(i + 1) * D].partition_broadcast(128), [("mrow_d", L)], [tile.name], cls="m")

        def bload_vec(tile, src1d):
            P.dma("sp", tile[:], src1d.partition_broadcast(128), (), [tile.name], cls="m")

        def mod_transpose(src, skey, mA, mB, hT, t, work, pbanks, logits=None):
            h, hk = work.next()
            TT(h[:], src, mA[:], ALU.mult, [skey, mA.name], [hk])
            TT(h[:], h[:], mB[:], ALU.add, [hk, mB.name], [hk])
            if t == 2:
                dump("h2", h[:], [hk])
                dump("mA", mA[:], [mA.name])
            ba, bb = pbanks
            for k in range(8):
                b = ba if k < 4 else bb
                TR(ps[b][:, (k % 4) * 128:(k % 4 + 1) * 128], h[:, k * 128:(k + 1) * 128], idf[:], [hk, "idf"], [PSK[b]])
            if logits is None:
                for j, b in enumerate((ba, bb)):
                    CP(hT[:, j * 4:(j + 1) * 4, t * 128:(t + 1) * 128], ps[b][:].rearrange("p (k n) -> p k n", k=4),
                       [PSK[b]], [("hT", t)], eng="act")
            if logits is not None:
                h2T, wrt, comb, lb = logits
                sstop(10)
                for j, b in enumerate((ba, bb)):
                    CP(h2T[:, j * 4:(j + 1) * 4, :], ps[b][:].rearrange("p (k n) -> p k n", k=4), [PSK[b]], ["h2T"])
                CP(hT[:, :, t * 128:(t + 1) * 128], h2T[:], ["h2T"], [("hT", t)], eng="act")
                sstop(11)
                for k in range(8):
                    MM(ps[lb][:, 0:8], h2T[:, k, :], wrt[:, k, :], k == 0, k == 7, ["h2T", "wrt"], [PSK[lb]])
                sstop(12)
                route(ps[lb][:, 0:8], PSK[lb], comb, t)

        def route(lg_ps, lgk, comb, t):
            lg, m8, msk, ex, ssum = comb["lg"], comb["m8"], comb["msk"], comb["ex"], comb["ssum"]
            CP(lg[:], lg_ps, [lgk], ["r_lg"])
            sstop(13)
            P.op("dve", lambda e: e.max(out=m8[:], in_=lg[:]), ["r_lg"], ["r_m8"])
            sstop(14)
            TS(msk[:], lg[:], m8[:, 1:2], None, ALU.is_ge, None, ["r_lg", "r_m8"], ["r_msk"])
            sstop(15)
            TS(ex[:], lg[:], m8[:, 0:1], None, ALU.subtract, None, ["r_lg", "r_m8"], ["r_ex"])
            ACT(ex[:], ex[:], AF.Exp, ["r_ex"], ["r_ex"])
            TT(ex[:], ex[:], msk[:], ALU.mult, ["r_ex", "r_msk"], ["r_ex"])
            P.op("dve", lambda e: e.reduce_sum(ssum[:], ex[:], AX.X), ["r_ex"], ["r_ss"])
            RECIP(ssum[:], ssum[:], ["r_ss"], ["r_ss"])
            TS(comb["comb"][:, t - NTC, :], ex[:], ssum[:, 0:1], None, ALU.mult, None, ["r_ex", "r_ss"], [("comb", t)])

        def layernorm_tile(tl, tk, gT, bT, dst, dk, small, sk):
            st, mv, rstd = small
            tv = tl[:].rearrange("p (c f) -> p c f", f=512)
            for c in range(2):
                P.op("dve", lambda e, c=c: e.bn_stats(st[:, c, :], tv[:, c, :]), [tk], [sk])
            P.op("dve", lambda e: e.bn_aggr(mv[:], st[:]), [sk], [sk])
            ACT(rstd[:], mv[:, 1:2], AF.Ln, [sk], [sk], bias=eps_ln[:], scale=1.0)
            ACT(rstd[:], rstd[:], AF.Exp, [sk], [sk], scale=-0.5)
            TS(tl[:], tl[:], mv[:, 0:1], rstd[:, 0:1], ALU.subtract, ALU.mult, [tk, sk], [tk])
            TT(tl[:], tl[:], gT[:], ALU.mult, [tk, gT.name], [tk])
            TT(dst, tl[:], bT[:], ALU.add, [tk, bT.name], [dk], eng="pool")

        def stream_w(ring, src_ap, eng="pool"):
            slot, sk = ring.next()
            P.dma(eng, slot[:] if src_ap.shape[-1] == slot.shape[-1] else slot[:, :, 0:src_ap.shape[-1]], src_ap, (), [sk], cls="w")
            return slot, sk

        def proj_fm(hT, ntok, slot, sk, c0, ncols, rows0, stg, banks, bctr):
            for (a, w) in tgroups(0, ntok):
                b = banks[bctr[0] % len(banks)]
                bctr[0] += 1
                for k in range(8):
                    MM(ps[b][0:ncols, 0:w], slot[:, k, c0:c0 + ncols], hT[:, k, a:a + w], k == 0, k == 7,
                       [sk] + tkeys("hT", a, w), [PSK[b]])
                s, stk = stg.next()
                CP(s[0:ncols, 0:w], ps[b][0:ncols, 0:w], [PSK[b]], [stk], eng="act")
                P.dma("sp", pfm[rows0:rows0 + ncols, a:a + w], s[0:ncols, 0:w], [stk], tkeys(("pfm", rows0 // 128), a, w), cls="st")

        def proj_tm(hT, tiles, slot, sk, ncols, col0, stg, banks, bctr):
            for t in tiles:
                b = banks[bctr[0] % len(banks)]
                bctr[0] += 1
                for k in range(8):
                    MM(ps[b][:, 0:ncols], hT[:, k, t * 128:(t + 1) * 128], slot[:, k, 0:ncols], k == 0, k == 7,
                       [sk, ("hT", t)], [PSK[b]])
                s, stk = stg.next()
                CP(s[:, 0:ncols], ps[b][:, 0:ncols], [PSK[b]], [stk], eng="act")
                P.dma("sp", ptm[t * 128:(t + 1) * 128, col0:col0 + ncols], s[:, 0:ncols], [stk], [("ptm", col0 // 512, t)], cls="st")

        def wsrc(w_ap, c0, n):
            return w_ap[:, c0:c0 + n].rearrange("(k p) n -> p k n", p=128)

        def attention(es, maps, Vaug, qranges, scale, finish, sbanks, abanks, tag=""):
            ptr = Ring([sbt(es, "pT%s_%d" % (tag, i), [128, 512], BF16) for i in range(3)], "pT" + tag)
            sctr = 0
            actr = 0
            for (q0, qw, ktiles) in qranges:
                nsub = qw // 128
                for mi, parts in enumerate(maps):
                    accs = abanks[actr % len(abanks)]
                    actr += 1
                    def score(idx_):
                        kt_ = ktiles[idx_]
                        sbk = sbanks[(sctr + idx_) % len(sbanks)]
                        for pi, (kfn, qfn, rk) in enumerate(parts):
                            MM(ps[sbk][:, 0:qw], kfn(kt_), qfn(q0, qw), pi == 0, pi == len(parts) - 1, rk, [PSK[sbk]])
                        return sbk
                    sb_cur = score(0)
                    for idx, kt in enumerate(ktiles):
                        sb_next = score(idx + 1) if idx + 1 < len(ktiles) else None
                        sb_ = sb_cur
                        pt, ptk = ptr.next()
                        ACT(pt[:, 0:qw], ps[sb_][:, 0:qw], AF.Exp, [PSK[sb_]], [ptk], scale=scale)
                        for s in range(nsub):
                            bk = accs[s // 2]
                            c0 = (s % 2) * 256
                            MM(ps[bk][:, c0:c0 + 130], pt[:, s * 128:(s + 1) * 128], Vaug(kt), idx == 0 and s % 2 == 0,
                               idx == len(ktiles) - 1, [ptk, ("Vaug", kt)], [PSK[bk]])
                        sb_cur = sb_next
                    sctr += len(ktiles)
                    for s in range(nsub):
                        bk = accs[s // 2]
                        c0 = (s % 2) * 256
                        finish(mi, s, q0, ps[bk][:, c0:c0 + 130], PSK[bk])

        def scan_phase(es, cfg, mixT):
            nch, hpc, dk, C, gsc = cfg["nch"], cfg["hpc"], cfg["dk"], cfg["C"], cfg["gsc"]
            nchunks = T // C
            cpt = 128 // C
            out_t0 = cfg["out_t0"]
            NG = 2
            ncg = nch // NG
            big = lambda n: sbt(es, n, [128, T], F32)
            ng, Cs, Ce, ee, qf, kf = big("sc_ng"), big("sc_Cs"), big("sc_Ce"), big("sc_ee"), big("sc_qf"), big("sc_kf")
            onesT = sbt(es, "sc_ones", [128, T], BF16)
            MEMSET(onesT[:], 1.0, (), ["sc_ones"])
            qh = [sbt(es, "sc_qh%d" % i, [128, T], BF16) for i in range(ncg)]
            kh = [sbt(es, "sc_kh%d" % i, [128, T], BF16) for i in range(ncg)]
            qm = None
            if hpc == 2:
                qm = [[sbt(es, "sc_qm%d_%d" % (i, j), [128, T], BF16) for j in range(2)] for i in range(ncg)]
                for i in range(ncg):
                    for j in range(2):
                        MEMSET(qm[i][j][:], 0.0, (), [("sc_qh", i)])
            c_out0 = out_t0 * cpt
            vbt = sbt(es, "sc_v", [128, nchunks, 256], BF16)
            oacc = sbt(es, "sc_oacc", [128, nchunks - c_out0, 256], F32)
            if C < 128:
                MEMSET(vbt[:], 0.0, (), [("sc_v", c) for c in range(nchunks)])
            cols = [[sbt(es, "sc_c%d_%d" % (i, j), [128, nchunks], F32) for j in range(6)] for i in range(ncg)]
            Sst = [sbt(es, "sc_S%d" % i, [128, 128], F32) for i in range(ncg)]
            Sef = [[sbt(es, "sc_Se%d_%d" % (i, j), [128, 128], BF16) for j in range(2)] for i in range(ncg)]
            tmpS = sbt(es, "sc_tmpS", [128, 128], F32)
            khtm = Ring([sbt(es, "sc_khtm%d" % i, [128, 256], BF16) for i in range(2)], "khtm")
            Am = Ring([sbt(es, "sc_Am%d" % i, [128, 2 * C], BF16) for i in range(2)], "Am")
            ldr = Ring([sbt(es, "sc_ld%d" % i, [128, 256], F32) for i in range(3)], "scld")
            for tl_ in khtm.tiles:
                MEMSET(tl_[:], 0.0, (), [("khtm", 0), ("khtm", 1)])
            for tl_ in Am.tiles:
                MEMSET(tl_[:], 0.0, (), [("Am", 0), ("Am", 1)])
            gT = sbt(es, "sc_gT", [128, 128], F32)
            bload_vec(gT, cfg["norm_g"])
            wk = Ring([sbt(es, "sc_mw%d" % i, [128, 256], F32) for i in range(2)], "scmw")
            gk = Ring([sbt(es, "sc_mg%d" % i, [128, 256], F32) for i in range(2)], "scmg")
            ssq = sbt(es, "sc_ssq", [128, 2], F32)
            for hg in range(NG):
                vc0 = cfg["vcol"] + hg * 256
                for c in range(nchunks):
                    l, lk = ldr.next()
                    P.dma("sp", l[0:C, :], ptm[c * C:(c + 1) * C, vc0:vc0 + 256], [("ptm", cfg["vcol"] // 512, (c * C) // 128)], [lk])
                    CP(vbt[0:C, c, :], l[0:C, :], [lk], [("sc_v", c)], eng="pool")
                for d in range(2):
                    maskF = masks[:, (0 if C == 128 else 2) + d, 0:C]
                    for lc in range(ncg):
                        cc = hg * ncg + lc
                        cfg["load_q"](cc, qf)
                        cfg["make_ng_k"](cc, d, ng, kf, ee)
                        sstop(-3)
                        P.op("dve", lambda e: e.tensor_tensor_scan(Cs[:], onesT[:], ng[:], 0.0, ALU.mult, ALU.add),
                             ["sc_ones", "sc_ng"], ["sc_Cs"])
                        TT(Ce[:], Cs[:], ng[:], ALU.subtract, ["sc_Cs", "sc_ng"], ["sc_Ce"])
                        sstop(-2)
                        Cs3 = Cs[:].rearrange("p (n c) -> p n c", c=C)
                        Ce3 = Ce[:].rearrange("p (n c) -> p n c", c=C)
                        cA, cZ, cR, c1, c2_, c3 = cols[lc]
                        ck = ("sc_cols", lc)
                        CP(cA[:], Cs3[:, :, C - 1], ["sc_Cs"], [ck])
                        MEMSET(cZ[:, 0:1], 0.0, (), [ck])
                        CP(cZ[:, 1:nchunks], cA[:, 0:nchunks - 1], [ck], [ck])
                        if d == 0:
                            CP(cR[:], Cs3[:, :, C // 2 - 1], ["sc_Cs"], [ck])
                            base3 = Cs3
                        else:
                            CP(cR[:], Ce3[:, :, C // 2], ["sc_Ce"], [ck])
                            base3 = Ce3
                        TT(c1[:], cR[:], cZ[:], ALU.subtract, [ck], [ck])
                        TT(c2_[:], cA[:], cZ[:], ALU.subtract, [ck], [ck])
                        TT(c3[:], cA[:], cR[:], ALU.subtract, [ck], [ck])
                        for c_ in (c1, c2_, c3):
                            ACT(c_[:], c_[:], AF.Exp, [ck], [ck], scale=-gsc)
                        sstop(-1)
                        rel = ee
                        TT(rel[:].rearrange("p (n c) -> p n c", c=C), base3, cR[:].unsqueeze(2).to_broadcast([128, nchunks, C]),
                           ALU.subtract, ["sc_Cs", "sc_Ce", ck], ["sc_ee"])
                        sq = -gsc if d == 0 else gsc
                        ACT(Ce[:], rel[:], AF.Exp, ["sc_ee"], ["sc_Ce"], scale=sq)
                        TT(qh[lc][:], qf[:], Ce[:], ALU.mult, ["sc_qf", "sc_Ce"], [("sc_qh", lc)])
                        if hpc == 2:
                            for j in range(2):
                                CP(qm[lc][j][j * 64:(j + 1) * 64, :], qh[lc][j * 64:(j + 1) * 64, :], [("sc_qh", lc)], [("sc_qh", lc)], eng="pool")
                        ACT(Ce[:], rel[:], AF.Exp, ["sc_ee"], ["sc_Ce"], scale=-sq)
                        TT(kh[lc][:], kf[:], Ce[:], ALU.mult, ["sc_kf", "sc_Ce"], [("sc_kh", lc)])
                    sstop(0)
                    if d == 0:
                        order = list(range(nchunks))
                    else:
                        nctx = CTX // C
                        order = list(range(nctx - 1, -1, -1)) + list(range(nchunks - 1, nctx - 1, -1))
                    for lc in range(ncg):
                        MEMSET(Sst[lc][:], 0.0, (), [("sc_S", lc)])
                        MEMSET(Sef[lc][0][:], 0.0, (), [("sc_Se", lc, 0)])
                    bK, bA, bO, bD = [0, 1], [2, 3], [4, 5], [6, 7]
                    eM = [cols[lc][3 if d == 0 else 5] for lc in range(ncg)]
                    eL = [cols[lc][4] for lc in range(ncg)]
                    eLM = [cols[lc][5 if d == 0 else 3] for lc in range(ncg)]
                    sstop(2)
                    def qsel(lc, hl, tok0_):
                        return qm[lc][hl % 2][:, tok0_:tok0_ + C] if hpc == 2 else qh[lc][:, tok0_:tok0_ + C]

                    def front(oi_):
                        tok0_ = order[oi_] * C
                        kb = bK[oi_ % 2]
                        for lc in range(ncg):
                            MM(ps[kb][0:C, lc * 128:(lc + 1) * 128], kh[lc][:, tok0_:tok0_ + C], idb[:], lc == 0, True,
                               [("sc_kh", lc), "idb"], [PSK[kb]])
                        ktm_, ktk_ = khtm.next()
                        CP(ktm_[0:C, 0:ncg * 128], ps[kb][0:C, 0:ncg * 128], [PSK[kb]], [ktk_], eng="act")
                        ab = bA[oi_ % 2]
                        for hl in range(2):
                            lc = hl // hpc
                            MM(ps[ab][0:C, hl * C:(hl + 1) * C], kh[lc][:, tok0_:tok0_ + C],
                               qsel(lc, hl, tok0_), hl == 0, True, [("sc_kh", lc), ("sc_qh", lc)], [PSK[ab]])
                        am_, amk_ = Am.next()
                        TT(am_[0:C, :].rearrange("p (h c) -> p h c", c=C),
                           ps[ab][0:C, 0:2 * C].rearrange("p (h c) -> p h c", c=C),
                           maskF[0:C, None, :].to_broadcast([C, 2, C]), ALU.mult, [PSK[ab], "masks"], [amk_])
                        return ktm_, ktk_, am_, amk_

                    cur = front(0)
                    for oi, c in enumerate(order):
                        pb = 0
                        t = (c * C) // 128
                        tok0 = c * C
                        r2 = oi % 2
                        nxt = front(oi + 1) if oi + 1 < len(order) else None
                        ktm, ktk, am, amk = cur
                        cur = nxt
                        for _once in (0,):
                            if oi == len(order) - 1:
                                break
                            sstop(5)
                            db = bD[r2]
                            for hl in range(2):
                                lc, r0 = hl // hpc, (hl % hpc) * dk
                                MM(ps[db][:, hl * 128:(hl + 1) * 128], ktm[:, lc * 128:(lc + 1) * 128],
                                   vbt[:, c, hl * 128:(hl + 1) * 128], hl == 0, True, [ktk, ("sc_v", c)], [PSK[db]])
                            cn = order[oi + 1]
                            for lc in range(ncg):
                                for j in range(hpc):
                                    hl = lc * hpc + j
                                    r0 = j * dk
                                    TS(tmpS[r0:r0 + dk, :], ps[db][r0:r0 + dk, hl * 128:(hl + 1) * 128], eLM[lc][r0:r0 + dk, c:c + 1], None, ALU.mult, None,
                                       [PSK[db], ("sc_cols", lc)], ["sc_tmpS"])
                                STT(Sst[lc][:], Sst[lc][:], eL[lc][:, c:c + 1], tmpS[:], ALU.mult, ALU.add,
                                    [("sc_S", lc), "sc_tmpS", ("sc_cols", lc)], [("sc_S", lc)])
                                TS(Sef[lc][(oi + 1) % 2][:], Sst[lc][:], eM[lc][:, cn:cn + 1], None, ALU.mult, None,
                                   [("sc_S", lc), ("sc_cols", lc)], [("sc_Se", lc, (oi + 1) % 2)])
                        sstop(4)
                        need_out = t >= out_t0
                        ob = bO[r2]
                        if need_out:
                            for hl in range(2):
                                lc, r0 = hl // hpc, (hl % hpc) * dk
                                MM(ps[ob][pb:pb + C, hl * 128:(hl + 1) * 128], am[:, hl * C:(hl + 1) * C],
                                   vbt[:, c, hl * 128:(hl + 1) * 128], hl == 0, False, [amk, ("sc_v", c)], [PSK[ob]])
                                MM(ps[ob][pb:pb + C, hl * 128:(hl + 1) * 128], qsel(lc, hl, tok0),
                                   Sef[lc][oi % 2][:, :], False, True, [("sc_qh", lc), ("sc_Se", lc, oi % 2)], [PSK[ob]])
                            okey = ("sc_oacc", c)
                            if d == 0:
                                CP(oacc[0:C, c - c_out0, :], ps[ob][0:C, 0:256], [PSK[ob]], [okey], eng="act")
                            else:
                                TT(oacc[0:C, c - c_out0, :], oacc[0:C, c - c_out0, :], ps[ob][0:C, 0:256], ALU.add,
                                   [PSK[ob], okey], [okey])
                sstop(6)
                gc0 = cfg["gcol"] + hg * 256
                for c in range(c_out0, nchunks):
                    t = (c * C) // 128
                    o = oacc[0:C, c - c_out0, :]
                    okeys = [("sc_oacc", c)]
                    w_, wk_ = wk.next()
                    g_, gk_ = gk.next()
                    P.dma("sp", g_[0:C, :], ptm[c * C:(c + 1) * C, gc0:gc0 + 256], [("ptm", cfg["gcol"] // 512, t)], [gk_])
                    TT(w_[0:C, :], o, o, ALU.mult, okeys, [wk_])
                    P.op("dve", lambda e, w_=w_: e.reduce_sum(ssq[0:C, :], w_[0:C, :].rearrange("p (h v) -> p h v", v=128), AX.X), [wk_], ["sc_ssq"])
                    ACT(ssq[0:C, :], ssq[0:C, :], AF.Ln, ["sc_ssq"], ["sc_ssq"], bias=eps_rms[0:C, :], scale=1.0 / 128)
                    ACT(ssq[0:C, :], ssq[0:C, :], AF.Exp, ["sc_ssq"], ["sc_ssq"], scale=-0.5)
                    TT(w_[0:C, :].rearrange("p (h v) -> p h v", v=128), o.rearrange("p (h v) -> p h v", v=128),
                       ssq[0:C, :].unsqueeze(2).to_broadcast([C, 2, 128]), ALU.mult, okeys + ["sc_ssq"], [wk_])
                    TT(w_[0:C, :].rearrange("p (h v) -> p h v", v=128), w_[0:C, :].rearrange("p (h v) -> p h v", v=128),
                       gT[0:C, None, :].to_broadcast([C, 2, 128]), ALU.mult, [wk_, gT.name], [wk_])
                    e_, ek_ = ldr.next()
                    ACT(e_[0:C, :], g_[0:C, :], AF.Exp, [gk_], [ek_], scale=-1.0)
                    ACT(e_[0:C, :], e_[0:C, :], AF.Ln, [ek_], [ek_], bias=ones_f[0:C, 0:1], scale=1.0)
                    ACT(e_[0:C, :], e_[0:C, :], AF.Exp, [ek_], [ek_], scale=-1.0)
                    TT(g_[0:C, :], g_[0:C, :], e_[0:C, :], ALU.mult, [gk_, ek_], [gk_])
                    TT(w_[0:C, :], w_[0:C, :], g_[0:C, :], ALU.mult, [wk_, gk_], [wk_])
                    b = 6 + (c % 2)
                    for hl in range(2):
                        TR(ps[b][:, hl * C:(hl + 1) * C], w_[0:C, hl * 128:(hl + 1) * 128], idf[0:C, 0:C], [wk_, "idf"], [PSK[b]])
                    CP(mixT[:, 4 + 2 * hg:6 + 2 * hg, c * C:(c + 1) * C], ps[b][:, 0:2 * C].rearrange("p (h n) -> p h n", h=2),
                       [PSK[b]], [("mixT", 1, t)], eng="act")

        def outproj_phase(es, L, mixT, hT, w_out, xsrc, xdst, t0, route_cfg=None):
            wo = sbt(es, "wo", [128, 8, D], BF16)
            for hlf in range(2):
                P.dma("pool", wo[:, :, hlf * 512:(hlf + 1) * 512], wsrc(w_out, hlf * 512, 512), (), [("wo", hlf)], cls="w")
            names = ["m2"]
            mb = {}
            for r in range(2):
                if r == 1 and t0 >= NTC:
                    continue
                for i, nm in zip((2,), names):
                    tl = sbt(es, "ob_%s_%d" % (nm, r), [128, D], F32)
                    bload(tl, L, 0 if r == 0 else 1, i)
                    mb[(nm, r)] = tl
            gT = sbt(es, "ob_g", [128, D], F32)
            bT = sbt(es, "ob_b", [128, D], F32)
            bload_vec(gT, ln_g[L, 0])
            bload_vec(bT, ln_b[L, 0])
            xr = Ring([sbt(es, "ob_x%d" % i, [128, D], F32) for i in range(2)], "obx")
            yr = Ring([sbt(es, "ob_y%d" % i, [128, D], F32) for i in range(2)], "oby")
            x1r = Ring([sbt(es, "ob_x1%d" % i, [128, D], F32) for i in range(2)], "obx1")
            st = sbt(es, "ob_st", [128, 2, 6], F32)
            mv = sbt(es, "ob_mv", [128, 2], F32)
            rstd = sbt(es, "ob_rstd", [128, 1], F32)
            for t in range(t0, NT):
                r = 1 if t < NTC else 0
                xt, xk = xr.next()
                P.dma("sp", xt[:], xsrc(t), [("xsrc", L, t)], [xk])
                y, yk = yr.next()
                for hlf in range(2):
                    b = 2 * (t % 2) + hlf
                    for k in range(8):
                        MM(ps[b][:, :], mixT[:, k, t * 128:(t + 1) * 128], wo[:, k, hlf * 512:(hlf + 1) * 512], k == 0, k == 7,
                           [("mixT", k // 4, t), ("wo", hlf)], [PSK[b]])
                    TT(y[:, hlf * 512:(hlf + 1) * 512], ps[b][:, :], mb[("m2", r)][:, hlf * 512:(hlf + 1) * 512], ALU.mult,
                       [PSK[b], mb[("m2", r)].name], [yk])
                STT(y[:], xt[:], ALU_ALPHA, y[:], ALU.mult, ALU.add, [xk, yk], [yk])
                x1, x1k = x1r.next()
                layernorm_tile(y, yk, gT, bT, x1[:], x1k, (st, mv, rstd), "ob_small")
                P.dma("pool", xdst[t * 128:(t + 1) * 128, :], x1[:], [x1k], [("xdst", L, 0, t)], cls="st")

        ALU_ALPHA = float(ALPHA)

        class View:
            def __init__(self, ap, name):
                self.ap, self.name = ap, name

            def __getitem__(self, k):
                return self.ap[k]

        def ffn_phase(es, L, hT, experts, groups, xsrc, xdst, comb=None, final=False):
            maxw = max(w for _, w in groups)
            hidraw = sbt(es, "ff_hid", [128, NFF * maxw], BF16)
            hid = hidraw[:].rearrange("p (f w) -> p f w", w=maxw)
            hf32 = hidraw[:].bitcast(F32)
            ev = [View(hf32[:, i * D:(i + 1) * D], "ff_ev%d" % i) for i in range(10)]
            acc = sbt(es, "ff_acc", [128, 8, maxw], F32)
            wring = Ring([sbt(es, "ff_w_%d" % i, [128, 8192], BF16) for i in range(3)], "ffw")
            sil = Ring([sbt(es, "ff_sil%d" % i, [128, 512], F32) for i in range(2)], "ffsil")
            tmp = Ring([sbt(es, "ff_tmp%d" % i, [128, 512], F32) for i in range(2)], "fftmp")
            st = sbt(es, "ff_st", [128, 2, 6], F32)
            mv = sbt(es, "ff_mv", [128, 2], F32)
            rstd = sbt(es, "ff_rstd", [128, 1], F32)
            cbt = None
            if comb is not None:
                cbt = sbt(es, "ff_cb", [128, maxw], F32)
            hb = [0, 1, 2, 3]
            hbc = 0
            for gi, (g0, gw) in enumerate(groups):
                subs = tgroups(g0, g0 + gw)
                for ei, (w1, w2) in enumerate(experts):
                    for f4 in range(NFF // 4):
                        if comb is not None and f4 == 0:
                            P.dma("sp", cbt[:, 0:gw], comb["comb_d"][ei, g0 - CTX:g0 - CTX + gw].partition_broadcast(128),
                                  ["comb_d"], ["ff_cb"], cls="m")
                        slot_, sk = wring.next()
                        slot = slot_[:].rearrange("p (k n) -> p k n", k=8)
                        P.dma("pool", slot[:, :, 0:512], wsrc(w1, f4 * 512, 512), (), [sk], cls="w")
                        P.dma("pool", slot[:, :, 512:1024], wsrc(w1, DFF + f4 * 512, 512), (), [sk], cls="w")
                        for fj in range(4):
                            f = f4 * 4 + fj
                            for (a, w) in subs:
                                bg = hb[hbc % 4]
                                bu = hb[(hbc + 1) % 4]
                                hbc += 2
                                for k in range(8):
                                    MM(ps[bg][:, 0:w], slot[:, k, fj * 128:(fj + 1) * 128], hT[:, k, a:a + w], k == 0, k == 7,
                                       [sk] + tkeys("hT", a, w), [PSK[bg]])
                                for k in range(8):
                                    MM(ps[bu][:, 0:w], slot[:, k, 512 + fj * 128:512 + (fj + 1) * 128], hT[:, k, a:a + w], k == 0, k == 7,
                                       [sk] + tkeys("hT", a, w), [PSK[bu]])
                                s_, sk_ = sil.next()
                                ACT(s_[:, 0:w], ps[bg][:, 0:w], AF.Silu, [PSK[bg]], [sk_])
                                TT(hid[:, f, a - g0:a - g0 + w], s_[:, 0:w], ps[bu][:, 0:w], ALU.mult, [sk_, PSK[bu]], [("ff_hid", f)])
                    HF = NFF // 2
                    for c4 in range(2):
                        for fh in range(2):
                            slot_, sk = wring.next()
                            slot = slot_[:, 0:HF * 512].rearrange("p (f n) -> p f n", n=512)
                            P.dma("pool", slot, w2[fh * HF * 128:(fh + 1) * HF * 128, c4 * 512:(c4 + 1) * 512].rearrange("(f p) n -> p f n", p=128),
                                  (), [sk], cls="w")
                            first = ei == 0 and fh == 0
                            for cj in range(4):
                                c = c4 * 4 + cj
                                for (a, w) in subs:
                                    b = 4 + (hbc % 2)
                                    hbc += 1
                                    for fi in range(HF):
                                        f = fh * HF + fi
                                        MM(ps[b][:, 0:w], slot[:, fi, cj * 128:(cj + 1) * 128], hid[:, f, a - g0:a - g0 + w], fi == 0, fi == HF - 1,
                                           [sk, ("ff_hid", f)], [PSK[b]])
                                    av = acc[:, c, a - g0:a - g0 + w]
                                    ak = ("ff_acc", c, a)
                                    if comb is None:
                                        if first:
                                            CP(av, ps[b][:, 0:w], [PSK[b]], [ak], eng="act")
                                        else:
                                            TT(av, av, ps[b][:, 0:w], ALU.add, [ak, PSK[b]], [ak])
                                    elif first:
                                        TT(av, ps[b][:, 0:w], cbt[:, a - g0:a - g0 + w], ALU.mult, [PSK[b], "ff_cb"], [ak])
                                    else:
                                        t_, tk_ = tmp.next()
                                        TT(t_[:, 0:w], ps[b][:, 0:w], cbt[:, a - g0:a - g0 + w], ALU.mult, [PSK[b], "ff_cb"], [tk_])
                                        TT(av, av, t_[:, 0:w], ALU.add, [ak, tk_], [ak])
                P.barrier()
                tiles = list(range(g0 // 128, (g0 + gw) // 128))
                mb = {}
                for r in sorted(set(1 if t < NTC else 0 for t in tiles)):
                    mb[r] = ev[5 + r]
                    bload(mb[r], L, 0 if r == 0 else 1, 5)
                gT, bT = ev[3], ev[4]
                bload_vec(gT, ln_g[L, 1])
                bload_vec(bT, ln_b[L, 1])
                for ti, t in enumerate(tiles):
                    r = 1 if t < NTC else 0
                    a512 = g0 + ((t * 128 - g0) // 512) * 512
                    eb = 0 if ti % 2 == 0 else 7
                    xt, xk = ev[eb], ev[eb].name
                    P.dma("sp", xt[:], xsrc[t * 128:(t + 1) * 128, :], [("xdst", L, 0, t)], [xk])
                    y, yk = ev[eb + 1], ev[eb + 1].name
                    for hlf in range(2):
                        bq = (6 if ti % 2 == 0 else 4) + hlf
                        for cq in range(4):
                            c = hlf * 4 + cq
                            TR(ps[bq][:, cq * 128:(cq + 1) * 128], acc[:, c, t * 128 - g0:(t + 1) * 128 - g0], idf[:],
                               [("ff_acc", c, a512), "idf"], [PSK[bq]])
                        TT(y[:, hlf * 512:(hlf + 1) * 512], ps[bq][:, :], mb[r][:, hlf * 512:(hlf + 1) * 512], ALU.mult,
                           [PSK[bq], mb[r].name], [yk])
                    STT(y[:], xt[:], ALU_ALPHA, y[:], ALU.mult, ALU.add, [xk, yk], [yk])
                    x2, x2k = ev[eb + 2], ev[eb + 2].name
                    layernorm_tile(y, yk, gT, bT, x2[:], x2k, (st, mv, rstd), "ff_small")
                    if final:
                        P.dma("pool", out_d[(t - NTC) * 128:(t - NTC + 1) * 128, :], x2[:], [x2k], [("out", t)], cls="st")
                    else:
                        P.dma("pool", xdst[t * 128:(t + 1) * 128, :], x2[:], [x2k], [("xsrc", L + 1, t)], cls="st")
                P.barrier()

        def p1_phase(es, L, hT, xsrc_fn, idx=(0, 1), t0=0, rkey=None, route_cfg=None):
            mbs = {}
            for r in range(2):
                if r == 1 and t0 >= NTC:
                    continue
                for i in idx:
                    tl = sbt(es, "p1_m%d_%d" % (i, r), [128, D], F32)
                    bload(tl, L, r, i)
                    mbs[(i, r)] = tl
            xr = Ring([sbt(es, "p1_x%d" % i, [128, D], F32) for i in range(2)], "p1x")
            wk = Ring([sbt(es, "p1_h%d" % i, [128, D], F32) for i in range(2)], "p1h")
            for t in range(t0, NT):
                r = 1 if t < NTC else 0
                xt, xk = xr.next()
                P.dma("sp", xt[:], xsrc_fn(t), [rkey(t) if rkey else ("xsrc", L, t)], [xk])
                mod_transpose(xt[:], xk, mbs[(idx[1], r)], mbs[(idx[0], r)], hT, t, wk, (2 * (t % 2), 2 * (t % 2) + 1), logits=route_cfg)

        def xsrc0(t):
            return ctx_in[t * 128:(t + 1) * 128, :] if t < NTC else x_in[(t - NTC) * 128:(t - NTC + 1) * 128, :]

        upto = None
        if debug is not None:
            for d_ in debug:
                if d_.startswith("upto:"):
                    upto = d_[5:]

        ORDER = ["inproj", "attn", "gla", "outproj", "l0", "l1inproj", "mla", "hgrn", "l1out", "route", "all"]
        lvl = ORDER.index(upto) if upto else len(ORDER) - 1

        SL = 99
        if debug is not None:
            for d_ in debug:
                if d_.startswith("scan:"):
                    SL = int(d_[5:])

        def sstop(k):
            if SL < k:
                P.mute = True

        def stage(ph):
            if lvl < ORDER.index(ph):
                P.mute = True

        def inproj(hT, fm, tm):
            P.barrier()
            with ExitStack() as e2:
                wr = Ring([sbt(e2, "ipw%d" % i, [128, 8, 512], BF16) for i in range(3)], "ipw")
                stg = Ring([sbt(e2, "ipstg%d" % i, [128, 512], F32) for i in range(3)], "ipstg")
                bctr = [0]
                banks = [4, 5, 6, 7]
                for (wap, c0, n, rc0, cw) in fm:
                    slot, sk = stream_w(wr, wsrc(wap, c0, n))
                    for j in range(n // cw):
                        proj_fm(hT, T, slot, sk, j * cw, cw, (rc0 + j) * 128, stg, banks, bctr)
                for j, (wap, c0) in enumerate(tm):
                    slot, sk = stream_w(wr, wsrc(wap, c0, 512))
                    proj_tm(hT, range(NT), slot, sk, 512, j * 512, stg, banks, bctr)

        def rope_prep(ea, ld, cs, items, a, w):
            P.dma("sp", cs[0][:, 0:w], cos_d[:, a:a + w], (), ["cs0"])
            P.dma("sp", cs[1][:, 0:w], sin_d[:, a:a + w], (), ["cs1"])
            for (dst, rows, rcq, rcs) in items:
                l1, k1 = ld.next()
                l2, k2 = ld.next()
                P.dma("sp", l1[0:rows, 0:w], pfm[rcq * 128:rcq * 128 + rows, a:a + w], tkeys(("pfm", rcq), a, w), [k1])
                P.dma("sp", l2[0:rows, 0:w], pfm[rcs * 128:rcs * 128 + rows, a:a + w], tkeys(("pfm", rcs), a, w), [k2])
                TT(l1[0:rows, 0:w], l1[0:rows, 0:w], cs[0][0:rows, 0:w], ALU.mult, [k1, "cs0"], [k1])
                TT(l2[0:rows, 0:w], l2[0:rows, 0:w], cs[1][0:rows, 0:w], ALU.mult, [k2, "cs1"], [k2])
                TT(dst, l1[0:rows, 0:w], l2[0:rows, 0:w], ALU.add, [k1, k2], ["ropeout"])

        def vaug_prep(Vaug, ld, col0):
            MEMSET(Vaug[:], 1.0, (), [("Vaug", t) for t in range(NT)])
            for t in range(NT):
                l, lk = ld.next()
                P.dma("sp", l[:], ptm[t * 128:(t + 1) * 128, col0:col0 + 512], [("ptm", col0 // 512, t)], [lk])
                CP(Vaug[:, t, :, 0:128], l[:].rearrange("p (h v) -> p h v", v=128), [lk], [("Vaug", t)], eng="act")

        LATQ = [(CTX + 512 * g, 512, list(range(NT))) for g in range(4)]

        with ExitStack() as l0:
            mixT = sbt(l0, "mixT", [128, 8, T], BF16)
            with ExitStack() as es:
                hT = sbt(es, "hT", [128, 8, T], BF16)
                P.barrier()
                with ExitStack() as e1:
                    p1_phase(e1, 0, hT, xsrc0)
                fm = [(ev_w_in, 0, 512, 0, 128), (ev_w_in_sw, 0, 512, 4, 128), (ev_w_in, 512, 512, 8, 128),
                      (ev_w_in_sw, 512, 512, 12, 128), (ev_w_in, 1536, 512, 16, 128), (ev_w_in, 3072, 32, 20, 16)]
                inproj(hT, fm, [(ev_w_in, 1024), (ev_w_in, 2048), (ev_w_in, 2560)])
            dump("inproj", pfm[0:128, :], tkeys(("pfm", 0), 0, T))
            stage("attn")
            P.barrier()
            with ExitStack() as ea:
                qT = sbt(ea, "qT", [128, 4, T], BF16)
                kT = sbt(ea, "kT", [128, 4, T], BF16)
                Vaug = sbt(ea, "Vaug", [128, NT, 4, 130], BF16)
                ld = Ring([sbt(ea, "a0ld%d" % i, [128, 512], F32) for i in range(6)], "a0ld")
                cs = [sbt(ea, "a0cos", [128, 512], F32), sbt(ea, "a0sin", [128, 512], F32)]
                for (a, w) in tgroups(0, T):
                    items = []
                    for h in range(4):
                        items.append((qT[:, h, a:a + w], 128, h, 4 + h))
                        items.append((kT[:, h, a:a + w], 128, 8 + h, 12 + h))
                    rope_prep(ea, ld, cs, items, a, w)
                vaug_prep(Vaug, ld, 0)
                lv = sbt(ea, "lv", [1, 256], F32)
                lp = sbt(ea, "lp", [1, 128], F32)
                ls = sbt(ea, "ls", [1, 2], F32)
                lam2 = sbt(ea, "lam2", [1, 2], F32)
                nlam = sbt(ea, "nlam", [128, 2], F32)
                P.dma("sp", lv[:], ev_lam, (), ["lv"], cls="m")
                TT(lp[:, 0:64], lv[:, 0:64], lv[:, 64:128], ALU.mult, ["lv"], ["lp"])
                TT(lp[:, 64:128], lv[:, 128:192], lv[:, 192:256], ALU.mult, ["lv"], ["lp"])
                P.op("dve", lambda e: e.reduce_sum(ls[:], lp[:].rearrange("p (a b) -> p a b", b=64), AX.X), ["lp"], ["ls"])
                ACT(ls[:], ls[:], AF.Exp, ["ls"], ["ls"])
                TT(lam2[:, 0:1], ls[:, 1:2], ls[:, 0:1], ALU.subtract, ["ls"], ["lam2"])
                TS(lam2[:, 0:1], lam2[:, 0:1], -0.2, None, ALU.add, None, ["lam2"], ["lam2"])
                CP(lam2[:, 1:2], lam2[:, 0:1], ["lam2"], ["lam2"])
                MM(ps[7][:, 0:2], ones_f[0:1, :], lam2[0:1, 0:2], True, True, ["ones_f", "lam2"], [PSK[7]])
                CP(nlam[:], ps[7][:, 0:2], [PSK[7]], ["nlam"])
                gsub = sbt(ea, "gsub", [128, 128], F32)
                bload_vec(gsub, ev_subln_g[0])
                TS(gsub[:], gsub[:], 0.8, None, ALU.mult, None, [gsub.name], [gsub.name])
                o1n = sbt(ea, "o1n", [128, 4, 128], F32)
                r1 = sbt(ea, "a0r1", [128, 1], F32)
                ss = sbt(ea, "a0ss", [128, 1], F32)
                ofr = Ring([sbt(ea, "a0of%d" % i, [128, 128], F32) for i in range(2)], "a0of")
                junk = sbt(ea, "a0junk", [128, 128], F32)
                dump("qT0", qT[:, 0, :], ["ropeout"], eng="pool")
                for h in range(4 if not (debug and "noattn" in debug) else 0):
                    def finish(mi, s, q0, acc, acck, h=h):
                        t = (q0 + s * 128) // 128
                        if mi == 0:
                            RECIP(r1[:], acc[:, 128:129], [acck], ["a0r"])
                            TS(o1n[:, s, :], acc[:, 0:128], r1[:, 0:1], None, ALU.mult, None, [acck, "a0r"], [("o1n", s)])
                            return
                        RECIP(r1[:], acc[:, 128:129], [acck], ["a0r"])
                        TT(r1[:], r1[:], nlam[:, 0:1], ALU.mult, ["a0r", "nlam"], ["a0r"])
                        of, ofk = ofr.next()
                        STT(of[:], acc[:, 0:128], r1[:, 0:1], o1n[:, s, :], ALU.mult, ALU.add, [acck, "a0r", ("o1n", s)], [ofk])
                        TT(junk[:], of[:], of[:], ALU.mult, [ofk], ["a0junk"])
                        P.op("dve", lambda e: e.reduce_sum(ss[:], junk[:], AX.X), ["a0junk"], ["a0ss"])
                        ACT(ss[:], ss[:], AF.Ln, ["a0ss"], ["a0ss"], bias=eps_rms[:], scale=1.0 / 128)
                        ACT(ss[:], ss[:], AF.Exp, ["a0ss"], ["a0ss"], scale=-0.5)
                        STT(of[:], of[:], ss[:, 0:1], gsub[:], ALU.mult, ALU.mult, [ofk, "a0ss", gsub.name], [ofk])
                        TR(ps[7][:, 0:128], of[:], idf[:], [ofk, "idf"], [PSK[7]])
                        CP(mixT[:, h, t * 128:(t + 1) * 128], ps[7][:, 0:128], [PSK[7]], [("mixT", 0, t)])
                    maps = []
                    for m in range(2):
                        pb = 64 * m
                        maps.append([(lambda kt, pb=pb, h=h: kT[pb:pb + 64, h, kt * 128:(kt + 1) * 128],
                                      lambda q0, qw, pb=pb, h=h: qT[pb:pb + 64, h, q0:q0 + qw], ["ropeout"])])
                    qr_ = [(0, CTX, list(range(NTC)))] + LATQ
                    attention(ea, maps, lambda kt, h=h: Vaug[:, kt, h, :], qr_, 0.125, finish, [0, 1, 2], [[3, 4], [5, 6]], tag="a%d" % h)
            stage("gla")
            P.barrier()
            with ExitStack() as eg:
                lrf = sbt(eg, "g_lrf", [16, 2, T], F32)
                lrT = sbt(eg, "g_lrT", [16, 2, T], BF16)
                gkf = sbt(eg, "g_gkf", [16, 2, 256], F32)
                gkw = sbt(eg, "g_gkw", [16, 2, 256], BF16)
                nb = sbt(eg, "g_nb", [128, 4], F32)
                for d in range(2):
                    P.dma("sp", lrf[:, d, :], pfm[(20 + d) * 128:(20 + d) * 128 + 16, :], tkeys(("pfm", 20 + d), 0, T), ["g_lrf"])
                    P.dma("sp", gkf[:, d, :], ev_gk_w2[d], (), ["g_gkf"], cls="m")
                    for cc in range(2):
                        col_load(nb[:, d * 2 + cc:d * 2 + cc + 1], ev_gk_b[d, cc * 128:(cc + 1) * 128], ["g_nb"])
                CP(lrT[:], lrf[:], ["g_lrf"], ["g_lrT"])
                CP(gkw[:], gkf[:], ["g_gkf"], ["g_gkw"])
                TS(nb[:], nb[:], -1.0, None, ALU.mult, None, ["g_nb"], ["g_nb"])

                def load_q(cc, qf):
                    P.dma("sp", qf[:], pfm[(16 + cc) * 128:(17 + cc) * 128, :], tkeys(("pfm", 16 + cc), 0, T), ["sc_qf"])
                    TS(qf[:], qf[:], 0.125, None, ALU.mult, None, ["sc_qf"], ["sc_qf"])

                def make_ng_k(cc, d, ng, kf, ee):
                    P.dma("sp", kf[:], pfm[(18 + cc) * 128:(19 + cc) * 128, :], tkeys(("pfm", 18 + cc), 0, T), ["sc_kf"])
                    for gi, (a, w) in enumerate(tgroups(0, T)):
                        b = gi % 2
                        MM(ps[b][:, 0:w], gkw[:, d, cc * 128:(cc + 1) * 128], lrT[:, d, a:a + w], True, True, ["g_gkw", "g_lrT"], [PSK[b]])
                        ACT(ee[:, a:a + w], ps[b][:, 0:w], AF.Exp, [PSK[b], "g_nb"], ["sc_ee"], bias=nb[:, d * 2 + cc:d * 2 + cc + 1], scale=-1.0)
                    ACT(ng[:], ee[:], AF.Ln, ["sc_ee"], ["sc_ng"], bias=ones_f[:, 0:1], scale=1.0)

                cfg = dict(nch=2, hpc=2, dk=64, C=128, gsc=1.0 / 16, out_t0=0, vcol=512, gcol=1024, norm_g=ev_gla_g[0],
                           load_q=load_q, make_ng_k=make_ng_k)
                scan_phase(eg, cfg, mixT)
            dump("mixT0", mixT[:, 0, :], [("mixT", 0, t) for t in range(NT)], eng="pool")
            dump("mixT4", mixT[:, 4, :], [("mixT", 1, t) for t in range(NT)], eng="pool")
            stage("outproj")
            P.barrier()
            with ExitStack() as eo:
                outproj_phase(eo, 0, mixT, None, ev_w_out, xsrc0, xa, 0)
            dump("x1", xa[CTX:CTX + 128, :], [("xdst", 0, 0, NTC)])
        stage("l0")
        P.barrier()
        with ExitStack() as es:
            hT = sbt(es, "hTb", [128, 8, T], BF16)
            with ExitStack() as e1:
                p1_phase(e1, 0, hT, lambda t: xa[t * 128:(t + 1) * 128, :], idx=(3, 4), rkey=lambda t: ("xdst", 0, 0, t))
            P.barrier()
            with ExitStack() as ef:
                ffn_phase(ef, 0, hT, [(ev_w1, ev_w2)], [(0, 768), (768, 768), (1536, 768)], xa, xb)
        dump("x2", xb[CTX:CTX + 128, :], [("xsrc", 1, NTC)])

        stage("l1inproj")
        if True:
          with ExitStack() as l1:
            mixT = sbt(l1, "mixT1", [128, 8, T], BF16)
            P.barrier()
            with ExitStack() as es:
                hT = sbt(es, "hT1", [128, 8, T], BF16)
                with ExitStack() as e1:
                    p1_phase(e1, 1, hT, lambda t: xb[t * 128:(t + 1) * 128, :])
                fm = [(od_w_in, 0, 256, 0, 128), (od_w_in, 256, 128, 2, 128), (od_w_in, 384, 64, 3, 64), (od_kr_sw, 0, 64, 4, 64),
                      (od_w_in, 448, 512, 5, 128), (od_w_in, 960, 512, 9, 128), (od_w_in, 1472, 512, 13, 128)]
                inproj(hT, fm, [(od_w_in, 1984), (od_w_in, 2496)])
            stage("mla")
            P.barrier()
            with ExitStack() as em:
                qn = sbt(em, "m_qn", [128, 4, T], BF16)
                qr = sbt(em, "m_qr", [128, 4, T], BF16)
                kn = sbt(em, "m_kn", [128, 4, T], BF16)
                kr = sbt(em, "m_kr", [128, T], BF16)
                MEMSET(qr[64:128, :, :], 0.0, (), ["m_qpad"])
                MEMSET(kr[64:128, :], 0.0, (), ["m_kpad"])
                Vaug = sbt(em, "m_Vaug", [128, NT, 4, 130], BF16)
                cqn = sbt(em, "m_cqn", [128, 2, T], BF16)
                ckvn = sbt(em, "m_ckvn", [128, T], BF16)
                wuq = sbt(em, "m_wuq", [128, 2, 768], BF16)
                wuqs = sbt(em, "m_wuqs", [128, 2, 256], BF16)
                wukv = sbt(em, "m_wukv", [128, 1024], BF16)
                gcol = sbt(em, "m_gcol", [128, 3], F32)
                P.dma("pool", wuq[:], od_w_uq.rearrange("(k p) n -> p k n", p=128), (), ["m_wuq"], cls="w")
                P.dma("pool", wuqs[:], od_w_uq_sw.rearrange("(k p) n -> p k n", p=128), (), ["m_wuqs"], cls="w")
                P.dma("pool", wukv[:], od_w_ukv, (), ["m_wukv"], cls="w")
                for c in range(2):
                    col_load(gcol[:, c:c + 1], od_qg[c * 128:(c + 1) * 128], ["m_gcol"])
                col_load(gcol[:, 2:3], od_kvg, ["m_gcol"])
                ld = Ring([sbt(em, "m_ld%d" % i, [128, 512], F32) for i in range(6)], "m_ld")
                cs = [sbt(em, "m_cos", [128, 512], F32), sbt(em, "m_sin", [128, 512], F32)]
                rs = sbt(em, "m_rs", [128, 512], F32)
                for gi, (a, w) in enumerate(tgroups(0, T)):
                    for (rcs, nrm, dst, gc0) in (((0, 1), 256.0, lambda c: cqn[:, c, a:a + w], 0), ((2,), 128.0, lambda c: ckvn[:, a:a + w], 2)):
                        lt = []
                        b = gi % 2
                        for ci, rc in enumerate(rcs):
                            l, lk = ld.next()
                            s2, s2k = ld.next()
                            P.dma("sp", l[:, 0:w], pfm[rc * 128:(rc + 1) * 128, a:a + w], tkeys(("pfm", rc), a, w), [lk])
                            TT(s2[:, 0:w], l[:, 0:w], l[:, 0:w], ALU.mult, [lk], [s2k])
                            MM(ps[b][:, 0:w], ones_f[:], s2[:, 0:w], ci == 0, ci == len(rcs) - 1, ["ones_f", s2k], [PSK[b]])
                            lt.append((l, lk))
                        ACT(rs[:, 0:w], ps[b][:, 0:w], AF.Ln, [PSK[b]], ["m_rs"], bias=eps_rms[:], scale=1.0 / nrm)
                        ACT(rs[:, 0:w], rs[:, 0:w], AF.Exp, ["m_rs"], ["m_rs"], scale=-0.5)
                        for ci, (l, lk) in enumerate(lt):
                            STT(dst(ci), l[:, 0:w], gcol[:, gc0 + ci:gc0 + ci + 1], rs[:, 0:w], ALU.mult, ALU.mult,
                                [lk, "m_gcol", "m_rs"], tkeys("m_cn%d" % gc0, a, w))
                    rope_prep(em, ld, cs, [(kr[0:64, a:a + w], 64, 3, 4)], a, w)
                    for h in range(4):
                        b = 2 + (h % 2)
                        for c in range(2):
                            MM(ps[b][:, 0:w], wuq[:, c, h * 192:h * 192 + 128], cqn[:, c, a:a + w], c == 0, c == 1,
                               ["m_wuq"] + tkeys("m_cn0", a, w), [PSK[b]])
                        CP(qn[:, h, a:a + w], ps[b][:, 0:w], [PSK[b]], ["m_q"], eng="act")
                        MM(ps[b][:, 0:w], wukv[:, h * 256:h * 256 + 128], ckvn[:, a:a + w], True, True,
                           ["m_wukv"] + tkeys("m_cn2", a, w), [PSK[b]])
                        CP(kn[:, h, a:a + w], ps[b][:, 0:w], [PSK[b]], ["m_k"], eng="act")
                        for c in range(2):
                            MM(ps[4][0:64, 0:w], wuq[:, c, h * 192 + 128:h * 192 + 192], cqn[:, c, a:a + w], c == 0, c == 1,
                               ["m_wuq"] + tkeys("m_cn0", a, w), [PSK[4]])
                        for c in range(2):
                            MM(ps[5][0:64, 0:w], wuqs[:, c, h * 64:(h + 1) * 64], cqn[:, c, a:a + w], c == 0, c == 1,
                               ["m_wuqs"] + tkeys("m_cn0", a, w), [PSK[5]])
                        l1, k1 = ld.next()
                        l2, k2 = ld.next()
                        TT(l1[0:64, 0:w], ps[4][0:64, 0:w], cs[0][0:64, 0:w], ALU.mult, [PSK[4], "cs0"], [k1])
                        TT(l2[0:64, 0:w], ps[5][0:64, 0:w], cs[1][0:64, 0:w], ALU.mult, [PSK[5], "cs1"], [k2])
                        TT(qr[0:64, h, a:a + w], l1[0:64, 0:w], l2[0:64, 0:w], ALU.add, [k1, k2], ["m_q"])
                MEMSET(Vaug[:], 1.0, (), [("Vaug", t) for t in range(NT)])
                for t in range(NT):
                    b = 6 + (t % 2)
                    for h in range(4):
                        MM(ps[b][:, h * 128:(h + 1) * 128], ckvn[:, t * 128:(t + 1) * 128], wukv[:, h * 256 + 128:h * 256 + 256], h == 0, True,
                           ["m_wukv", ("m_cn2", t)], [PSK[b]])
                    CP(Vaug[:, t, :, 0:128], ps[b][:].rearrange("p (h v) -> p h v", v=128), [PSK[b]], [("Vaug", t)], eng="act")
                r1 = sbt(em, "m_r1", [128, 1], F32)
                ofr = Ring([sbt(em, "m_of%d" % i, [128, 128], F32) for i in range(2)], "m_of")
                for h in range(4):
                    def finish(mi, s, q0, acc, acck, h=h):
                        t = (q0 + s * 128) // 128
                        RECIP(r1[:], acc[:, 128:129], [acck], ["m_r"])
                        of, ofk = ofr.next()
                        TS(of[:], acc[:, 0:128], r1[:, 0:1], None, ALU.mult, None, [acck, "m_r"], [ofk])
                        TR(ps[7][:, 0:128], of[:], idf[:], [ofk, "idf"], [PSK[7]])
                        CP(mixT[:, h, t * 128:(t + 1) * 128], ps[7][:, 0:128], [PSK[7]], [("mixT", 0, t)])
                    maps = [[(lambda kt, h=h: kn[:, h, kt * 128:(kt + 1) * 128], lambda q0, qw, h=h: qn[:, h, q0:q0 + qw], ["m_k", "m_q"]),
                             (lambda kt: kr[:, kt * 128:(kt + 1) * 128], lambda q0, qw, h=h: qr[:, h, q0:q0 + qw], ["ropeout", "m_q", "m_qpad", "m_kpad"])]]
                    attention(em, maps, lambda kt, h=h: Vaug[:, kt, h, :], LATQ, 192.0 ** -0.5, finish, [0, 1, 2], [[3, 4], [5, 6]], tag="m%d" % h)
            stage("hgrn")
            P.barrier()
            with ExitStack() as eg:
                lbt = sbt(eg, "h_lbt", [128, 2, 4], F32)
                lbc = sbt(eg, "h_lbc", [128, 4], F32)
                oml = sbt(eg, "h_oml", [128, 4], F32)
                for l_ in range(2):
                    for cc in range(4):
                        col_load(lbt[:, l_, cc:cc + 1], lb_table[l_, cc * 128:(cc + 1) * 128], ["h_lbt"])
                TT(lbc[:], lbt[:, 0, :], lbt[:, 1, :], ALU.subtract, ["h_lbt"], ["h_lb"])
                ACT(lbc[:], lbc[:], AF.Exp, ["h_lb"], ["h_lb"])
                TS(lbc[:], lbc[:], 1.0, None, ALU.add, None, ["h_lb"], ["h_lb"])
                RECIP(lbc[:], lbc[:], ["h_lb"], ["h_lb"])
                TS(oml[:], lbc[:], -1.0, 1.0, ALU.mult, ALU.add, ["h_lb"], ["h_oml"])

                def load_q(cc, qf):
                    P.dma("sp", qf[:], pfm[(5 + cc) * 128:(6 + cc) * 128, :], tkeys(("pfm", 5 + cc), 0, T), ["sc_qf"])

                def make_ng_k(cc, d, ng, kf, ee):
                    rc = 9 + 4 * d + cc
                    P.dma("sp", ng[:], pfm[rc * 128:(rc + 1) * 128, :], tkeys(("pfm", rc), 0, T), ["sc_ng"])
                    ACT(ee[:], ng[:], AF.Exp, ["sc_ng"], ["sc_ee"], scale=-1.0)
                    ACT(ee[:], ee[:], AF.Ln, ["sc_ee"], ["sc_ee"], bias=ones_f[:, 0:1], scale=1.0)
                    ACT(ee[:], ee[:], AF.Exp, ["sc_ee"], ["sc_ee"], scale=-1.0)
                    TS(ee[:], ee[:], oml[:, cc:cc + 1], lbc[:, cc:cc + 1], ALU.mult, ALU.add, ["sc_ee", "h_lb", "h_oml"], ["sc_ee"])
                    TS(kf[:], ee[:], -1.0, 1.0, ALU.mult, ALU.add, ["sc_ee"], ["sc_kf"])
                    ACT(ng[:], ee[:], AF.Ln, ["sc_ee"], ["sc_ng"])
                    TS(ng[:], ng[:], -1.0, None, ALU.mult, None, ["sc_ng"], ["sc_ng"])

                cfg = dict(nch=4, hpc=1, dk=128, C=64, gsc=1.0, out_t0=NTC, vcol=0, gcol=512, norm_g=od_hg_g[0],
                           load_q=load_q, make_ng_k=make_ng_k)
                scan_phase(eg, cfg, mixT)
            dump("l1mixT0", mixT[:, 0, :], [("mixT", 0, t) for t in range(NTC, NT)], eng="pool")
            dump("l1mixT4", mixT[:, 4, :], [("mixT", 1, t) for t in range(NTC, NT)], eng="pool")
            stage("l1out")
            P.barrier()
            with ExitStack() as eo:
                outproj_phase(eo, 1, mixT, None, od_w_out, lambda t: xb[t * 128:(t + 1) * 128, :], xc, NTC)
            dump("x3", xc[CTX:CTX + 128, :], [("xdst", 1, 0, NTC)])
          stage("route")
          P.barrier()
          with ExitStack() as es:
                hT = sbt(es, "hT1b", [128, 8, T], BF16)
                combt = sbt(es, "r_comb", [128, NT - NTC, NE], F32)
                comb_d = dscr("comb_d", [NE, S])
                with ExitStack() as eo:
                    combT = sbt(eo, "r_combT", [128, S], F32)
                    MEMSET(combT[:], 0.0, (), [("combT", tt) for tt in range(NTC, NT)])
                    h2T = sbt(eo, "r_h2T", [128, 8, 128], F32)
                    wrt = sbt(eo, "r_wrt", [128, 8, NE], F32)
                    P.dma("sp", wrt[:], od_router.rearrange("(k p) e -> p k e", p=128), (), ["wrt"], cls="m")
                    comb = dict(comb=combt, lg=sbt(eo, "r_lg", [128, NE], F32), m8=sbt(eo, "r_m8", [128, 8], F32),
                                msk=sbt(eo, "r_msk", [128, NE], F32), ex=sbt(eo, "r_ex", [128, NE], F32), ssum=sbt(eo, "r_ss", [128, 1], F32))
                    p1_phase(eo, 1, hT, lambda t: xc[t * 128:(t + 1) * 128, :], idx=(3, 4), t0=NTC, rkey=lambda t: ("xdst", 1, 0, t),
                             route_cfg=(h2T, wrt, comb, 6))
                    cpad = sbt(eo, "r_cpad", [128, 128], F32)
                    MEMSET(cpad[:], 0.0, (), ["cpad"])
                    for t in range(NTC, NT):
                        i = t - NTC
                        b = 4 + i % 2
                        CP(cpad[:, 0:NE], combt[:, i, :], [("comb", t), "cpad"], ["cpad"])
                        MM(ps[b][:, 0:128], cpad[:], idf[:], True, True, ["cpad", "idf"], [PSK[b]])
                        CP(combT[0:NE, i * 128:(i + 1) * 128], ps[b][0:NE, 0:128], [PSK[b]], [("combT", t)])
                    P.dma("sp", comb_d, combT[0:NE, :], [("combT", tt) for tt in range(NTC, NT)], ["comb_d"], cls="st")
                dump("comb", combt[:].rearrange("p t e -> p (t e)"), [("comb", t) for t in range(NTC, NT)])
                stage("all")
                P.barrier()
                with ExitStack() as ef:
                    ffn_phase(ef, 1, hT, [(od_w1[e], od_w2[e]) for e in range(NE)], [(CTX, 1024), (CTX + 1024, 1024)], xc, None,
                              comb=dict(comb_d=comb_d), final=True)
          done_keys = [("out", t) for t in range(NTC, NT)]
        P.emit(done_keys + dbg_keys + [("mrow_d", 1)])
        free_ps01()
    return nc, P


def _host_consts():
    ident = np.eye(128, dtype=np.float32)
    n_freq = 16
    inv = (10000.0 ** (-np.arange(n_freq, dtype=np.float32) / n_freq)).astype(np.float32)
    rows = S // 64
    row = np.repeat(np.arange(rows, dtype=np.float32), 64)
    col = np.tile(np.arange(64, dtype=np.float32), rows)
    ang = np.concatenate([row[:, None] * inv, col[:, None] * inv], axis=-1).astype(np.float32)
    cos = np.cos(ang).astype(np.float32)
    sin = np.sin(ang).astype(np.float32)
    cosT = np.ones((128, T), np.float32)
    sinT = np.zeros((128, T), np.float32)
    for p in range(128):
        i = (p % 64) // 2
        cosT[p, CTX:] = cos[:, i]
        sinT[p, CTX:] = -sin[:, i] if p % 2 == 0 else sin[:, i]
    j = np.arange(128)[:, None]
    i = np.arange(128)[None, :]
    mF = (j <= i).astype(np.float32)
    mB = (j >= i).astype(np.float32)
    m64F = np.zeros((128, 128), np.float32)
    m64B = np.zeros((128, 128), np.float32)
    jj = (np.arange(128) % 64)[:, None]
    ii = np.arange(64)[None, :]
    m64F[:, :64] = (jj <= ii)
    m64B[:, :64] = (jj >= ii)
    masks = np.stack([mF, mB, m64F, m64B]).astype(np.float32)
    return ident, cosT, sinT, masks


def _prep_inputs(inp):
    f = lambda a: np.ascontiguousarray(np.asarray(a, dtype=np.float32))
    ident, cosT, sinT, masks = _host_consts()
    sw = np.arange(1024) ^ 1
    ev_w_in = f(inp["ev_w_in"][0])
    od_w_in = f(inp["od_w_in"][0])
    od_w_uq = f(inp["od_w_uq"][0])
    sw64 = np.arange(64) ^ 1
    uq_sw = np.concatenate([od_w_uq[:, h * 192 + 128:h * 192 + 192][:, sw64] for h in range(4)], axis=1)
    shared = {
        "ada_w": f(inp["ada_w"]), "ada_b": f(inp["ada_b"]), "post_ln_g": f(inp["post_ln_g"]), "post_ln_b": f(inp["post_ln_b"]),
        "lb_table": f(inp["lb_table"]), "ev_w_in": ev_w_in, "ev_w_in_sw": f(ev_w_in[:, :1024][:, sw]),
        "ev_lam": f(inp["ev_lam"][0].reshape(1, 256)), "ev_subln_g": f(inp["ev_subln_g"]), "ev_gk_w2": f(inp["ev_gk_w2"][0]),
        "ev_gk_b": f(inp["ev_gk_b"][0]), "ev_gla_norm_g": f(inp["ev_gla_norm_g"]), "ev_w_out": f(inp["ev_w_out"][0]),
        "ev_ffn_w1": f(inp["ev_ffn_w1"][0]), "ev_ffn_w2": f(inp["ev_ffn_w2"][0]), "od_w_in": od_w_in,
        "od_kr_sw": f(od_w_in[:, 384:448][:, sw64]), "od_q_norm_g": f(inp["od_q_norm_g"][0]), "od_kv_norm_g": f(inp["od_kv_norm_g"][0]),
        "od_w_uq": od_w_uq, "od_w_uq_sw": f(uq_sw), "od_w_ukv": f(inp["od_w_ukv"][0]), "od_hg_norm_g": f(inp["od_hg_norm_g"]),
        "od_w_out": f(inp["od_w_out"][0]), "od_router": f(inp["od_router"][0]), "od_exp_w1": f(inp["od_exp_w1"][0]),
        "od_exp_w2": f(inp["od_exp_w2"][0]), "ident": ident, "cosT": cosT, "sinT": sinT, "masks": masks,
    }
    maps = []
    for b in range(8):
        m = dict(shared)
        m["x"] = f(inp["x"][b])
        m["ctx"] = f(inp["ctx"][b])
        m["c2"] = f(np.stack([inp["c"][b], inp["c_ctx"]]))
        maps.append(m)
    return maps


def kernel(**inputs):
    nc, P = build()
    maps = _prep_inputs(inputs)
    res = run_bass_kernel_spmd(nc, maps, core_ids=list(range(8)))
    return np.stack([np.asarray(r["out"], dtype=np.float32) for r in res.results], axis=0)
```
